# Optimizing a Trainium2 kernel written in Bass

```python
import math
import jax, jax.numpy as jnp
from jax import lax
import numpy as np

D_MODEL = 2048
BATCH = 4
SEQ = 4096
DEPTH = 2

CHUNK = 64
QBLK = 128
N_MIXERS = 2
N_LAYERS_A = (DEPTH + 1) // 2
N_LAYERS_B = DEPTH // 2
A_HEADS = 16
A_HEAD_DIM = D_MODEL // A_HEADS
B_HEADS = 16
B_HEAD_DIM = 128
B_V_DIM = 128
Q_LATENT = D_MODEL // 4
KV_LATENT = D_MODEL // 8
IDX_HEADS = 16
IDX_DIM = 64
IDX_TOPK = 256
REL_BUCKETS = 32
REL_MAX_DIST = 128
N_EXPERTS = 64
TOP_K = 8
D_EXPERT = 512
D_SHARED = 512
ROUTED_SCALE = 2.5
EXPERT_BLOCK = 128
DEEPNORM_ALPHA = (2 * DEPTH) ** 0.25
DEEPNORM_BETA = (8 * DEPTH) ** -0.25
LN_EPS = 1e-5
RMS_EPS = 1e-6

kernel_name = "fox_dsa_moe_deepnorm_hybrid"


def layer_norm(x, g, b):
    xf = x.astype(jnp.float32)
    mu = jnp.mean(xf, axis=-1, keepdims=True)
    var = jnp.mean(jnp.square(xf - mu), axis=-1, keepdims=True)
    return ((xf - mu) * lax.rsqrt(var + LN_EPS) * g.astype(jnp.float32) + b.astype(jnp.float32)).astype(x.dtype)


def rms_norm(x, g):
    xf = x.astype(jnp.float32)
    ms = jnp.mean(jnp.square(xf), axis=-1, keepdims=True)
    return (xf * lax.rsqrt(ms + RMS_EPS) * g.astype(jnp.float32)).astype(x.dtype)


def t5_bucket(rel):
    half = REL_BUCKETS // 2
    max_exact = half // 2
    n = jnp.abs(rel)
    large = max_exact + (jnp.log(jnp.maximum(n, 1).astype(jnp.float32) / max_exact)
                         / math.log(REL_MAX_DIST / max_exact) * (half - max_exact)).astype(jnp.int32)
    large = jnp.minimum(large, half - 1)
    return jnp.where(rel > 0, half, 0) + jnp.where(n < max_exact, n, large)


def forgetting_attention(h, w_in, b_f, w_out):
    Bn, S, _ = h.shape
    dA = A_HEADS * A_HEAD_DIM
    proj = h @ w_in
    q = proj[..., :dA].reshape(Bn, S, A_HEADS, A_HEAD_DIM)
    k = proj[..., dA:2 * dA].reshape(Bn, S, A_HEADS, A_HEAD_DIM)
    v = proj[..., 2 * dA:3 * dA].reshape(Bn, S, A_HEADS, A_HEAD_DIM)
    logf = jax.nn.log_sigmoid((proj[..., 3 * dA:] + b_f).astype(jnp.float32))
    F = jnp.cumsum(logf, axis=1)
    F_k = jnp.transpose(F, (0, 2, 1))[:, :, None, :]
    nblk = S // QBLK
    q_blocks = jnp.swapaxes(q.reshape(Bn, nblk, QBLK, A_HEADS, A_HEAD_DIM), 0, 1)
    F_blocks = jnp.swapaxes(F.reshape(Bn, nblk, QBLK, A_HEADS), 0, 1)
    kpos = jnp.arange(S, dtype=jnp.int32)
    scale = A_HEAD_DIM ** -0.5

    def block(args):
        q_blk, F_blk, i = args
        qpos = i * QBLK + jnp.arange(QBLK, dtype=jnp.int32)
        logits = jnp.einsum('bqhd,bshd->bhqs', q_blk, k).astype(jnp.float32) * scale
        logits = logits + jnp.transpose(F_blk, (0, 2, 1))[..., None] - F_k
        logits = jnp.where((kpos[None, :] <= qpos[:, None])[None, None], logits, -jnp.inf)
        p = jax.nn.softmax(logits, axis=-1).astype(v.dtype)
        return jnp.einsum('bhqs,bshd->bqhd', p, v).reshape(Bn, QBLK, dA)

    o = lax.map(block, (q_blocks, F_blocks, jnp.arange(nblk, dtype=jnp.int32)))
    o = jnp.swapaxes(o, 0, 1).reshape(Bn, S, dA)
    return o @ w_out


def indexed_sparse_attention(h, w_in, q_norm, kv_norm, w_uq, w_iq, w_uk, w_uv, w_out, rel_bias):
    Bn, S, _ = h.shape
    topk = min(IDX_TOPK, S // 4)
    proj = h @ w_in
    o1 = Q_LATENT
    o2 = o1 + KV_LATENT
    o3 = o2 + IDX_DIM
    c_q = rms_norm(proj[..., :o1], q_norm)
    c_kv = rms_norm(proj[..., o1:o2], kv_norm)
    k_idx = proj[..., o2:o3]
    w_idx = proj[..., o3:] * (IDX_HEADS ** -0.5)
    q = (c_q @ w_uq).reshape(Bn, S, B_HEADS, B_HEAD_DIM)
    q_lat = jnp.einsum('bthd,hcd->bthc', q, w_uk)
    q_idx = (c_q @ w_iq).reshape(Bn, S, IDX_HEADS, IDX_DIM)
    nblk = S // QBLK
    qi_blocks = jnp.swapaxes(q_idx.reshape(Bn, nblk, QBLK, IDX_HEADS, IDX_DIM), 0, 1)
    wi_blocks = jnp.swapaxes(w_idx.reshape(Bn, nblk, QBLK, IDX_HEADS), 0, 1)
    ql_blocks = jnp.swapaxes(q_lat.reshape(Bn, nblk, QBLK, B_HEADS, KV_LATENT), 0, 1)
    kchunk = jnp.arange(S, dtype=jnp.int32) // CHUNK
    scale = B_HEAD_DIM ** -0.5
    gather_rows = jax.vmap(lambda c, idx: c[idx])

    def block(args):
        qi, wi, ql, i = args
        qpos = i * QBLK + jnp.arange(QBLK, dtype=jnp.int32)
        qchunk = qpos // CHUNK
        sc = jnp.einsum('bqhd,bsd->bqhs', qi, k_idx).astype(jnp.float32)
        I = jnp.einsum('bqhs,bqh->bqs', jax.nn.relu(sc), wi.astype(jnp.float32)) * (IDX_DIM ** -0.5)
        I = jnp.where((kchunk[None, :] <= qchunk[:, None])[None], I, -jnp.inf)
        _, sel = lax.top_k(I, topk)
        cg = gather_rows(c_kv, sel)
        valid = (sel // CHUNK) <= qchunk[None, :, None]
        bias = rel_bias[t5_bucket(sel - qpos[None, :, None])]
        logits = jnp.einsum('bqhc,bqkc->bqhk', ql, cg).astype(jnp.float32) * scale
        logits = logits + jnp.moveaxis(bias, -1, 2).astype(jnp.float32)
        logits = jnp.where(valid[:, :, None, :], logits, -jnp.inf)
        p = jax.nn.softmax(logits, axis=-1).astype(cg.dtype)
        o_lat = jnp.einsum('bqhk,bqkc->bqhc', p, cg)
        return jnp.einsum('bqhc,hcd->bqhd', o_lat, w_uv).reshape(Bn, QBLK, B_HEADS * B_V_DIM)

    o = lax.map(block, (qi_blocks, wi_blocks, ql_blocks, jnp.arange(nblk, dtype=jnp.int32)))
    o = jnp.swapaxes(o, 0, 1).reshape(Bn, S, B_HEADS * B_V_DIM)
    return o @ w_out


def moe_ffn(h, router_w, router_b, w_gate, w_up, w_down, sh_gate, sh_up, sh_down):
    Bn, S, D = h.shape
    x2 = h.reshape(-1, D)
    N = x2.shape[0]
    scores = jax.nn.sigmoid(x2.astype(jnp.float32) @ router_w.astype(jnp.float32))
    _, sel = lax.top_k(scores + router_b.astype(jnp.float32), TOP_K)
    gate = jnp.take_along_axis(scores, sel, axis=-1)
    gate = gate / jnp.sum(gate, axis=-1, keepdims=True) * ROUTED_SCALE
    flat_e = sel.reshape(-1)
    order = jnp.argsort(flat_e)
    e_sorted = flat_e[order]
    tok_sorted = (order // TOP_K).astype(jnp.int32)
    g_sorted = gate.reshape(-1)[order]
    counts = jnp.bincount(flat_e, length=N_EXPERTS)
    padded = (counts + EXPERT_BLOCK - 1) // EXPERT_BLOCK * EXPERT_BLOCK
    pend = jnp.cumsum(padded)
    pstart = pend - padded
    cstart = jnp.cumsum(counts) - counts
    dest = pstart[e_sorted] + jnp.arange(N * TOP_K) - cstart[e_sorted]
    P = N * TOP_K + N_EXPERTS * EXPERT_BLOCK
    nblk = P // EXPERT_BLOCK
    row_tok = jnp.full((P,), N, jnp.int32).at[dest].set(tok_sorted)
    row_g = jnp.zeros((P,), jnp.float32).at[dest].set(g_sorted)
    blk_e = jnp.minimum(jnp.searchsorted(pend, jnp.arange(nblk) * EXPERT_BLOCK, side='right'),
                        N_EXPERTS - 1).astype(jnp.int32)
    xpad = jnp.concatenate([x2, jnp.zeros((1, D), x2.dtype)], axis=0)

    def step(y, inp):
        rows, g, e = inp
        xb = xpad[rows]
        hb = jax.nn.silu(xb @ w_gate[e]) * (xb @ w_up[e])
        yb = (hb @ w_down[e]) * g[:, None].astype(xb.dtype)
        return y.at[rows].add(yb), None

    y0 = jnp.zeros((N + 1, D), x2.dtype)
    y, _ = lax.scan(step, y0, (row_tok.reshape(nblk, EXPERT_BLOCK),
                               row_g.reshape(nblk, EXPERT_BLOCK), blk_e))
    shared = (jax.nn.silu(x2 @ sh_gate) * (x2 @ sh_up)) @ sh_down
    return (y[:N] + shared).reshape(Bn, S, D)


def setup_inputs(seed: int = 0) -> dict:
    key = jax.random.key(seed)
    ks = jax.random.split(key, 32)
    f32 = jnp.float32
    D = D_MODEL
    beta = DEEPNORM_BETA
    dA = A_HEADS * A_HEAD_DIM
    b_width = Q_LATENT + KV_LATENT + IDX_DIM + IDX_HEADS

    def nrm(k, shape, scale):
        return jax.random.normal(k, shape, f32) * scale

    a_w_in = nrm(ks[1], (N_LAYERS_A, D, 3 * dA + A_HEADS), D ** -0.5)
    a_w_in = a_w_in.at[:, :, 2 * dA:3 * dA].multiply(beta)
    return {
        "x": nrm(ks[0], (BATCH, SEQ, D), 1.0),
        "a_w_in": a_w_in,
        "a_b_f": 2.0 + nrm(ks[2], (N_LAYERS_A, A_HEADS), 0.5),
        "a_w_out": nrm(ks[3], (N_LAYERS_A, dA, D), dA ** -0.5 * beta),
        "b_w_in": nrm(ks[4], (N_LAYERS_B, D, b_width), D ** -0.5),
        "b_q_norm": 1.0 + nrm(ks[5], (N_LAYERS_B, Q_LATENT), 0.02),
        "b_kv_norm": 1.0 + nrm(ks[6], (N_LAYERS_B, KV_LATENT), 0.02),
        "b_w_uq": nrm(ks[7], (N_LAYERS_B, Q_LATENT, B_HEADS * B_HEAD_DIM), Q_LATENT ** -0.5),
        "b_w_iq": nrm(ks[8], (N_LAYERS_B, Q_LATENT, IDX_HEADS * IDX_DIM), Q_LATENT ** -0.5),
        "b_w_uk": nrm(ks[9], (N_LAYERS_B, B_HEADS, KV_LATENT, B_HEAD_DIM), KV_LATENT ** -0.5),
        "b_w_uv": nrm(ks[10], (N_LAYERS_B, B_HEADS, KV_LATENT, B_V_DIM), KV_LATENT ** -0.5 * beta),
        "b_w_out": nrm(ks[11], (N_LAYERS_B, B_HEADS * B_V_DIM, D), (B_HEADS * B_V_DIM) ** -0.5 * beta),
        "rel_bias": nrm(ks[12], (REL_BUCKETS, B_HEADS), 0.5),
        "ln1_g": 1.0 + nrm(ks[13], (DEPTH, D), 0.02),
        "ln1_b": nrm(ks[14], (DEPTH, D), 0.02),
        "ln2_g": 1.0 + nrm(ks[15], (DEPTH, D), 0.02),
        "ln2_b": nrm(ks[16], (DEPTH, D), 0.02),
        "router_w": nrm(ks[17], (DEPTH, D, N_EXPERTS), D ** -0.5),
        "router_b": nrm(ks[18], (DEPTH, N_EXPERTS), 0.01),
        "w_gate": nrm(ks[19], (DEPTH, N_EXPERTS, D, D_EXPERT), D ** -0.5),
        "w_up": nrm(ks[20], (DEPTH, N_EXPERTS, D, D_EXPERT), D ** -0.5 * beta),
        "w_down": nrm(ks[21], (DEPTH, N_EXPERTS, D_EXPERT, D), D_EXPERT ** -0.5 * beta),
        "sh_gate": nrm(ks[22], (DEPTH, D, D_SHARED), D ** -0.5),
        "sh_up": nrm(ks[23], (DEPTH, D, D_SHARED), D ** -0.5 * beta),
        "sh_down": nrm(ks[24], (DEPTH, D_SHARED, D), D_SHARED ** -0.5 * beta),
    }


def reference(x, a_w_in, a_b_f, a_w_out, b_w_in, b_q_norm, b_kv_norm, b_w_uq, b_w_iq, b_w_uk,
              b_w_uv, b_w_out, rel_bias, ln1_g, ln1_b, ln2_g, ln2_b, router_w, router_b,
              w_gate, w_up, w_down, sh_gate, sh_up, sh_down):
    for i in range(DEPTH):
        j = i // N_MIXERS
        if i % N_MIXERS == 0:
            m = forgetting_attention(x, a_w_in[j], a_b_f[j], a_w_out[j])
        else:
            m = indexed_sparse_attention(x, b_w_in[j], b_q_norm[j], b_kv_norm[j], b_w_uq[j],
                                         b_w_iq[j], b_w_uk[j], b_w_uv[j], b_w_out[j], rel_bias)
        x = layer_norm(DEEPNORM_ALPHA * x + m, ln1_g[i], ln1_b[i])
        f = moe_ffn(x, router_w[i], router_b[i], w_gate[i], w_up[i], w_down[i],
                    sh_gate[i], sh_up[i], sh_down[i])
        x = layer_norm(DEEPNORM_ALPHA * x + f, ln2_g[i], ln2_b[i])
    return x
```

```python
import math
import numpy as np
from contextlib import ExitStack
import concourse.bass as bass
import concourse.mybir as mybir
from concourse.bass_utils import run_bass_kernel_spmd

F32 = mybir.dt.float32
BF16 = mybir.dt.bfloat16
AF = mybir.ActivationFunctionType
ALU = mybir.AluOpType
AX = mybir.AxisListType

ENG_NAMES = ("pe", "act", "dve", "pool", "sp")
SEM_EPOCH = 30000
NEG = -1.0e30


class Buf:
    __slots__ = ("name", "writers", "readers", "dsem", "dcount", "war")

    def __init__(self, name):
        self.name = name
        self.writers = []
        self.readers = []
        self.war = []
        self.dsem = None
        self.dcount = 0


class Op:
    __slots__ = ("eng", "fn", "deps", "is_dma", "dbuf", "signal", "event")


class Sched:
    def __init__(self, nc):
        self.nc = nc
        self.es = ExitStack()
        self.ops = []
        self._sems = []
        self._n = 0
        self.done = 0
        self.bar = 0
        self.last_eng = {}
        self.dma_last = {}
        self.eng_sems = {e: [] for e in ENG_NAMES}
        self.eng_cnt = {e: 0 for e in ENG_NAMES}
        self.waited = {e: {} for e in ENG_NAMES}
        self.pes = None
        self.uid = 0
        self.stats = {e: [0, 0] for e in ENG_NAMES}
        self.free_dsems = []
        self.live_dbufs = []

    def sbuf(self, name, shape, dtype):
        self.uid += 1
        es = self.pes if self.pes is not None else self.es
        t = es.enter_context(self.nc.sbuf_tensor("%s_%d" % (name, self.uid), list(shape), dtype))
        return t, Buf(name)

    def begin_phase(self):
        assert self.pes is None
        self.pes = ExitStack()
        self.phase_no = getattr(self, "phase_no", 0) + 1
        self.skip = self.phase_no > getattr(self, "limit", 10 ** 9)

    def end_phase(self):
        if self.skip:
            self.pes.close()
            self.pes = None
            return
        self.barrier()
        self.emit()
        self.pes.close()
        self.pes = None

    def barrier(self):
        for x in ENG_NAMES:
            deps = [o for y, o in self.last_eng.items() if y != x and o >= self.bar]
            deps += [o for o in self.dma_last.values() if o >= self.bar]
            op = Op()
            op.eng = x
            op.fn = lambda e: e.nop()
            op.is_dma = False
            op.dbuf = None
            op.signal = False
            op.event = None
            op.deps = sorted(set(deps))
            self.ops.append(op)
        self.bar = len(self.ops)
        self.last_eng = {}
        self.dma_last = {}

    def psum(self, name, shape, dtype):
        t = self.es.enter_context(self.nc.psum_tensor(name, list(shape), dtype))
        return t, Buf(name)

    def sem(self, name):
        s = self.es.enter_context(self.nc.semaphore(name))
        self._sems.append(s)
        return s

    def _record(self, eng, fn, reads, writes, pwrites, is_dma, dbuf):
        if getattr(self, "skip", False):
            return -1
        deps = set()
        for b in reads:
            deps.update(b.writers)
        for b in writes:
            deps.update(b.writers)
            deps.update(b.readers)
        for b in pwrites:
            deps.update(b.readers)
            deps.update(b.war)
        op = Op()
        op.eng = eng
        op.fn = fn
        op.is_dma = is_dma
        op.dbuf = dbuf
        op.signal = False
        op.event = None
        oid = len(self.ops)
        keep = []
        if is_dma:
            self.dma_last[id(dbuf)] = oid
        else:
            self.last_eng[eng] = oid
        for d in deps:
            if d < self.bar:
                continue
            p = self.ops[d]
            if (not p.is_dma) and p.eng == eng and eng in ("pe", "sp"):
                continue
            keep.append(d)
        op.deps = sorted(keep)
        self.ops.append(op)
        for b in reads:
            b.readers.append(oid)
        for b in writes:
            b.war = [d for d in list(b.writers) + list(b.readers) if d >= self.bar]
            b.writers = [oid]
            b.readers = []
        for b in pwrites:
            b.writers.append(oid)
        return oid

    def op(self, eng, fn, reads=(), writes=(), pwrites=()):
        return self._record(eng, fn, reads, writes, pwrites, False, None)

    def dma(self, eng, out, in_, reads=(), writes=(), pwrites=(), sem_buf=None, **kw):
        assert sem_buf is not None

        def fn(e):
            return e.dma_start(out=out, in_=in_, **kw)
        return self._record(eng, fn, reads, writes, pwrites, True, sem_buf)

    def emit(self):
        nc = self.nc
        ops = self.ops
        lo = self.done
        for op in ops[lo:]:
            for d in op.deps:
                assert d >= lo, "dep on an op emitted in an earlier batch"
                if not ops[d].is_dma:
                    ops[d].signal = True
        for op in ops[lo:]:
            if op.is_dma:
                b = op.dbuf
                if b.dsem is None:
                    if self.free_dsems:
                        b.dsem, b.dcount = self.free_dsems.pop()
                    else:
                        b.dsem = self.sem("d%d" % len(self._sems))
                        b.dcount = 0
                    self.live_dbufs.append(b)
                b.dcount += 16
                op.event = (b.dsem, b.dcount)
            elif op.signal:
                e = op.eng
                k = self.eng_cnt[e] // SEM_EPOCH
                if k >= len(self.eng_sems[e]):
                    self.eng_sems[e].append(self.sem("e_%s_%d" % (e, k)))
                self.eng_cnt[e] += 1
                op.event = (self.eng_sems[e][k], self.eng_cnt[e] - k * SEM_EPOCH)
        per_eng = {e: [] for e in ENG_NAMES}
        for op in ops[lo:]:
            per_eng[op.eng].append(op)

        def run_engine(ename, eobj):
            waited = self.waited[ename]
            for op in per_eng[ename]:
                need = {}
                for d in op.deps:
                    s, v = ops[d].event
                    key = id(s)
                    if waited.get(key, 0) >= v:
                        continue
                    if key not in need or need[key][1] < v:
                        need[key] = (s, v)
                for key, (s, v) in need.items():
                    eobj.wait_ge(s, v)
                    waited[key] = v
                    self.stats[ename][1] += 1
                ins = op.fn(eobj)
                self.stats[ename][0] += 1
                if op.event is not None:
                    ins.then_inc(op.event[0], 16 if op.is_dma else 1)
                op.fn = None

        with nc.Block() as block:
            @block.sync
            def _(e):
                run_engine("sp", e)

            @block.scalar
            def _(e):
                run_engine("act", e)

            @block.vector
            def _(e):
                run_engine("dve", e)

            @block.gpsimd
            def _(e):
                run_engine("pool", e)

            @block.tensor
            def _(e):
                run_engine("pe", e)
        self.done = len(ops)
        self.n_sems = len(self._sems)
        for b in self.live_dbufs:
            self.free_dsems.append((b.dsem, b.dcount))
            b.dsem = None
        self.live_dbufs = []
        return self.stats

    def close(self):
        self.es.close()


class Ring:
    def __init__(self, S, name, n, shape, dtype, psum=False):
        self.slots = []
        for i in range(n):
            self.slots.append((S.psum if psum else S.sbuf)("%s%d" % (name, i), shape, dtype))
        self.i = 0

    def get(self):
        s = self.slots[self.i % len(self.slots)]
        self.i += 1
        return s


class Cfg:
    def __init__(self, S=4096, E=64, TOPK=8, IDX_TOPK=256, NSEQ=1, DEPTH=2):
        self.S = S
        self.NB = S // 128
        self.D = 2048
        self.H = 16
        self.E = E
        self.TOPK = TOPK
        self.DE = 512
        self.IDX_TOPK = min(IDX_TOPK, S // 4)
        self.NSEQ = NSEQ
        self.DEPTH = DEPTH
        self.ALPHA = (2 * DEPTH) ** 0.25
        self.QL = 512
        self.KVL = 256
        self.IH = 16
        self.ID = 64
        self.BW = 512 + 256 + 64 + 16


def t5_bucket_np(rel):
    half = 16
    max_exact = 8
    n = np.abs(rel)
    large = max_exact + (np.log(np.maximum(n, 1).astype(np.float32) / max_exact)
                         / math.log(128 / max_exact) * (half - max_exact)).astype(np.int32)
    large = np.minimum(large, half - 1)
    return np.where(rel > 0, half, 0) + np.where(n < max_exact, n, large)


def build_program(cfg, debug_outs=()):
    nc = bass.Bass("TRN2", target_bir_lowering=False)
    S = Sched(nc)
    c = cfg
    S.limit = getattr(cfg, "LIMIT", 10 ** 9)
    NB, D, H, E, SL = c.NB, c.D, c.H, c.E, c.S
    DC = D // 128
    NG = SL // 512

    def din(name, shape, dt=F32):
        return nc.dram_tensor(name, list(shape), dt, kind="ExternalInput").ap()

    def dscr(name, shape, dt):
        return nc.dram_tensor(name, list(shape), dt, kind="Internal").ap()

    x_in = din("x", [c.NSEQ, SL, D])
    a_w_in = din("a_w_in", [D, 3 * D + H])
    a_b_f = din("a_b_f", [1, H])
    a_w_out = din("a_w_out", [D, D])
    b_w_in = din("b_w_in", [D, c.BW])
    b_q_norm = din("b_q_norm", [1, c.QL])
    b_kv_norm = din("b_kv_norm", [1, c.KVL])
    b_w_uq = din("b_w_uq", [c.QL, D])
    b_w_iq = din("b_w_iq", [c.QL, c.IH * c.ID])
    b_w_uk = din("b_w_uk", [H, c.KVL, 128])
    b_w_uv = din("b_w_uv", [H, c.KVL, 128])
    b_w_out = din("b_w_out", [D, D])
    bt_in = din("bias_tiles", [2, H, 128, 128])
    b15_in = din("bias_far", [1, H])
    ln1_g = din("ln1_g", [c.DEPTH, D])
    ln1_b = din("ln1_b", [c.DEPTH, D])
    ln2_g = din("ln2_g", [c.DEPTH, D])
    ln2_b = din("ln2_b", [c.DEPTH, D])
    router_w = din("router_w", [c.DEPTH, D, E])
    router_b = din("router_b", [c.DEPTH, E])
    w_gate = din("w_gate", [c.DEPTH, E, D, c.DE])
    w_up = din("w_up", [c.DEPTH, E, D, c.DE])
    w_down = din("w_down", [c.DEPTH, E, c.DE, D])
    sh_gate = din("sh_gate", [c.DEPTH, D, c.DE])
    sh_up = din("sh_up", [c.DEPTH, D, c.DE])
    sh_down = din("sh_down", [c.DEPTH, c.DE, D])
    consts = din("consts", [5, 128, 128])
    out = nc.dram_tensor("out", [c.NSEQ, SL, D], F32, kind="ExternalOutput").ap()
    dbg = {}
    for nm, shp in debug_outs:
        dbg[nm] = nc.dram_tensor("dbg_" + nm, list(shp), F32, kind="ExternalOutput").ap()

    xT_d = dscr("xT_d", [DC, 128, SL], BF16)
    qT_d = dscr("qT_d", [H, 128, SL], BF16)
    kT_d = dscr("kT_d", [H, 128, SL], BF16)
    v_d = dscr("v_d", [SL, D], BF16)
    o_d = dscr("o_d", [SL, D], BF16)
    x1_d = dscr("x1_d", [SL, D], F32)
    x1T_d = dscr("x1T_d", [DC, 128, SL], BF16)
    x2_d = dscr("x2_d", [SL, D], F32)
    cqT_d = dscr("cqT_d", [4, 128, SL], BF16)
    ckvT_d = dscr("ckvT_d", [2, 128, SL], BF16)
    kiT_d = dscr("kiT_d", [64, SL], BF16)
    qiT_d = dscr("qiT_d", [c.IH, 64, SL], BF16)
    mT_d = dscr("mT_d", [NB, NB, 128, 128], BF16)

    identf, identf_b = S.sbuf("identf", [128, 128], F32)
    identb, identb_b = S.sbuf("identb", [128, 128], BF16)
    trif, trif_b = S.sbuf("trif", [128, 128], F32)
    onesf, onesf_b = S.sbuf("onesf", [128, 128], F32)
    causb, causb_b = S.sbuf("causb", [128, 128], BF16)
    chneg, chneg_b = S.sbuf("chneg", [128, 128], F32)
    epsb, epsb_b = S.sbuf("epsb", [128, 4], F32)
    logf, logf_b = S.sbuf("logf", [128, NB, H], F32)
    Fs, Fs_b = S.sbuf("Fs", [128, NB, H], F32)
    Fend, Fend_b = S.sbuf("Fend", [128, NB + 1, H], F32)
    G_all, G_b = S.sbuf("G_all", [128, NB, E + 1], F32)
    wI, wI_b = S.sbuf("wI", [128, NB, c.IH], F32)
    bfb, bfb_b = S.sbuf("bfb", [128, H], F32)
    b15b, b15b_b = S.sbuf("b15b", [128, H], F32)
    lng, lng_b = S.sbuf("lng", [128, D], F32)
    lnb, lnb_b = S.sbuf("lnb", [128, D], F32)
    small = Ring(S, "small", 6, [128, 64], F32)
    stats = Ring(S, "stats", 2, [128, 4, 6], F32)
    obf = Ring(S, "obf", 3, [128, 512], BF16)

    mmR = Ring(S, "pmm", 4, [128, 512], F32, psum=True)
    tpfR = Ring(S, "ptf", 1, [128, 512], F32, psum=True)
    tpbR = Ring(S, "ptb", 1, [128, 1024], BF16, psum=True)
    poR = Ring(S, "ppo", 2, [128, 512], F32, psum=True)

    S.begin_phase()
    S.dma("sp", identf[:], consts[0], writes=[identf_b], sem_buf=identf_b)
    S.dma("pool", identb[:], consts[0], writes=[identb_b], sem_buf=identb_b)
    S.dma("sp", trif[:], consts[1], writes=[trif_b], sem_buf=trif_b)
    S.dma("sp", onesf[:], consts[2], writes=[onesf_b], sem_buf=onesf_b)
    S.dma("pool", causb[:], consts[3], writes=[causb_b], sem_buf=causb_b)
    S.dma("sp", chneg[:], consts[4], writes=[chneg_b], sem_buf=chneg_b)
    S.dma("sp", bfb[:], bass.AP(a_b_f.tensor, a_b_f.offset, [[0, 128], [1, H]]), writes=[bfb_b], sem_buf=bfb_b)
    S.dma("sp", b15b[:], bass.AP(b15_in.tensor, b15_in.offset, [[0, 128], [1, H]]), writes=[b15b_b], sem_buf=b15b_b)
    S.op("dve", lambda e: e.memset(epsb[:, 0:1], 1e-5), writes=[epsb_b])
    S.op("dve", lambda e: e.memset(epsb[:, 1:2], 1e-6), pwrites=[epsb_b])
    S.op("dve", lambda e: e.memset(epsb[:, 2:3], 1.0), pwrites=[epsb_b])
    S.op("dve", lambda e: e.memset(epsb[:, 3:4], 0.0), pwrites=[epsb_b])
    S.end_phase()

    rr = {"i": 0}

    def bcast_rows(ap_row, n):
        return bass.AP(ap_row.tensor, ap_row.offset, [[0, 128], [1, n]])

    def evac_eng():
        return "dve"

    def copy(eng, o, ob, i, ib, partial=False):
        w = dict(pwrites=[ob]) if partial else dict(writes=[ob])
        if eng == "act":
            S.op("act", lambda e: e.copy(o, i), reads=[ib], **w)
        else:
            S.op(eng, lambda e: e.tensor_copy(o, i), reads=[ib], **w)

    def mm_chain(ps, psb, pairs, reads):
        n = len(pairs)
        for k, (l, r) in enumerate(pairs):
            S.op("pe", lambda e, l=l, r=r, k=k: e.matmul(ps, l, r, start=(k == 0), stop=(k == n - 1)),
                 reads=reads, writes=[psb] if k == 0 else (), pwrites=[psb] if k > 0 else ())

    def transpose_cols(src, srcb, ncols, dst_fn, dstb, dt, first_full=True):
        nchunk = ncols // 128
        per = 4 if dt == F32 else 8
        first = first_full
        for k0 in range(0, nchunk, per):
            kn = min(per, nchunk - k0)
            pt, ptb = (tpfR if dt == F32 else tpbR).get()
            idt, idb = (identf, identf_b) if dt == F32 else (identb, identb_b)
            for k in range(kn):
                S.op("pe", lambda e, k=k, k0=k0, pt=pt, idt=idt: e.transpose(
                    pt[:, k * 128:(k + 1) * 128], src[:, (k0 + k) * 128:(k0 + k + 1) * 128], idt[:]),
                    reads=[srcb, idb], writes=[ptb] if k == 0 else (), pwrites=[ptb] if k > 0 else ())
            for k in range(kn):
                copy(evac_eng(), dst_fn(k0 + k), dstb, pt[:, k * 128:(k + 1) * 128], ptb, partial=not first)
                first = False

    def layer_norm_rows(y, yb):
        st, stb = stats.get()
        for k in range(4):
            S.op("dve", lambda e, k=k: e.bn_stats(st[:, k, :], y[:, k * 512:(k + 1) * 512]),
                 reads=[yb], writes=[stb] if k == 0 else (), pwrites=[stb] if k else ())
        sm, smb = small.get()
        S.op("dve", lambda e: e.bn_aggr(sm[:, 0:2], st[:].rearrange("p a b -> p (a b)")), reads=[stb], writes=[smb])
        S.op("act", lambda e: e.activation(sm[:, 2:3], sm[:, 1:2], AF.Sqrt, bias=epsb[:, 0:1], scale=1.0),
             reads=[smb, epsb_b], pwrites=[smb])
        S.op("dve", lambda e: e.reciprocal(sm[:, 3:4], sm[:, 2:3]), reads=[smb], pwrites=[smb])
        S.op("dve", lambda e: e.tensor_scalar(y[:], y[:], sm[:, 0:1], sm[:, 3:4], ALU.subtract, ALU.mult),
             reads=[smb, yb], writes=[yb])
        S.op("pool", lambda e: e.tensor_tensor(y[:], y[:], lng[:], ALU.mult), reads=[yb, lng_b], writes=[yb])
        S.op("pool", lambda e: e.tensor_tensor(y[:], y[:], lnb[:], ALU.add), reads=[yb, lnb_b], writes=[yb])

    def load_ln(gsrc, bsrc, li):
        S.dma("sp", lng[:], bcast_rows(gsrc[li:li + 1, :], D), writes=[lng_b], sem_buf=lng_b)
        S.dma("sp", lnb[:], bcast_rows(bsrc[li:li + 1, :], D), writes=[lnb_b], sem_buf=lnb_b)

    def phase_transpose_in(src_d, src_b, dstT_d, dstT_b):
        S.begin_phase()
        xrow = Ring(S, "xrow", 2, [128, D], F32)
        xTblk = Ring(S, "xTblk", 2, [128, DC, 128], BF16)
        for b in range(NB):
            xr, xrb = xrow.get()
            S.dma("sp", xr[:], src_d[b * 128:(b + 1) * 128, :], reads=[src_b], writes=[xrb], sem_buf=xrb)
            xt, xtb = xTblk.get()
            import os
            if os.environ.get("DBG_P2", "") == "load":
                continue
            transpose_cols(xr, xrb, D, lambda k, xt=xt: xt[:, k, :], xtb, F32)
            v = os.environ.get("DBG_ST", "pool")
            if v != "none":
                S.dma(v, dstT_d[:, :, b * 128:(b + 1) * 128].rearrange("c p t -> p c t"), xt[:],
                      reads=[xtb], pwrites=[dstT_b], sem_buf=xtb)
        S.end_phase()

    def mk_load_w(wt):
        def load_w_tile(wsrc, col0, ncol, nk=DC):
            w, wb = wt.get()
            S.dma("pool", w[:, 0:nk, 0:ncol], wsrc[:, col0:col0 + ncol].rearrange("(c p) n -> p c n", p=128),
                  writes=[wb], sem_buf=wb)
            return w, wb
        return load_w_tile

    def proj_fm(xg, xgb, w, wb, nk, mw, nm, dst_fn, dst_b, k0=0):
        for m in range(nm):
            ps, psb = mmR.get()
            mm_chain(ps[0:mw, :], psb, [(w[:, k, m * mw:(m + 1) * mw], xg[:, k0 + k, :]) for k in range(nk)], [xgb, wb])
            ob_, obb = obf.get()
            copy(evac_eng(), ob_[0:mw, :], obb, ps[0:mw, :], psb)
            S.dma("sp", dst_fn(m), ob_[0:mw, :], reads=[obb], pwrites=[dst_b], sem_buf=obb)

    def proj_tm(xg, xgb, w, wb, nk, ncol, tb, dst_ap, dst_b, k0=0):
        ps, psb = mmR.get()
        mm_chain(ps[:, 0:ncol], psb, [(xg[:, k0 + k, tb * 128:(tb + 1) * 128], w[:, k, 0:ncol]) for k in range(nk)], [xgb, wb])
        ob_, obb = obf.get()
        copy(evac_eng(), ob_[:, 0:ncol], obb, ps[:, 0:ncol], psb)
        S.dma("sp", dst_ap, ob_[:, 0:ncol], reads=[obb], pwrites=[dst_b], sem_buf=obb)

    def fox_projections(xT_b, qT_b, kT_b, v_b):
        S.begin_phase()
        xTg = Ring(S, "xTg", 2, [128, DC, 512], BF16)
        wt = Ring(S, "wt", 3, [128, DC, 512], BF16)
        load_w_tile = mk_load_w(wt)
        for g in range(NG):
            xg, xgb = xTg.get()
            S.dma("sp", xg[:], xT_d[:, :, g * 512:(g + 1) * 512].rearrange("c p t -> p c t"),
                  reads=[xT_b], writes=[xgb], sem_buf=xgb)
            for ct in range(8):
                w, wb = load_w_tile(a_w_in, ct * 512, 512)
                dstT, dstb = (qT_d, qT_b) if ct < 4 else (kT_d, kT_b)
                h0 = (ct % 4) * 4
                proj_fm(xg, xgb, w, wb, DC, 128, 4,
                        lambda m, dstT=dstT, h0=h0, g=g: dstT[h0 + m, :, g * 512:(g + 1) * 512], dstb)
            for ct in range(8, 12):
                w, wb = load_w_tile(a_w_in, ct * 512, 512)
                for tb in range(4):
                    r0 = g * 512 + tb * 128
                    proj_tm(xg, xgb, w, wb, DC, 512, tb, v_d[r0:r0 + 128, (ct - 8) * 512:(ct - 7) * 512], v_b)
            w, wb = load_w_tile(a_w_in, 3 * D, H)
            for tb in range(4):
                blk = g * 4 + tb
                ps, psb = poR.get()
                mm_chain(ps[:, 0:H], psb, [(xg[:, k, tb * 128:(tb + 1) * 128], w[:, k, 0:H]) for k in range(DC)], [xgb, wb])
                sm, smb = small.get()
                S.op("dve", lambda e, sm=sm, ps=ps: e.tensor_tensor(sm[:, 0:16], ps[:, 0:H], bfb[:], ALU.add),
                     reads=[psb, bfb_b], writes=[smb])
                S.op("act", lambda e, sm=sm: e.activation(sm[:, 16:32], sm[:, 0:16], AF.Abs),
                     reads=[smb], pwrites=[smb])
                S.op("act", lambda e, sm=sm: e.activation(sm[:, 32:48], sm[:, 16:32], AF.Exp, scale=-1.0),
                     reads=[smb], pwrites=[smb])
                S.op("act", lambda e, sm=sm: e.activation(sm[:, 32:48], sm[:, 32:48], AF.Ln, bias=epsb[:, 2:3], scale=1.0),
                     reads=[smb, epsb_b], pwrites=[smb])
                S.op("dve", lambda e, sm=sm: e.tensor_scalar_min(sm[:, 48:64], sm[:, 0:16], 0.0), reads=[smb], pwrites=[smb])
                S.op("dve", lambda e, sm=sm, blk=blk: e.tensor_tensor(logf[:, blk, :], sm[:, 48:64], sm[:, 32:48], ALU.subtract),
                     reads=[smb], pwrites=[logf_b])
        S.op("dve", lambda e: e.memset(Fend[:, 0, :], 0.0), reads=[logf_b], writes=[Fend_b])
        for b in range(NB):
            ps, psb = poR.get()
            S.op("pe", lambda e, ps=ps, b=b: e.matmul(ps[:, 0:H], trif[:], logf[:, b, :], start=True, stop=True),
                 reads=[trif_b, logf_b], writes=[psb])
            S.op("pe", lambda e, ps=ps, b=b: e.matmul(ps[:, 32:32 + H], onesf[:], logf[:, b, :], start=True, stop=True),
                 reads=[onesf_b, logf_b], pwrites=[psb])
            S.op("dve", lambda e, ps=ps, b=b: e.tensor_tensor(Fs[:, b, :], ps[:, 0:H], Fend[:, b, :], ALU.add),
                 reads=[psb, Fend_b], pwrites=[Fs_b])
            S.op("dve", lambda e, ps=ps, b=b: e.tensor_tensor(Fend[:, b + 1, :], ps[:, 32:32 + H], Fend[:, b, :], ALU.add),
                 reads=[psb], pwrites=[Fend_b])
        S.end_phase()

    def attention(mode, qT_b, kT_b, v_b, o_b, mT_b=None):
        S.begin_phase()
        scale = 128 ** -0.5
        kTh = Ring(S, "kTh", 2, [128, SL], BF16)
        qTh = Ring(S, "qTh", 2, [128, SL], BF16)
        vh = Ring(S, "vh", 2, [128, NB, 132], BF16)
        pbf = Ring(S, "pbf", 4, [128, 128], BF16)
        biasij = Ring(S, "biasij", 4, [128, H], F32)
        if mode == "dsa":
            pf32 = Ring(S, "pf32", 2, [128, 128], F32)
            mrow = Ring(S, "mrow", 2, [128, NB, 128], BF16)
            btile, btile_b = S.sbuf("btile", [128, 2, H, 128], F32)
            S.dma("sp", btile[:], bt_in.rearrange("o h s t -> s o h t"), writes=[btile_b], sem_buf=btile_b)
        for h in range(H):
            kt, ktb = kTh.get()
            qt, qtb = qTh.get()
            vv, vvb = vh.get()
            S.dma("sp", kt[:], kT_d[h], reads=[kT_b], writes=[ktb], sem_buf=ktb)
            S.dma("sp", qt[:], qT_d[h], reads=[qT_b], writes=[qtb], sem_buf=qtb)
            S.dma("sp", vv[:, :, 0:128], v_d[:, h * 128:(h + 1) * 128].rearrange("(b p) d -> p b d", p=128),
                  reads=[v_b], writes=[vvb], sem_buf=vvb)
            S.op("pool", lambda e, vv=vv: e.memset(vv[:, :, 128:129], 1.0), reads=[vvb], pwrites=[vvb])
            for i in range(NB):
                po, pob = poR.get()
                if mode == "dsa":
                    mr, mrb = mrow.get()
                    S.dma("sp", mr[:, 0:i + 1, :], mT_d[i, 0:i + 1].rearrange("j s t -> s j t"),
                          reads=[mT_b], writes=[mrb], sem_buf=mrb)
                for j in range(i + 1):
                    ps, psb = mmR.get()
                    S.op("pe", lambda e, ps=ps, kt=kt, qt=qt, i=i, j=j: e.matmul(
                        ps[:, 0:128], kt[:, j * 128:(j + 1) * 128], qt[:, i * 128:(i + 1) * 128], start=True, stop=True),
                        reads=[ktb, qtb], writes=[psb])
                    p, pb = pbf.get()
                    if mode == "fox":
                        bi, bib = biasij.get()
                        S.op("dve", lambda e, bi=bi, i=i, j=j: e.tensor_tensor(bi[:], Fend[:, i + 1, :], Fs[:, j, :], ALU.subtract),
                             reads=[Fend_b, Fs_b], writes=[bib])
                        S.op("act", lambda e, p=p, ps=ps, bi=bi, h=h: e.activation(p[:], ps[:, 0:128], AF.Exp,
                                                                                   bias=bi[:, h:h + 1], scale=scale),
                             reads=[psb, bib], writes=[pb])
                        if j == i:
                            S.op("dve", lambda e, p=p: e.tensor_tensor(p[:], p[:], causb[:], ALU.mult),
                                 reads=[pb, causb_b], writes=[pb])
                    else:
                        if j >= i - 1:
                            tf, tfb = pf32.get()
                            S.op("dve", lambda e, tf=tf, ps=ps, i=i, j=j, h=h: e.scalar_tensor_tensor(
                                tf[:], ps[:, 0:128], scale, btile[:, i - j, h, :], ALU.mult, ALU.add),
                                reads=[psb, btile_b], writes=[tfb])
                            S.op("act", lambda e, p=p, tf=tf: e.activation(p[:], tf[:], AF.Exp), reads=[tfb], writes=[pb])
                        else:
                            S.op("act", lambda e, p=p, ps=ps, h=h: e.activation(p[:], ps[:, 0:128], AF.Exp,
                                                                                bias=b15b[:, h:h + 1], scale=scale),
                                 reads=[psb, b15b_b], writes=[pb])
                        S.op("pool", lambda e, p=p, mr=mr, j=j: e.tensor_tensor(p[:], p[:], mr[:, j, :], ALU.mult),
                             reads=[pb, mrb], writes=[pb])
                    S.op("pe", lambda e, po=po, p=p, vv=vv, i=i, j=j: e.matmul(po[:, 0:129], p[:], vv[:, j, 0:129],
                                                                              start=(j == 0), stop=(j == i)),
                         reads=[pb, vvb], writes=[pob] if j == 0 else (), pwrites=[pob] if j > 0 else ())
                sm, smb = small.get()
                S.op("dve", lambda e, sm=sm, po=po: e.reciprocal(sm[:, 0:1], po[:, 128:129]), reads=[pob], writes=[smb])
                ob_, obb = obf.get()
                S.op("dve", lambda e, ob_=ob_, po=po, sm=sm: e.tensor_scalar(ob_[:, 0:128], po[:, 0:128], sm[:, 0:1], None, ALU.mult),
                     reads=[pob, smb], writes=[obb])
                S.dma("sp", o_d[i * 128:(i + 1) * 128, h * 128:(h + 1) * 128], ob_[:, 0:128],
                      reads=[obb], pwrites=[o_b], sem_buf=obb)
        S.end_phase()

    def outproj_ln_router(li, w_out_ap, xres_d, xres_b, o_b, x1_b, x1T_b):
        S.begin_phase()
        wt = Ring(S, "wt", 3, [128, DC, 512], BF16)
        load_w_tile = mk_load_w(wt)
        obfw = Ring(S, "obfw", 2, [128, D], BF16)
        xTblk = Ring(S, "xTblk", 2, [128, DC, 128], BF16)
        xTf32 = Ring(S, "xTf32", 1, [128, DC, 128], F32)
        xrow = Ring(S, "xrow", 2, [128, D], F32)
        yrow = Ring(S, "yrow", 2, [128, D], F32)
        smallw = Ring(S, "smallw", 2, [128, 2 * E + 16], F32)
        rwt, rwt_b = S.sbuf("rwt", [128, DC, E], F32)
        rbb, rbb_b = S.sbuf("rbb", [128, E], F32)
        load_ln(ln1_g, ln1_b, li)
        S.dma("sp", rwt[:], router_w[li].rearrange("(c p) n -> p c n", p=128), writes=[rwt_b], sem_buf=rwt_b)
        S.dma("sp", rbb[:], bcast_rows(router_b[li:li + 1, :], E), writes=[rbb_b], sem_buf=rbb_b)
        for b in range(NB):
            r0 = b * 128
            orow, orowb = obfw.get()
            S.dma("sp", orow[:], o_d[r0:r0 + 128, :], reads=[o_b], writes=[orowb], sem_buf=orowb)
            oT, oTb = xTblk.get()
            transpose_cols(orow, orowb, D, lambda k, oT=oT: oT[:, k, :], oTb, BF16)
            xr, xrb = xrow.get()
            S.dma("sp", xr[:], xres_d[r0:r0 + 128, :], reads=[xres_b], writes=[xrb], sem_buf=xrb)
            y, yb = yrow.get()
            for n in range(4):
                w, wb = load_w_tile(w_out_ap, n * 512, 512)
                ps, psb = mmR.get()
                mm_chain(ps[:], psb, [(oT[:, k, :], w[:, k, :]) for k in range(DC)], [oTb, wb])
                S.op("dve", lambda e, y=y, xr=xr, ps=ps, n=n: e.scalar_tensor_tensor(
                    y[:, n * 512:(n + 1) * 512], xr[:, n * 512:(n + 1) * 512], c.ALPHA, ps[:], ALU.mult, ALU.add),
                    reads=[xrb, psb], writes=[yb] if n == 0 else (), pwrites=[yb] if n else ())
            layer_norm_rows(y, yb)
            S.dma("sp", x1_d[r0:r0 + 128, :], y[:], reads=[yb], pwrites=[x1_b], sem_buf=yb)
            xTf, xTfb = xTf32.get()
            transpose_cols(y, yb, D, lambda k, xTf=xTf: xTf[:, k, :], xTfb, F32)
            xt, xtb = xTblk.get()
            S.op("pool", lambda e, xt=xt, xTf=xTf: e.tensor_copy(xt[:], xTf[:]), reads=[xTfb], writes=[xtb])
            S.dma("pool", x1T_d[:, :, r0:r0 + 128].rearrange("c p t -> p c t"), xt[:], reads=[xtb], pwrites=[x1T_b], sem_buf=xtb)
            ps, psb = poR.get()
            mm_chain(ps[:, 0:E], psb, [(xTf[:, k, :], rwt[:, k, :]) for k in range(DC)], [xTfb, rwt_b])
            sm, smb = smallw.get()
            S.op("act", lambda e, sm=sm, ps=ps: e.activation(sm[:, 0:E], ps[:, 0:E], AF.Sigmoid), reads=[psb], writes=[smb])
            S.op("dve", lambda e, sm=sm: e.tensor_tensor(sm[:, E:2 * E], sm[:, 0:E], rbb[:], ALU.add), reads=[smb, rbb_b], pwrites=[smb])
            S.op("dve", lambda e, sm=sm: e.max(sm[:, 2 * E:2 * E + 8], sm[:, E:2 * E]), reads=[smb], pwrites=[smb])
            assert c.TOPK == 8
            S.op("dve", lambda e, sm=sm: e.tensor_tensor(sm[:, 2 * E + 10:2 * E + 11], sm[:, 2 * E:2 * E + 1], sm[:, 2 * E + 7:2 * E + 8], ALU.min),
                 reads=[smb], pwrites=[smb])
            S.op("dve", lambda e, sm=sm: e.tensor_scalar(sm[:, E:2 * E], sm[:, E:2 * E], sm[:, 2 * E + 10:2 * E + 11], None, ALU.is_ge),
                 reads=[smb], pwrites=[smb])
            S.op("dve", lambda e, sm=sm: e.tensor_tensor(sm[:, 0:E], sm[:, 0:E], sm[:, E:2 * E], ALU.mult), reads=[smb], pwrites=[smb])
            S.op("dve", lambda e, sm=sm: e.reduce_sum(sm[:, 2 * E + 8:2 * E + 9], sm[:, 0:E], axis=AX.X), reads=[smb], pwrites=[smb])
            S.op("dve", lambda e, sm=sm: e.reciprocal(sm[:, 2 * E + 9:2 * E + 10], sm[:, 2 * E + 8:2 * E + 9]), reads=[smb], pwrites=[smb])
            S.op("dve", lambda e, sm=sm: e.tensor_scalar(sm[:, 0:E], sm[:, 0:E], sm[:, 2 * E + 9:2 * E + 10], None, ALU.mult),
                 reads=[smb], pwrites=[smb])
            S.op("dve", lambda e, sm=sm, b=b: e.tensor_scalar(G_all[:, b, 0:E], sm[:, 0:E], 2.5, None, ALU.mult),
                 reads=[smb], pwrites=[G_b])
            S.op("dve", lambda e, b=b: e.memset(G_all[:, b, E:E + 1], 1.0), pwrites=[G_b])
        import os
        if os.environ.get("DBG_G"):
            S.dma("sp", out[0][0:128, 0:NB * (E + 1)], G_all[:].rearrange("p b e -> p (b e)"), reads=[G_b], pwrites=[out_b], sem_buf=G_b)
        S.end_phase()

    def moe_and_ln2(li, x1_b, x1T_b, dst_d, dst_b):
        S.begin_phase()
        xTg = Ring(S, "xTg", 1, [128, DC, 512], BF16)
        wt = Ring(S, "wt", 3, [128, DC, 512], BF16)
        load_w_tile = mk_load_w(wt)
        wdn = Ring(S, "wdn", 1, [128, 4, D], BF16)
        hT = Ring(S, "hT", 2, [128, 4, 512], BF16)
        sg = Ring(S, "sg", 2, [128, 512], F32)
        acc, acc_b = S.sbuf("acc", [128, 4, D], F32)
        xrow = Ring(S, "xrow", 2, [128, D], F32)
        load_ln(ln2_g, ln2_b, li)
        for g in range(NG):
            xg, xgb = xTg.get()
            S.dma("sp", xg[:], x1T_d[:, :, g * 512:(g + 1) * 512].rearrange("c p t -> p c t"),
                  reads=[x1T_b], writes=[xgb], sem_buf=xgb)
            for ei in range(E + 1):
                if ei < E:
                    wgs, wus, wds = w_gate[li, ei], w_up[li, ei], w_down[li, ei]
                else:
                    wgs, wus, wds = sh_gate[li], sh_up[li], sh_down[li]
                wg, wgb = load_w_tile(wgs, 0, 512)
                wu, wub = load_w_tile(wus, 0, 512)
                wd, wdb = wdn.get()
                S.dma("pool", wd[:], wds.rearrange("(c p) n -> p c n", p=128), writes=[wdb], sem_buf=wdb)
                ht, htb = hT.get()
                for m in range(4):
                    psg, psgb = mmR.get()
                    mm_chain(psg[:], psgb, [(wg[:, k, m * 128:(m + 1) * 128], xg[:, k, :]) for k in range(DC)], [xgb, wgb])
                    psu, psub = mmR.get()
                    mm_chain(psu[:], psub, [(wu[:, k, m * 128:(m + 1) * 128], xg[:, k, :]) for k in range(DC)], [xgb, wub])
                    s_, sb_ = sg.get()
                    S.op("act", lambda e, s_=s_, psg=psg: e.activation(s_[:], psg[:], AF.Silu), reads=[psgb], writes=[sb_])
                    S.op("dve", lambda e, ht=ht, s_=s_, psu=psu, m=m: e.tensor_tensor(ht[:, m, :], s_[:], psu[:], ALU.mult),
                         reads=[sb_, psub], writes=[htb] if m == 0 else (), pwrites=[htb] if m else ())
                for tb in range(4):
                    blk = g * 4 + tb
                    for n in range(4):
                        ps, psb = mmR.get()
                        mm_chain(ps[:], psb, [(ht[:, m, tb * 128:(tb + 1) * 128], wd[:, m, n * 512:(n + 1) * 512]) for m in range(4)],
                                 [htb, wdb])
                        a = acc[:, tb, n * 512:(n + 1) * 512]
                        if ei == 0:
                            S.op("dve", lambda e, a=a, ps=ps, blk=blk, ei=ei: e.tensor_scalar(a, ps[:], G_all[:, blk, ei:ei + 1], None, ALU.mult),
                                 reads=[psb, G_b], pwrites=[acc_b])
                        else:
                            S.op("dve", lambda e, a=a, ps=ps, blk=blk, ei=ei: e.scalar_tensor_tensor(
                                a, ps[:], G_all[:, blk, ei:ei + 1], a, ALU.mult, ALU.add),
                                reads=[psb, G_b, acc_b], pwrites=[acc_b])
            for tb in range(4):
                r0 = g * 512 + tb * 128
                xr, xrb = xrow.get()
                S.dma("sp", xr[:], x1_d[r0:r0 + 128, :], reads=[x1_b], writes=[xrb], sem_buf=xrb)
                S.op("dve", lambda e, xr=xr, tb=tb: e.scalar_tensor_tensor(xr[:], xr[:], c.ALPHA, acc[:, tb, :], ALU.mult, ALU.add),
                     reads=[xrb, acc_b], writes=[xrb])
                layer_norm_rows(xr, xrb)
                S.dma("sp", dst_d[r0:r0 + 128, :], xr[:], reads=[xrb], pwrites=[dst_b], sem_buf=xrb)
        S.end_phase()

    def dsa_projections(xT_b, qT_b, kT_b, v_b, kiT_b, qiT_b):
        cqT_b, ckvT_b = Buf("cqT"), Buf("ckvT")
        aux = {}
        S.begin_phase()
        xTg = Ring(S, "xTg", 2, [128, DC, 512], BF16)
        wt = Ring(S, "wt", 3, [128, DC, 512], BF16)
        load_w_tile = mk_load_w(wt)
        of32 = Ring(S, "of32", 4, [128, 512], F32)
        sg = Ring(S, "sg", 2, [128, 512], F32)
        xTblk = Ring(S, "xTblk", 2, [128, 8, 128], BF16)
        qnb, qnb_b = S.sbuf("qnb", [128, c.QL], F32)
        kvnb, kvnb_b = S.sbuf("kvnb", [128, c.KVL], F32)
        S.dma("sp", qnb[:], bcast_rows(b_q_norm, c.QL), writes=[qnb_b], sem_buf=qnb_b)
        S.dma("sp", kvnb[:], bcast_rows(b_kv_norm, c.KVL), writes=[kvnb_b], sem_buf=kvnb_b)
        for g in range(NG):
            xg, xgb = xTg.get()
            S.dma("sp", xg[:], xT_d[:, :, g * 512:(g + 1) * 512].rearrange("c p t -> p c t"),
                  reads=[xT_b], writes=[xgb], sem_buf=xgb)
            w0, w0b = load_w_tile(b_w_in, 0, 512)
            w1, w1b = load_w_tile(b_w_in, 512, c.BW - 512)
            for tb in range(4):
                blk = g * 4 + tb
                t0 = blk * 128
                ps0, ps0b = mmR.get()
                mm_chain(ps0[:], ps0b, [(xg[:, k, tb * 128:(tb + 1) * 128], w0[:, k, :]) for k in range(DC)], [xgb, w0b])
                ps1, ps1b = mmR.get()
                mm_chain(ps1[:, 0:336], ps1b, [(xg[:, k, tb * 128:(tb + 1) * 128], w1[:, k, 0:336]) for k in range(DC)], [xgb, w1b])
                cq, cqb = of32.get()
                ckv, ckvb = of32.get()
                sm, smb = small.get()
                sq, sqb = sg.get()
                S.op("act", lambda e, sq=sq, ps0=ps0, sm=sm: e.activation(sq[:], ps0[:], AF.Square, accum_out=sm[:, 0:1]),
                     reads=[ps0b], writes=[sqb, smb])
                S.op("act", lambda e, sq=sq, ps1=ps1, sm=sm: e.activation(sq[:, 0:256], ps1[:, 0:256], AF.Square, accum_out=sm[:, 1:2]),
                     reads=[ps1b], writes=[sqb], pwrites=[smb])
                S.op("act", lambda e, sm=sm: e.activation(sm[:, 2:3], sm[:, 0:1], AF.Sqrt, bias=epsb[:, 1:2], scale=1.0 / c.QL),
                     reads=[smb, epsb_b], pwrites=[smb])
                S.op("act", lambda e, sm=sm: e.activation(sm[:, 3:4], sm[:, 1:2], AF.Sqrt, bias=epsb[:, 1:2], scale=1.0 / c.KVL),
                     reads=[smb, epsb_b], pwrites=[smb])
                S.op("dve", lambda e, sm=sm: e.reciprocal(sm[:, 4:6], sm[:, 2:4]), reads=[smb], pwrites=[smb])
                S.op("dve", lambda e, cq=cq, ps0=ps0, sm=sm: e.scalar_tensor_tensor(cq[:], ps0[:], sm[:, 4:5], qnb[:], ALU.mult, ALU.mult),
                     reads=[ps0b, smb, qnb_b], writes=[cqb])
                S.op("dve", lambda e, ckv=ckv, ps1=ps1, sm=sm: e.scalar_tensor_tensor(ckv[:, 0:256], ps1[:, 0:256], sm[:, 5:6], kvnb[:], ALU.mult, ALU.mult),
                     reads=[ps1b, smb, kvnb_b], writes=[ckvb])
                S.op("dve", lambda e, ckv=ckv, ps1=ps1: e.tensor_copy(ckv[:, 256:320], ps1[:, 256:320]), reads=[ps1b], pwrites=[ckvb])
                S.op("dve", lambda e, ps1=ps1, blk=blk: e.tensor_scalar(wI[:, blk, :], ps1[:, 320:336], 0.25 * 0.125, None, ALU.mult),
                     reads=[ps1b], pwrites=[wI_b])
                tT, tTb = xTblk.get()
                transpose_cols(cq, cqb, 512, lambda k, tT=tT: tT[:, k, :], tTb, F32)
                transpose_cols(ckv, ckvb, 256, lambda k, tT=tT: tT[:, 4 + k, :], tTb, F32, first_full=False)
                pt, ptb = tpfR.get()
                S.op("pe", lambda e, pt=pt, ckv=ckv: e.transpose(pt[0:64, 0:128], ckv[:, 256:320], identf[:]),
                     reads=[ckvb, identf_b], writes=[ptb])
                copy("dve", tT[0:64, 6, :], tTb, pt[0:64, 0:128], ptb, partial=True)
                s1 = aux.setdefault((id(tTb), 1), Buf("s1"))
                s2 = aux.setdefault((id(tTb), 2), Buf("s2"))
                S.dma("pool", cqT_d[:, :, t0:t0 + 128].rearrange("c p t -> p c t"), tT[:, 0:4, :], reads=[tTb], pwrites=[cqT_b], sem_buf=tTb)
                S.dma("pool", ckvT_d[:, :, t0:t0 + 128].rearrange("c p t -> p c t"), tT[:, 4:6, :], reads=[tTb], pwrites=[ckvT_b], sem_buf=s1)
                S.dma("pool", kiT_d[:, t0:t0 + 128], tT[0:64, 6, :], reads=[tTb], pwrites=[kiT_b], sem_buf=s2)
        for g in range(NG):
            cg, cgb = xTg.get()
            S.dma("sp", cg[:, 0:4, :], cqT_d[:, :, g * 512:(g + 1) * 512].rearrange("c p t -> p c t"),
                  reads=[cqT_b], writes=[cgb], sem_buf=cgb)
            s3 = aux.setdefault((id(cgb), 3), Buf("s3"))
            S.dma("sp", cg[:, 4:6, :], ckvT_d[:, :, g * 512:(g + 1) * 512].rearrange("c p t -> p c t"),
                  reads=[ckvT_b], pwrites=[cgb], sem_buf=s3)
            for ct in range(4):
                w, wb = load_w_tile(b_w_uq, ct * 512, 512, nk=4)
                proj_fm(cg, cgb, w, wb, 4, 128, 4, lambda m, ct=ct, g=g: qT_d[ct * 4 + m, :, g * 512:(g + 1) * 512], qT_b)
            for ct in range(2):
                w, wb = load_w_tile(b_w_iq, ct * 512, 512, nk=4)
                proj_fm(cg, cgb, w, wb, 4, 64, 8, lambda m, ct=ct, g=g: qiT_d[ct * 8 + m, :, g * 512:(g + 1) * 512], qiT_b)
            for hq in range(4):
                w, wb = wt.get()
                for hh in range(4):
                    S.dma("pool", w[:, 0:2, hh * 128:(hh + 1) * 128], b_w_uk[hq * 4 + hh].rearrange("(k p) d -> p k d", p=128),
                          sem_buf=wb, **(dict(writes=[wb]) if hh == 0 else dict(pwrites=[wb])))
                proj_fm(cg, cgb, w, wb, 2, 128, 4, lambda m, hq=hq, g=g: kT_d[hq * 4 + m, :, g * 512:(g + 1) * 512], kT_b, k0=4)
                w2, w2b = wt.get()
                for hh in range(4):
                    S.dma("pool", w2[:, 0:2, hh * 128:(hh + 1) * 128], b_w_uv[hq * 4 + hh].rearrange("(k p) d -> p k d", p=128),
                          sem_buf=w2b, **(dict(writes=[w2b]) if hh == 0 else dict(pwrites=[w2b])))
                for tb in range(4):
                    r0 = g * 512 + tb * 128
                    proj_tm(cg, cgb, w2, w2b, 2, 512, tb, v_d[r0:r0 + 128, hq * 512:(hq + 1) * 512], v_b, k0=4)
        S.end_phase()

    def dsa_indexer(kiT_b, qiT_b, mT_b):
        S.begin_phase()
        qiR = Ring(S, "qiR", 2, [64, c.IH, 128], BF16)
        kiAll, kiAll_b = S.sbuf("kiAll", [64, SL], BF16)
        relu = Ring(S, "relu", 3, [128, 512], BF16)
        Irow, Irow_b = S.sbuf("Irow", [128, SL], F32)
        Iwork, Iwork_b = S.sbuf("Iwork", [128, SL], F32)
        Mrow, Mrow_b = S.sbuf("Mrow", [128, SL], BF16)
        m8 = Ring(S, "m8", 2, [128, 8], F32)
        mrow = Ring(S, "mrowi", 2, [128, NB, 128], BF16)
        S.dma("sp", kiAll[:], kiT_d, reads=[kiT_b], writes=[kiAll_b], sem_buf=kiAll_b)
        nrounds = c.IDX_TOPK // 8
        for i in range(NB):
            L = (i + 1) * 128
            qi, qib = qiR.get()
            S.dma("sp", qi[:], qiT_d[:, :, i * 128:(i + 1) * 128].rearrange("h d t -> d h t"), reads=[qiT_b], writes=[qib], sem_buf=qib)
            for s0 in range(0, L, 512):
                sn = min(512, L - s0)
                for hh in range(c.IH):
                    ps, psb = mmR.get()
                    S.op("pe", lambda e, ps=ps, qi=qi, hh=hh, s0=s0, sn=sn: e.matmul(ps[:, 0:sn], qi[:, hh, :], kiAll[:, s0:s0 + sn],
                                                                                      start=True, stop=True),
                         reads=[qib, kiAll_b], writes=[psb])
                    r, rb = relu.get()
                    S.op("act", lambda e, r=r, ps=ps, sn=sn: e.activation(r[:, 0:sn], ps[:, 0:sn], AF.Relu), reads=[psb], writes=[rb])
                    if hh == 0:
                        S.op("dve", lambda e, r=r, i=i, hh=hh, s0=s0, sn=sn: e.tensor_scalar(
                            Irow[:, s0:s0 + sn], r[:, 0:sn], wI[:, i, hh:hh + 1], None, ALU.mult),
                            reads=[rb, wI_b, Irow_b], writes=[Irow_b])
                    else:
                        S.op("dve", lambda e, r=r, i=i, hh=hh, s0=s0, sn=sn: e.scalar_tensor_tensor(
                            Irow[:, s0:s0 + sn], r[:, 0:sn], wI[:, i, hh:hh + 1], Irow[:, s0:s0 + sn], ALU.mult, ALU.add),
                            reads=[rb, wI_b, Irow_b], writes=[Irow_b])
            S.op("dve", lambda e, i=i: e.tensor_tensor(Irow[:, i * 128:(i + 1) * 128], Irow[:, i * 128:(i + 1) * 128], chneg[:], ALU.add),
                 reads=[Irow_b, chneg_b], writes=[Irow_b])
            S.op("dve", lambda e, L=L: e.tensor_copy(Iwork[:, 0:L], Irow[:, 0:L]), reads=[Irow_b], writes=[Iwork_b])
            mx, mxb = m8.get()
            for r_ in range(nrounds):
                S.op("dve", lambda e, mx=mx, L=L: e.max(mx[:], Iwork[:, 0:L]), reads=[Iwork_b], writes=[mxb])
                if r_ < nrounds - 1:
                    S.op("dve", lambda e, mx=mx, L=L: e.match_replace(Iwork[:, 0:L], mx[:], Iwork[:, 0:L], NEG),
                         reads=[mxb, Iwork_b], writes=[Iwork_b])
            sm, smb = small.get()
            S.op("dve", lambda e, sm=sm, mx=mx: e.tensor_tensor(sm[:, 1:2], mx[:, 0:1], mx[:, 7:8], ALU.min), reads=[mxb], writes=[smb])
            S.op("dve", lambda e, sm=sm: e.tensor_scalar_max(sm[:, 0:1], sm[:, 1:2], -1.0e29), reads=[smb], pwrites=[smb])
            S.op("dve", lambda e, sm=sm, L=L: e.tensor_scalar(Mrow[:, 0:L], Irow[:, 0:L], sm[:, 0:1], None, ALU.is_ge),
                 reads=[smb, Irow_b], writes=[Mrow_b])
            mt, mtb = mrow.get()
            transpose_cols(Mrow, Mrow_b, L, lambda k, mt=mt: mt[:, k, :], mtb, BF16)
            S.dma("pool", mT_d[i, 0:i + 1].rearrange("j s t -> s j t"), mt[:, 0:i + 1, :], reads=[mtb], pwrites=[mT_b], sem_buf=mtb)
        S.end_phase()

    out_b = Buf("out")
    for q in range(c.NSEQ):
        cur_d, cur_b = x_in[q], Buf("xin")
        run_depth = getattr(c, "RUN_DEPTH", c.DEPTH)
        for li in range(run_depth):
            xT_b, qT_b, kT_b, v_b, o_b = Buf("xT"), Buf("qT"), Buf("kT"), Buf("v"), Buf("o")
            x1_b, x1T_b, x2_b = Buf("x1"), Buf("x1T"), Buf("x2")
            phase_transpose_in(cur_d, cur_b, xT_d, xT_b)
            if li % 2 == 0:
                fox_projections(xT_b, qT_b, kT_b, v_b)
                attention("fox", qT_b, kT_b, v_b, o_b)
                w_out_ap = a_w_out
            else:
                kiT_b, qiT_b, mT_b = Buf("kiT"), Buf("qiT"), Buf("mT")
                dsa_projections(xT_b, qT_b, kT_b, v_b, kiT_b, qiT_b)
                dsa_indexer(kiT_b, qiT_b, mT_b)
                attention("dsa", qT_b, kT_b, v_b, o_b, mT_b)
                w_out_ap = b_w_out
            outproj_ln_router(li, w_out_ap, cur_d, cur_b, o_b, x1_b, x1T_b)
            if li == run_depth - 1:
                moe_and_ln2(li, x1_b, x1T_b, out[q], out_b)
            else:
                moe_and_ln2(li, x1_b, x1T_b, x2_d, x2_b)
                cur_d, cur_b = x2_d, x2_b
    S.begin_phase()
    S.op("sp", lambda e: e.nop(), reads=[out_b])
    S.end_phase()
    st = S.stats
    S.close()
    return nc, st


def make_consts():
    i = np.arange(128)
    ident = np.eye(128, dtype=np.float32)
    tri = (i[:, None] <= i[None, :]).astype(np.float32)
    ones = np.ones((128, 128), np.float32)
    caus = (i[:, None] <= i[None, :]).astype(np.float32)
    chn = np.where((i[None, :] // 64) <= (i[:, None] // 64), 0.0, NEG).astype(np.float32)
    return np.stack([ident, tri, ones, caus, chn]).astype(np.float32)


def bias_layout(rel_bias):
    s = np.arange(128)[:, None]
    t = np.arange(128)[None, :]
    tiles = []
    for off in (0, 1):
        rel = (s - off * 128) - t
        bk = t5_bucket_np(rel.astype(np.int32))
        tiles.append(np.transpose(rel_bias[bk], (2, 0, 1)))
    return np.ascontiguousarray(np.stack(tiles)).astype(np.float32), np.ascontiguousarray(rel_bias[15:16, :])


_CACHE = {}


def run_cfg(cfg, inputs, core_seqs):
    key = (cfg.S, cfg.E, cfg.TOPK, cfg.IDX_TOPK, cfg.NSEQ, getattr(cfg, 'RUN_DEPTH', 2), getattr(cfg, 'LIMIT', 0))
    if key not in _CACHE:
        _CACHE[key] = build_program(cfg)
    nc, st = _CACHE[key]
    bt, b15 = bias_layout(np.asarray(inputs["rel_bias"], np.float32))
    consts = make_consts()
    shared = {}
    for k in ("a_w_in", "a_b_f", "a_w_out", "b_w_in", "b_q_norm", "b_kv_norm", "b_w_uq", "b_w_iq", "b_w_uk",
              "b_w_uv", "b_w_out"):
        shared[k] = np.ascontiguousarray(np.asarray(inputs[k], np.float32)[0])
    for k in ("ln1_g", "ln1_b", "ln2_g", "ln2_b", "router_w", "router_b", "w_gate", "w_up", "w_down",
              "sh_gate", "sh_up", "sh_down"):
        shared[k] = np.ascontiguousarray(np.asarray(inputs[k], np.float32))
    shared["bias_tiles"] = bt
    shared["bias_far"] = b15
    shared["consts"] = consts
    x = np.asarray(inputs["x"], np.float32)
    in_maps = []
    for seqs in core_seqs:
        m = dict(shared)
        m["x"] = np.ascontiguousarray(x[seqs])
        in_maps.append(m)
    res = run_bass_kernel_spmd(nc, in_maps, core_ids=list(range(len(core_seqs))))
    outp = np.zeros_like(x)
    for ci, seqs in enumerate(core_seqs):
        outp[seqs] = res.results[ci]["out"]
    return outp


def kernel(**inputs):
    x = np.asarray(inputs["x"])
    B, SL, D = x.shape
    E = np.asarray(inputs["router_w"]).shape[-1]
    ncore = 2
    nseq = B // ncore
    cfg = Cfg(S=SL, E=E, TOPK=8, IDX_TOPK=256, NSEQ=nseq)
    core_seqs = [list(range(ci * nseq, (ci + 1) * nseq)) for ci in range(ncore)]
    return run_cfg(cfg, inputs, core_seqs)
```

```python
import math
import numpy as np
from contextlib import ExitStack
import concourse.bass as bass
import concourse.mybir as mybir
from concourse.bass_utils import run_bass_kernel_spmd

F32 = mybir.dt.float32
BF16 = mybir.dt.bfloat16
AF = mybir.ActivationFunctionType
ALU = mybir.AluOpType
AX = mybir.AxisListType

ENG_NAMES = ("pe", "act", "dve", "pool", "sp")
SEM_EPOCH = 30000
NEG = -1.0e30


class Buf:
    __slots__ = ("name", "writers", "readers", "dsem", "dcount", "war")

    def __init__(self, name):
        self.name = name
        self.writers = []
        self.readers = []
        self.war = []
        self.dsem = None
        self.dcount = 0


class Op:
    __slots__ = ("eng", "fn", "deps", "is_dma", "dbuf", "signal", "event")


class Sched:
    def __init__(self, nc):
        self.nc = nc
        self.es = ExitStack()
        self.ops = []
        self._sems = []
        self._n = 0
        self.done = 0
        self.bar = 0
        self.last_eng = {}
        self.dma_last = {}
        self.eng_sems = {e: [] for e in ENG_NAMES}
        self.eng_cnt = {e: 0 for e in ENG_NAMES}
        self.waited = {e: {} for e in ENG_NAMES}
        self.pes = None
        self.uid = 0
        self.stats = {e: [0, 0] for e in ENG_NAMES}
        self.free_dsems = []
        self.live_dbufs = []

    def sbuf(self, name, shape, dtype):
        self.uid += 1
        es = self.pes if self.pes is not None else self.es
        t = es.enter_context(self.nc.sbuf_tensor("%s_%d" % (name, self.uid), list(shape), dtype))
        return t, Buf(name)

    def begin_phase(self):
        assert self.pes is None
        self.pes = ExitStack()
        self.phase_no = getattr(self, "phase_no", 0) + 1
        self.skip = self.phase_no > getattr(self, "limit", 10 ** 9)

    def end_phase(self):
        if self.skip:
            self.pes.close()
            self.pes = None
            return
        self.barrier()
        self.emit()
        self.pes.close()
        self.pes = None

    def barrier(self):
        for x in ENG_NAMES:
            deps = [o for y, o in self.last_eng.items() if y != x and o >= self.bar]
            deps += [o for o in self.dma_last.values() if o >= self.bar]
            op = Op()
            op.eng = x
            op.fn = lambda e: e.nop()
            op.is_dma = False
            op.dbuf = None
            op.signal = False
            op.event = None
            op.deps = sorted(set(deps))
            self.ops.append(op)
        self.bar = len(self.ops)
        self.last_eng = {}
        self.dma_last = {}

    def psum(self, name, shape, dtype):
        t = self.es.enter_context(self.nc.psum_tensor(name, list(shape), dtype))
        return t, Buf(name)

    def sem(self, name):
        s = self.es.enter_context(self.nc.semaphore(name))
        self._sems.append(s)
        return s

    def _record(self, eng, fn, reads, writes, pwrites, is_dma, dbuf):
        if getattr(self, "skip", False):
            return -1
        deps = set()
        for b in reads:
            deps.update(b.writers)
        for b in writes:
            deps.update(b.writers)
            deps.update(b.readers)
        for b in pwrites:
            deps.update(b.readers)
            deps.update(b.war)
        op = Op()
        op.eng = eng
        op.fn = fn
        op.is_dma = is_dma
        op.dbuf = dbuf
        op.signal = False
        op.event = None
        oid = len(self.ops)
        keep = []
        if is_dma:
            self.dma_last[id(dbuf)] = oid
        else:
            self.last_eng[eng] = oid
        for d in deps:
            if d < self.bar:
                continue
            p = self.ops[d]
            if (not p.is_dma) and p.eng == eng and eng in ("pe", "sp"):
                continue
            keep.append(d)
        op.deps = sorted(keep)
        self.ops.append(op)
        for b in reads:
            b.readers.append(oid)
        for b in writes:
            b.war = [d for d in list(b.writers) + list(b.readers) if d >= self.bar]
            b.writers = [oid]
            b.readers = []
        for b in pwrites:
            b.writers.append(oid)
        return oid

    def op(self, eng, fn, reads=(), writes=(), pwrites=()):
        return self._record(eng, fn, reads, writes, pwrites, False, None)

    def dma(self, eng, out, in_, reads=(), writes=(), pwrites=(), sem_buf=None, **kw):
        assert sem_buf is not None

        def fn(e):
            return e.dma_start(out=out, in_=in_, **kw)
        return self._record(eng, fn, reads, writes, pwrites, True, sem_buf)

    def emit(self):
        nc = self.nc
        ops = self.ops
        lo = self.done
        for op in ops[lo:]:
            for d in op.deps:
                assert d >= lo, "dep on an op emitted in an earlier batch"
                if not ops[d].is_dma:
                    ops[d].signal = True
        for op in ops[lo:]:
            if op.is_dma:
                b = op.dbuf
                if b.dsem is None:
                    if self.free_dsems:
                        b.dsem, b.dcount = self.free_dsems.pop()
                    else:
                        b.dsem = self.sem("d%d" % len(self._sems))
                        b.dcount = 0
                    self.live_dbufs.append(b)
                b.dcount += 16
                op.event = (b.dsem, b.dcount)
            elif op.signal:
                e = op.eng
                k = self.eng_cnt[e] // SEM_EPOCH
                if k >= len(self.eng_sems[e]):
                    self.eng_sems[e].append(self.sem("e_%s_%d" % (e, k)))
                self.eng_cnt[e] += 1
                op.event = (self.eng_sems[e][k], self.eng_cnt[e] - k * SEM_EPOCH)
        per_eng = {e: [] for e in ENG_NAMES}
        for op in ops[lo:]:
            per_eng[op.eng].append(op)

        def run_engine(ename, eobj):
            waited = self.waited[ename]
            for op in per_eng[ename]:
                need = {}
                for d in op.deps:
                    s, v = ops[d].event
                    key = id(s)
                    if waited.get(key, 0) >= v:
                        continue
                    if key not in need or need[key][1] < v:
                        need[key] = (s, v)
                for key, (s, v) in need.items():
                    eobj.wait_ge(s, v)
                    waited[key] = v
                    self.stats[ename][1] += 1
                ins = op.fn(eobj)
                self.stats[ename][0] += 1
                if op.event is not None:
                    ins.then_inc(op.event[0], 16 if op.is_dma else 1)
                op.fn = None

        with nc.Block() as block:
            @block.sync
            def _(e):
                run_engine("sp", e)

            @block.scalar
            def _(e):
                run_engine("act", e)

            @block.vector
            def _(e):
                run_engine("dve", e)

            @block.gpsimd
            def _(e):
                run_engine("pool", e)

            @block.tensor
            def _(e):
                run_engine("pe", e)
        self.done = len(ops)
        self.n_sems = len(self._sems)
        for b in self.live_dbufs:
            self.free_dsems.append((b.dsem, b.dcount))
            b.dsem = None
        self.live_dbufs = []
        return self.stats

    def close(self):
        self.es.close()


class Ring:
    def __init__(self, S, name, n, shape, dtype, psum=False):
        self.slots = []
        for i in range(n):
            self.slots.append((S.psum if psum else S.sbuf)("%s%d" % (name, i), shape, dtype))
        self.i = 0

    def get(self):
        s = self.slots[self.i % len(self.slots)]
        self.i += 1
        return s


class Cfg:
    def __init__(self, S=4096, E=64, TOPK=8, IDX_TOPK=256, NSEQ=1, DEPTH=2):
        self.S = S
        self.NB = S // 128
        self.D = 2048
        self.H = 16
        self.E = E
        self.TOPK = TOPK
        self.DE = 512
        self.IDX_TOPK = min(IDX_TOPK, S // 4)
        self.NSEQ = NSEQ
        self.DEPTH = DEPTH
        self.ALPHA = (2 * DEPTH) ** 0.25
        self.QL = 512
        self.KVL = 256
        self.IH = 16
        self.ID = 64
        self.BW = 512 + 256 + 64 + 16


def t5_bucket_np(rel):
    half = 16
    max_exact = 8
    n = np.abs(rel)
    large = max_exact + (np.log(np.maximum(n, 1).astype(np.float32) / max_exact)
                         / math.log(128 / max_exact) * (half - max_exact)).astype(np.int32)
    large = np.minimum(large, half - 1)
    return np.where(rel > 0, half, 0) + np.where(n < max_exact, n, large)


def build_program(cfg, debug_outs=()):
    nc = bass.Bass("TRN2", target_bir_lowering=False)
    S = Sched(nc)
    c = cfg
    S.limit = getattr(cfg, "LIMIT", 10 ** 9)
    NB, D, H, E, SL = c.NB, c.D, c.H, c.E, c.S
    DC = D // 128
    NG = SL // 512

    def din(name, shape, dt=F32):
        return nc.dram_tensor(name, list(shape), dt, kind="ExternalInput").ap()

    def dscr(name, shape, dt):
        return nc.dram_tensor(name, list(shape), dt, kind="Internal").ap()

    x_in = din("x", [c.NSEQ, SL, D])
    a_w_in = din("a_w_in", [D, 3 * D + H])
    a_b_f = din("a_b_f", [1, H])
    a_w_out = din("a_w_out", [D, D])
    b_w_in = din("b_w_in", [D, c.BW])
    b_q_norm = din("b_q_norm", [1, c.QL])
    b_kv_norm = din("b_kv_norm", [1, c.KVL])
    b_w_uq = din("b_w_uq", [c.QL, D])
    b_w_iq = din("b_w_iq", [c.QL, c.IH * c.ID])
    b_w_uk = din("b_w_uk", [H, c.KVL, 128])
    b_w_uv = din("b_w_uv", [H, c.KVL, 128])
    b_w_out = din("b_w_out", [D, D])
    bt_in = din("bias_tiles", [2, H, 128, 128])
    b15_in = din("bias_far", [1, H])
    ln1_g = din("ln1_g", [c.DEPTH, D])
    ln1_b = din("ln1_b", [c.DEPTH, D])
    ln2_g = din("ln2_g", [c.DEPTH, D])
    ln2_b = din("ln2_b", [c.DEPTH, D])
    router_w = din("router_w", [c.DEPTH, D, E])
    router_b = din("router_b", [c.DEPTH, E])
    w_gate = din("w_gate", [c.DEPTH, E, D, c.DE])
    w_up = din("w_up", [c.DEPTH, E, D, c.DE])
    w_down = din("w_down", [c.DEPTH, E, c.DE, D])
    sh_gate = din("sh_gate", [c.DEPTH, D, c.DE])
    sh_up = din("sh_up", [c.DEPTH, D, c.DE])
    sh_down = din("sh_down", [c.DEPTH, c.DE, D])
    consts = din("consts", [5, 128, 128])
    out = nc.dram_tensor("out", [c.NSEQ, SL, D], F32, kind="ExternalOutput").ap()
    dbg = {}
    for nm, shp in debug_outs:
        dbg[nm] = nc.dram_tensor("dbg_" + nm, list(shp), F32, kind="ExternalOutput").ap()

    xT_d = dscr("xT_d", [DC, 128, SL], BF16)
    qT_d = dscr("qT_d", [H, 128, SL], BF16)
    kT_d = dscr("kT_d", [H, 128, SL], BF16)
    v_d = dscr("v_d", [SL, D], BF16)
    o_d = dscr("o_d", [SL, D], BF16)
    x1_d = dscr("x1_d", [SL, D], F32)
    x1T_d = dscr("x1T_d", [DC, 128, SL], BF16)
    x2_d = dscr("x2_d", [SL, D], F32)
    cqT_d = dscr("cqT_d", [4, 128, SL], BF16)
    ckvT_d = dscr("ckvT_d", [2, 128, SL], BF16)
    kiT_d = dscr("kiT_d", [64, SL], BF16)
    qiT_d = dscr("qiT_d", [c.IH, 64, SL], BF16)
    mT_d = dscr("mT_d", [NB, NB, 128, 128], BF16)

    identf, identf_b = S.sbuf("identf", [128, 128], F32)
    identb, identb_b = S.sbuf("identb", [128, 128], BF16)
    trif, trif_b = S.sbuf("trif", [128, 128], F32)
    onesf, onesf_b = S.sbuf("onesf", [128, 128], F32)
    causb, causb_b = S.sbuf("causb", [128, 128], BF16)
    chneg, chneg_b = S.sbuf("chneg", [128, 128], F32)
    epsb, epsb_b = S.sbuf("epsb", [128, 4], F32)
    logf, logf_b = S.sbuf("logf", [128, NB, H], F32)
    Fs, Fs_b = S.sbuf("Fs", [128, NB, H], F32)
    Fend, Fend_b = S.sbuf("Fend", [128, NB + 1, H], F32)
    G_all, G_b = S.sbuf("G_all", [128, NB, E + 1], F32)
    wI, wI_b = S.sbuf("wI", [128, NB, c.IH], F32)
    bfb, bfb_b = S.sbuf("bfb", [128, H], F32)
    b15b, b15b_b = S.sbuf("b15b", [128, H], F32)
    lng, lng_b = S.sbuf("lng", [128, D], F32)
    lnb, lnb_b = S.sbuf("lnb", [128, D], F32)
    small = Ring(S, "small", 6, [128, 64], F32)
    stats = Ring(S, "stats", 2, [128, 4, 6], F32)
    obf = Ring(S, "obf", 3, [128, 512], BF16)

    mmR = Ring(S, "pmm", 4, [128, 512], F32, psum=True)
    tpfR = Ring(S, "ptf", 1, [128, 512], F32, psum=True)
    tpbR = Ring(S, "ptb", 1, [128, 1024], BF16, psum=True)
    poR = Ring(S, "ppo", 2, [128, 512], F32, psum=True)

    S.begin_phase()
    S.dma("sp", identf[:], consts[0], writes=[identf_b], sem_buf=identf_b)
    S.dma("pool", identb[:], consts[0], writes=[identb_b], sem_buf=identb_b)
    S.dma("sp", trif[:], consts[1], writes=[trif_b], sem_buf=trif_b)
    S.dma("sp", onesf[:], consts[2], writes=[onesf_b], sem_buf=onesf_b)
    S.dma("pool", causb[:], consts[3], writes=[causb_b], sem_buf=causb_b)
    S.dma("sp", chneg[:], consts[4], writes=[chneg_b], sem_buf=chneg_b)
    S.dma("sp", bfb[:], bass.AP(a_b_f.tensor, a_b_f.offset, [[0, 128], [1, H]]), writes=[bfb_b], sem_buf=bfb_b)
    S.dma("sp", b15b[:], bass.AP(b15_in.tensor, b15_in.offset, [[0, 128], [1, H]]), writes=[b15b_b], sem_buf=b15b_b)
    S.op("dve", lambda e: e.memset(epsb[:, 0:1], 1e-5), writes=[epsb_b])
    S.op("dve", lambda e: e.memset(epsb[:, 1:2], 1e-6), pwrites=[epsb_b])
    S.op("dve", lambda e: e.memset(epsb[:, 2:3], 1.0), pwrites=[epsb_b])
    S.op("dve", lambda e: e.memset(epsb[:, 3:4], 0.0), pwrites=[epsb_b])
    S.end_phase()

    rr = {"i": 0}

    def bcast_rows(ap_row, n):
        return bass.AP(ap_row.tensor, ap_row.offset, [[0, 128], [1, n]])

    def evac_eng():
        return "dve"

    def copy(eng, o, ob, i, ib, partial=False):
        w = dict(pwrites=[ob]) if partial else dict(writes=[ob])
        if eng == "act":
            S.op("act", lambda e: e.copy(o, i), reads=[ib], **w)
        else:
            S.op(eng, lambda e: e.tensor_copy(o, i), reads=[ib], **w)

    def mm_chain(ps, psb, pairs, reads):
        n = len(pairs)
        for k, (l, r) in enumerate(pairs):
            S.op("pe", lambda e, l=l, r=r, k=k: e.matmul(ps, l, r, start=(k == 0), stop=(k == n - 1)),
                 reads=reads, writes=[psb] if k == 0 else (), pwrites=[psb] if k > 0 else ())

    def transpose_cols(src, srcb, ncols, dst_fn, dstb, dt, first_full=True):
        nchunk = ncols // 128
        per = 4 if dt == F32 else 8
        first = first_full
        for k0 in range(0, nchunk, per):
            kn = min(per, nchunk - k0)
            pt, ptb = (tpfR if dt == F32 else tpbR).get()
            idt, idb = (identf, identf_b) if dt == F32 else (identb, identb_b)
            for k in range(kn):
                S.op("pe", lambda e, k=k, k0=k0, pt=pt, idt=idt: e.transpose(
                    pt[:, k * 128:(k + 1) * 128], src[:, (k0 + k) * 128:(k0 + k + 1) * 128], idt[:]),
                    reads=[srcb, idb], writes=[ptb] if k == 0 else (), pwrites=[ptb] if k > 0 else ())
            for k in range(kn):
                copy(evac_eng(), dst_fn(k0 + k), dstb, pt[:, k * 128:(k + 1) * 128], ptb, partial=not first)
                first = False

    def layer_norm_rows(y, yb):
        st, stb = stats.get()
        for k in range(4):
            S.op("dve", lambda e, k=k: e.bn_stats(st[:, k, :], y[:, k * 512:(k + 1) * 512]),
                 reads=[yb], writes=[stb] if k == 0 else (), pwrites=[stb] if k else ())
        sm, smb = small.get()
        S.op("dve", lambda e: e.bn_aggr(sm[:, 0:2], st[:].rearrange("p a b -> p (a b)")), reads=[stb], writes=[smb])
        S.op("act", lambda e: e.activation(sm[:, 2:3], sm[:, 1:2], AF.Sqrt, bias=epsb[:, 0:1], scale=1.0),
             reads=[smb, epsb_b], pwrites=[smb])
        S.op("dve", lambda e: e.reciprocal(sm[:, 3:4], sm[:, 2:3]), reads=[smb], pwrites=[smb])
        S.op("dve", lambda e: e.tensor_scalar(y[:], y[:], sm[:, 0:1], sm[:, 3:4], ALU.subtract, ALU.mult),
             reads=[smb, yb], writes=[yb])
        S.op("pool", lambda e: e.tensor_tensor(y[:], y[:], lng[:], ALU.mult), reads=[yb, lng_b], writes=[yb])
        S.op("pool", lambda e: e.tensor_tensor(y[:], y[:], lnb[:], ALU.add), reads=[yb, lnb_b], writes=[yb])

    def load_ln(gsrc, bsrc, li):
        S.dma("sp", lng[:], bcast_rows(gsrc[li:li + 1, :], D), writes=[lng_b], sem_buf=lng_b)
        S.dma("sp", lnb[:], bcast_rows(bsrc[li:li + 1, :], D), writes=[lnb_b], sem_buf=lnb_b)

    def phase_transpose_in(src_d, src_b, dstT_d, dstT_b):
        S.begin_phase()
        xrow = Ring(S, "xrow", 2, [128, D], F32)
        xTblk = Ring(S, "xTblk", 2, [128, DC, 128], BF16)
        for b in range(NB):
            xr, xrb = xrow.get()
            S.dma("sp", xr[:], src_d[b * 128:(b + 1) * 128, :], reads=[src_b], writes=[xrb], sem_buf=xrb)
            xt, xtb = xTblk.get()
            import os
            if os.environ.get("DBG_P2", "") == "load":
                continue
            transpose_cols(xr, xrb, D, lambda k, xt=xt: xt[:, k, :], xtb, F32)
            v = os.environ.get("DBG_ST", "pool")
            if v != "none":
                S.dma(v, dstT_d[:, :, b * 128:(b + 1) * 128].rearrange("c p t -> p c t"), xt[:],
                      reads=[xtb], pwrites=[dstT_b], sem_buf=xtb)
        S.end_phase()

    def mk_load_w(wt):
        def load_w_tile(wsrc, col0, ncol, nk=DC):
            w, wb = wt.get()
            S.dma("pool", w[:, 0:nk, 0:ncol], wsrc[:, col0:col0 + ncol].rearrange("(c p) n -> p c n", p=128),
                  writes=[wb], sem_buf=wb)
            return w, wb
        return load_w_tile

    def proj_fm(xg, xgb, w, wb, nk, mw, nm, dst_fn, dst_b, k0=0):
        for m in range(nm):
            ps, psb = mmR.get()
            mm_chain(ps[0:mw, :], psb, [(w[:, k, m * mw:(m + 1) * mw], xg[:, k0 + k, :]) for k in range(nk)], [xgb, wb])
            ob_, obb = obf.get()
            copy(evac_eng(), ob_[0:mw, :], obb, ps[0:mw, :], psb)
            S.dma("sp", dst_fn(m), ob_[0:mw, :], reads=[obb], pwrites=[dst_b], sem_buf=obb)

    def proj_tm(xg, xgb, w, wb, nk, ncol, tb, dst_ap, dst_b, k0=0):
        ps, psb = mmR.get()
        mm_chain(ps[:, 0:ncol], psb, [(xg[:, k0 + k, tb * 128:(tb + 1) * 128], w[:, k, 0:ncol]) for k in range(nk)], [xgb, wb])
        ob_, obb = obf.get()
        copy(evac_eng(), ob_[:, 0:ncol], obb, ps[:, 0:ncol], psb)
        S.dma("sp", dst_ap, ob_[:, 0:ncol], reads=[obb], pwrites=[dst_b], sem_buf=obb)

    def fox_projections(xT_b, qT_b, kT_b, v_b):
        S.begin_phase()
        xTg = Ring(S, "xTg", 2, [128, DC, 512], BF16)
        wt = Ring(S, "wt", 3, [128, DC, 512], BF16)
        load_w_tile = mk_load_w(wt)
        for g in range(NG):
            xg, xgb = xTg.get()
            S.dma("sp", xg[:], xT_d[:, :, g * 512:(g + 1) * 512].rearrange("c p t -> p c t"),
                  reads=[xT_b], writes=[xgb], sem_buf=xgb)
            for ct in range(8):
                w, wb = load_w_tile(a_w_in, ct * 512, 512)
                dstT, dstb = (qT_d, qT_b) if ct < 4 else (kT_d, kT_b)
                h0 = (ct % 4) * 4
                proj_fm(xg, xgb, w, wb, DC, 128, 4,
                        lambda m, dstT=dstT, h0=h0, g=g: dstT[h0 + m, :, g * 512:(g + 1) * 512], dstb)
            for ct in range(8, 12):
                w, wb = load_w_tile(a_w_in, ct * 512, 512)
                for tb in range(4):
                    r0 = g * 512 + tb * 128
                    proj_tm(xg, xgb, w, wb, DC, 512, tb, v_d[r0:r0 + 128, (ct - 8) * 512:(ct - 7) * 512], v_b)
            w, wb = load_w_tile(a_w_in, 3 * D, H)
            for tb in range(4):
                blk = g * 4 + tb
                ps, psb = poR.get()
                mm_chain(ps[:, 0:H], psb, [(xg[:, k, tb * 128:(tb + 1) * 128], w[:, k, 0:H]) for k in range(DC)], [xgb, wb])
                sm, smb = small.get()
                S.op("dve", lambda e, sm=sm, ps=ps: e.tensor_tensor(sm[:, 0:16], ps[:, 0:H], bfb[:], ALU.add),
                     reads=[psb, bfb_b], writes=[smb])
                S.op("act", lambda e, sm=sm: e.activation(sm[:, 16:32], sm[:, 0:16], AF.Abs),
                     reads=[smb], pwrites=[smb])
                S.op("act", lambda e, sm=sm: e.activation(sm[:, 32:48], sm[:, 16:32], AF.Exp, scale=-1.0),
                     reads=[smb], pwrites=[smb])
                S.op("act", lambda e, sm=sm: e.activation(sm[:, 32:48], sm[:, 32:48], AF.Ln, bias=epsb[:, 2:3], scale=1.0),
                     reads=[smb, epsb_b], pwrites=[smb])
                S.op("dve", lambda e, sm=sm: e.tensor_scalar_min(sm[:, 48:64], sm[:, 0:16], 0.0), reads=[smb], pwrites=[smb])
                S.op("dve", lambda e, sm=sm, blk=blk: e.tensor_tensor(logf[:, blk, :], sm[:, 48:64], sm[:, 32:48], ALU.subtract),
                     reads=[smb], pwrites=[logf_b])
        S.op("dve", lambda e: e.memset(Fend[:, 0, :], 0.0), reads=[logf_b], writes=[Fend_b])
        for b in range(NB):
            ps, psb = poR.get()
            S.op("pe", lambda e, ps=ps, b=b: e.matmul(ps[:, 0:H], trif[:], logf[:, b, :], start=True, stop=True),
                 reads=[trif_b, logf_b], writes=[psb])
            S.op("pe", lambda e, ps=ps, b=b: e.matmul(ps[:, 32:32 + H], onesf[:], logf[:, b, :], start=True, stop=True),
                 reads=[onesf_b, logf_b], pwrites=[psb])
            S.op("dve", lambda e, ps=ps, b=b: e.tensor_tensor(Fs[:, b, :], ps[:, 0:H], Fend[:, b, :], ALU.add),
                 reads=[psb, Fend_b], pwrites=[Fs_b])
            S.op("dve", lambda e, ps=ps, b=b: e.tensor_tensor(Fend[:, b + 1, :], ps[:, 32:32 + H], Fend[:, b, :], ALU.add),
                 reads=[psb], pwrites=[Fend_b])
        S.end_phase()

    def attention(mode, qT_b, kT_b, v_b, o_b, mT_b=None):
        S.begin_phase()
        scale = 128 ** -0.5
        kTh = Ring(S, "kTh", 2, [128, SL], BF16)
        qTh = Ring(S, "qTh", 2, [128, SL], BF16)
        vh = Ring(S, "vh", 2, [128, NB, 132], BF16)
        pbf = Ring(S, "pbf", 4, [128, 128], BF16)
        biasij = Ring(S, "biasij", 4, [128, H], F32)
        if mode == "dsa":
            pf32 = Ring(S, "pf32", 2, [128, 128], F32)
            mrow = Ring(S, "mrow", 2, [128, NB, 128], BF16)
            btile, btile_b = S.sbuf("btile", [128, 2, H, 128], F32)
            S.dma("sp", btile[:], bt_in.rearrange("o h s t -> s o h t"), writes=[btile_b], sem_buf=btile_b)
        for h in range(H):
            kt, ktb = kTh.get()
            qt, qtb = qTh.get()
            vv, vvb = vh.get()
            S.dma("sp", kt[:], kT_d[h], reads=[kT_b], writes=[ktb], sem_buf=ktb)
            S.dma("sp", qt[:], qT_d[h], reads=[qT_b], writes=[qtb], sem_buf=qtb)
            S.dma("sp", vv[:, :, 0:128], v_d[:, h * 128:(h + 1) * 128].rearrange("(b p) d -> p b d", p=128),
                  reads=[v_b], writes=[vvb], sem_buf=vvb)
            S.op("pool", lambda e, vv=vv: e.memset(vv[:, :, 128:129], 1.0), reads=[vvb], pwrites=[vvb])
            for i in range(NB):
                po, pob = poR.get()
                if mode == "dsa":
                    mr, mrb = mrow.get()
                    S.dma("sp", mr[:, 0:i + 1, :], mT_d[i, 0:i + 1].rearrange("j s t -> s j t"),
                          reads=[mT_b], writes=[mrb], sem_buf=mrb)
                for j in range(i + 1):
                    ps, psb = mmR.get()
                    S.op("pe", lambda e, ps=ps, kt=kt, qt=qt, i=i, j=j: e.matmul(
                        ps[:, 0:128], kt[:, j * 128:(j + 1) * 128], qt[:, i * 128:(i + 1) * 128], start=True, stop=True),
                        reads=[ktb, qtb], writes=[psb])
                    p, pb = pbf.get()
                    if mode == "fox":
                        bi, bib = biasij.get()
                        S.op("dve", lambda e, bi=bi, i=i, j=j: e.tensor_tensor(bi[:], Fend[:, i + 1, :], Fs[:, j, :], ALU.subtract),
                             reads=[Fend_b, Fs_b], writes=[bib])
                        S.op("act", lambda e, p=p, ps=ps, bi=bi, h=h: e.activation(p[:], ps[:, 0:128], AF.Exp,
                                                                                   bias=bi[:, h:h + 1], scale=scale),
                             reads=[psb, bib], writes=[pb])
                        if j == i:
                            S.op("dve", lambda e, p=p: e.tensor_tensor(p[:], p[:], causb[:], ALU.mult),
                                 reads=[pb, causb_b], writes=[pb])
                    else:
                        if j >= i - 1:
                            tf, tfb = pf32.get()
                            S.op("dve", lambda e, tf=tf, ps=ps, i=i, j=j, h=h: e.scalar_tensor_tensor(
                                tf[:], ps[:, 0:128], scale, btile[:, i - j, h, :], ALU.mult, ALU.add),
                                reads=[psb, btile_b], writes=[tfb])
                            S.op("act", lambda e, p=p, tf=tf: e.activation(p[:], tf[:], AF.Exp), reads=[tfb], writes=[pb])
                        else:
                            S.op("act", lambda e, p=p, ps=ps, h=h: e.activation(p[:], ps[:, 0:128], AF.Exp,
                                                                                bias=b15b[:, h:h + 1], scale=scale),
                                 reads=[psb, b15b_b], writes=[pb])
                        S.op("pool", lambda e, p=p, mr=mr, j=j: e.tensor_tensor(p[:], p[:], mr[:, j, :], ALU.mult),
                             reads=[pb, mrb], writes=[pb])
                    S.op("pe", lambda e, po=po, p=p, vv=vv, i=i, j=j: e.matmul(po[:, 0:129], p[:], vv[:, j, 0:129],
                                                                              start=(j == 0), stop=(j == i)),
                         reads=[pb, vvb], writes=[pob] if j == 0 else (), pwrites=[pob] if j > 0 else ())
                sm, smb = small.get()
                S.op("dve", lambda e, sm=sm, po=po: e.reciprocal(sm[:, 0:1], po[:, 128:129]), reads=[pob], writes=[smb])
                ob_, obb = obf.get()
                S.op("dve", lambda e, ob_=ob_, po=po, sm=sm: e.tensor_scalar(ob_[:, 0:128], po[:, 0:128], sm[:, 0:1], None, ALU.mult),
                     reads=[pob, smb], writes=[obb])
                S.dma("sp", o_d[i * 128:(i + 1) * 128, h * 128:(h + 1) * 128], ob_[:, 0:128],
                      reads=[obb], pwrites=[o_b], sem_buf=obb)
        S.end_phase()

    def outproj_ln_router(li, w_out_ap, xres_d, xres_b, o_b, x1_b, x1T_b):
        S.begin_phase()
        wt = Ring(S, "wt", 3, [128, DC, 512], BF16)
        load_w_tile = mk_load_w(wt)
        obfw = Ring(S, "obfw", 2, [128, D], BF16)
        xTblk = Ring(S, "xTblk", 2, [128, DC, 128], BF16)
        xTf32 = Ring(S, "xTf32", 1, [128, DC, 128], F32)
        xrow = Ring(S, "xrow", 2, [128, D], F32)
        yrow = Ring(S, "yrow", 2, [128, D], F32)
        smallw = Ring(S, "smallw", 2, [128, 2 * E + 16], F32)
        rwt, rwt_b = S.sbuf("rwt", [128, DC, E], F32)
        rbb, rbb_b = S.sbuf("rbb", [128, E], F32)
        load_ln(ln1_g, ln1_b, li)
        S.dma("sp", rwt[:], router_w[li].rearrange("(c p) n -> p c n", p=128), writes=[rwt_b], sem_buf=rwt_b)
        S.dma("sp", rbb[:], bcast_rows(router_b[li:li + 1, :], E), writes=[rbb_b], sem_buf=rbb_b)
        for b in range(NB):
            r0 = b * 128
            orow, orowb = obfw.get()
            S.dma("sp", orow[:], o_d[r0:r0 + 128, :], reads=[o_b], writes=[orowb], sem_buf=orowb)
            oT, oTb = xTblk.get()
            transpose_cols(orow, orowb, D, lambda k, oT=oT: oT[:, k, :], oTb, BF16)
            xr, xrb = xrow.get()
            S.dma("sp", xr[:], xres_d[r0:r0 + 128, :], reads=[xres_b], writes=[xrb], sem_buf=xrb)
            y, yb = yrow.get()
            for n in range(4):
                w, wb = load_w_tile(w_out_ap, n * 512, 512)
                ps, psb = mmR.get()
                mm_chain(ps[:], psb, [(oT[:, k, :], w[:, k, :]) for k in range(DC)], [oTb, wb])
                S.op("dve", lambda e, y=y, xr=xr, ps=ps, n=n: e.scalar_tensor_tensor(
                    y[:, n * 512:(n + 1) * 512], xr[:, n * 512:(n + 1) * 512], c.ALPHA, ps[:], ALU.mult, ALU.add),
                    reads=[xrb, psb], writes=[yb] if n == 0 else (), pwrites=[yb] if n else ())
            layer_norm_rows(y, yb)
            S.dma("sp", x1_d[r0:r0 + 128, :], y[:], reads=[yb], pwrites=[x1_b], sem_buf=yb)
            xTf, xTfb = xTf32.get()
            transpose_cols(y, yb, D, lambda k, xTf=xTf: xTf[:, k, :], xTfb, F32)
            xt, xtb = xTblk.get()
            S.op("pool", lambda e, xt=xt, xTf=xTf: e.tensor_copy(xt[:], xTf[:]), reads=[xTfb], writes=[xtb])
            S.dma("pool", x1T_d[:, :, r0:r0 + 128].rearrange("c p t -> p c t"), xt[:], reads=[xtb], pwrites=[x1T_b], sem_buf=xtb)
            ps, psb = poR.get()
            mm_chain(ps[:, 0:E], psb, [(xTf[:, k, :], rwt[:, k, :]) for k in range(DC)], [xTfb, rwt_b])
            sm, smb = smallw.get()
            S.op("act", lambda e, sm=sm, ps=ps: e.activation(sm[:, 0:E], ps[:, 0:E], AF.Sigmoid), reads=[psb], writes=[smb])
            S.op("dve", lambda e, sm=sm: e.tensor_tensor(sm[:, E:2 * E], sm[:, 0:E], rbb[:], ALU.add), reads=[smb, rbb_b], pwrites=[smb])
            S.op("dve", lambda e, sm=sm: e.max(sm[:, 2 * E:2 * E + 8], sm[:, E:2 * E]), reads=[smb], pwrites=[smb])
            assert c.TOPK == 8
            S.op("dve", lambda e, sm=sm: e.tensor_tensor(sm[:, 2 * E + 10:2 * E + 11], sm[:, 2 * E:2 * E + 1], sm[:, 2 * E + 7:2 * E + 8], ALU.min),
                 reads=[smb], pwrites=[smb])
            S.op("dve", lambda e, sm=sm: e.tensor_scalar(sm[:, E:2 * E], sm[:, E:2 * E], sm[:, 2 * E + 10:2 * E + 11], None, ALU.is_ge),
                 reads=[smb], pwrites=[smb])
            S.op("dve", lambda e, sm=sm: e.tensor_tensor(sm[:, 0:E], sm[:, 0:E], sm[:, E:2 * E], ALU.mult), reads=[smb], pwrites=[smb])
            S.op("dve", lambda e, sm=sm: e.reduce_sum(sm[:, 2 * E + 8:2 * E + 9], sm[:, 0:E], axis=AX.X), reads=[smb], pwrites=[smb])
            S.op("dve", lambda e, sm=sm: e.reciprocal(sm[:, 2 * E + 9:2 * E + 10], sm[:, 2 * E + 8:2 * E + 9]), reads=[smb], pwrites=[smb])
            S.op("dve", lambda e, sm=sm: e.tensor_scalar(sm[:, 0:E], sm[:, 0:E], sm[:, 2 * E + 9:2 * E + 10], None, ALU.mult),
                 reads=[smb], pwrites=[smb])
            S.op("dve", lambda e, sm=sm, b=b: e.tensor_scalar(G_all[:, b, 0:E], sm[:, 0:E], 2.5, None, ALU.mult),
                 reads=[smb], pwrites=[G_b])
            S.op("dve", lambda e, b=b: e.memset(G_all[:, b, E:E + 1], 1.0), pwrites=[G_b])
        import os
        if os.environ.get("DBG_G"):
            S.dma("sp", out[0][0:128, 0:NB * (E + 1)], G_all[:].rearrange("p b e -> p (b e)"), reads=[G_b], pwrites=[out_b], sem_buf=G_b)
        S.end_phase()

    def moe_and_ln2(li, x1_b, x1T_b, dst_d, dst_b):
        S.begin_phase()
        xTg = Ring(S, "xTg", 1, [128, DC, 512], BF16)
        wt = Ring(S, "wt", 3, [128, DC, 512], BF16)
        load_w_tile = mk_load_w(wt)
        wdn = Ring(S, "wdn", 1, [128, 4, D], BF16)
        hT = Ring(S, "hT", 2, [128, 4, 512], BF16)
        sg = Ring(S, "sg", 2, [128, 512], F32)
        acc, acc_b = S.sbuf("acc", [128, 4, D], F32)
        xrow = Ring(S, "xrow", 2, [128, D], F32)
        load_ln(ln2_g, ln2_b, li)
        for g in range(NG):
            xg, xgb = xTg.get()
            S.dma("sp", xg[:], x1T_d[:, :, g * 512:(g + 1) * 512].rearrange("c p t -> p c t"),
                  reads=[x1T_b], writes=[xgb], sem_buf=xgb)
            for ei in range(E + 1):
                if ei < E:
                    wgs, wus, wds = w_gate[li, ei], w_up[li, ei], w_down[li, ei]
                else:
                    wgs, wus, wds = sh_gate[li], sh_up[li], sh_down[li]
                wg, wgb = load_w_tile(wgs, 0, 512)
                wu, wub = load_w_tile(wus, 0, 512)
                wd, wdb = wdn.get()
                S.dma("pool", wd[:], wds.rearrange("(c p) n -> p c n", p=128), writes=[wdb], sem_buf=wdb)
                ht, htb = hT.get()
                for m in range(4):
                    psg, psgb = mmR.get()
                    mm_chain(psg[:], psgb, [(wg[:, k, m * 128:(m + 1) * 128], xg[:, k, :]) for k in range(DC)], [xgb, wgb])
                    psu, psub = mmR.get()
                    mm_chain(psu[:], psub, [(wu[:, k, m * 128:(m + 1) * 128], xg[:, k, :]) for k in range(DC)], [xgb, wub])
                    s_, sb_ = sg.get()
                    S.op("act", lambda e, s_=s_, psg=psg: e.activation(s_[:], psg[:], AF.Silu), reads=[psgb], writes=[sb_])
                    S.op("dve", lambda e, ht=ht, s_=s_, psu=psu, m=m: e.tensor_tensor(ht[:, m, :], s_[:], psu[:], ALU.mult),
                         reads=[sb_, psub], writes=[htb] if m == 0 else (), pwrites=[htb] if m else ())
                for tb in range(4):
                    blk = g * 4 + tb
                    for n in range(4):
                        ps, psb = mmR.get()
                        mm_chain(ps[:], psb, [(ht[:, m, tb * 128:(tb + 1) * 128], wd[:, m, n * 512:(n + 1) * 512]) for m in range(4)],
                                 [htb, wdb])
                        a = acc[:, tb, n * 512:(n + 1) * 512]
                        if ei == 0:
                            S.op("dve", lambda e, a=a, ps=ps, blk=blk, ei=ei: e.tensor_scalar(a, ps[:], G_all[:, blk, ei:ei + 1], None, ALU.mult),
                                 reads=[psb, G_b], pwrites=[acc_b])
                        else:
                            S.op("dve", lambda e, a=a, ps=ps, blk=blk, ei=ei: e.scalar_tensor_tensor(
                                a, ps[:], G_all[:, blk, ei:ei + 1], a, ALU.mult, ALU.add),
                                reads=[psb, G_b, acc_b], pwrites=[acc_b])
            for tb in range(4):
                r0 = g * 512 + tb * 128
                xr, xrb = xrow.get()
                S.dma("sp", xr[:], x1_d[r0:r0 + 128, :], reads=[x1_b], writes=[xrb], sem_buf=xrb)
                S.op("dve", lambda e, xr=xr, tb=tb: e.scalar_tensor_tensor(xr[:], xr[:], c.ALPHA, acc[:, tb, :], ALU.mult, ALU.add),
                     reads=[xrb, acc_b], writes=[xrb])
                layer_norm_rows(xr, xrb)
                S.dma("sp", dst_d[r0:r0 + 128, :], xr[:], reads=[xrb], pwrites=[dst_b], sem_buf=xrb)
        S.end_phase()

    def dsa_projections(xT_b, qT_b, kT_b, v_b, kiT_b, qiT_b):
        cqT_b, ckvT_b = Buf("cqT"), Buf("ckvT")
        aux = {}
        S.begin_phase()
        xTg = Ring(S, "xTg", 2, [128, DC, 512], BF16)
        wt = Ring(S, "wt", 3, [128, DC, 512], BF16)
        load_w_tile = mk_load_w(wt)
        of32 = Ring(S, "of32", 4, [128, 512], F32)
        sg = Ring(S, "sg", 2, [128, 512], F32)
        xTblk = Ring(S, "xTblk", 2, [128, 8, 128], BF16)
        qnb, qnb_b = S.sbuf("qnb", [128, c.QL], F32)
        kvnb, kvnb_b = S.sbuf("kvnb", [128, c.KVL], F32)
        S.dma("sp", qnb[:], bcast_rows(b_q_norm, c.QL), writes=[qnb_b], sem_buf=qnb_b)
        S.dma("sp", kvnb[:], bcast_rows(b_kv_norm, c.KVL), writes=[kvnb_b], sem_buf=kvnb_b)
        for g in range(NG):
            xg, xgb = xTg.get()
            S.dma("sp", xg[:], xT_d[:, :, g * 512:(g + 1) * 512].rearrange("c p t -> p c t"),
                  reads=[xT_b], writes=[xgb], sem_buf=xgb)
            w0, w0b = load_w_tile(b_w_in, 0, 512)
            w1, w1b = load_w_tile(b_w_in, 512, c.BW - 512)
            for tb in range(4):
                blk = g * 4 + tb
                t0 = blk * 128
                ps0, ps0b = mmR.get()
                mm_chain(ps0[:], ps0b, [(xg[:, k, tb * 128:(tb + 1) * 128], w0[:, k, :]) for k in range(DC)], [xgb, w0b])
                ps1, ps1b = mmR.get()
                mm_chain(ps1[:, 0:336], ps1b, [(xg[:, k, tb * 128:(tb + 1) * 128], w1[:, k, 0:336]) for k in range(DC)], [xgb, w1b])
                cq, cqb = of32.get()
                ckv, ckvb = of32.get()
                sm, smb = small.get()
                sq, sqb = sg.get()
                S.op("act", lambda e, sq=sq, ps0=ps0, sm=sm: e.activation(sq[:], ps0[:], AF.Square, accum_out=sm[:, 0:1]),
                     reads=[ps0b], writes=[sqb, smb])
                S.op("act", lambda e, sq=sq, ps1=ps1, sm=sm: e.activation(sq[:, 0:256], ps1[:, 0:256], AF.Square, accum_out=sm[:, 1:2]),
                     reads=[ps1b], writes=[sqb], pwrites=[smb])
                S.op("act", lambda e, sm=sm: e.activation(sm[:, 2:3], sm[:, 0:1], AF.Sqrt, bias=epsb[:, 1:2], scale=1.0 / c.QL),
                     reads=[smb, epsb_b], pwrites=[smb])
                S.op("act", lambda e, sm=sm: e.activation(sm[:, 3:4], sm[:, 1:2], AF.Sqrt, bias=epsb[:, 1:2], scale=1.0 / c.KVL),
                     reads=[smb, epsb_b], pwrites=[smb])
                S.op("dve", lambda e, sm=sm: e.reciprocal(sm[:, 4:6], sm[:, 2:4]), reads=[smb], pwrites=[smb])
                S.op("dve", lambda e, cq=cq, ps0=ps0, sm=sm: e.scalar_tensor_tensor(cq[:], ps0[:], sm[:, 4:5], qnb[:], ALU.mult, ALU.mult),
                     reads=[ps0b, smb, qnb_b], writes=[cqb])
                S.op("dve", lambda e, ckv=ckv, ps1=ps1, sm=sm: e.scalar_tensor_tensor(ckv[:, 0:256], ps1[:, 0:256], sm[:, 5:6], kvnb[:], ALU.mult, ALU.mult),
                     reads=[ps1b, smb, kvnb_b], writes=[ckvb])
                S.op("dve", lambda e, ckv=ckv, ps1=ps1: e.tensor_copy(ckv[:, 256:320], ps1[:, 256:320]), reads=[ps1b], pwrites=[ckvb])
                S.op("dve", lambda e, ps1=ps1, blk=blk: e.tensor_scalar(wI[:, blk, :], ps1[:, 320:336], 0.25 * 0.125, None, ALU.mult),
                     reads=[ps1b], pwrites=[wI_b])
                tT, tTb = xTblk.get()
                transpose_cols(cq, cqb, 512, lambda k, tT=tT: tT[:, k, :], tTb, F32)
                transpose_cols(ckv, ckvb, 256, lambda k, tT=tT: tT[:, 4 + k, :], tTb, F32, first_full=False)
                pt, ptb = tpfR.get()
                S.op("pe", lambda e, pt=pt, ckv=ckv: e.transpose(pt[0:64, 0:128], ckv[:, 256:320], identf[:]),
                     reads=[ckvb, identf_b], writes=[ptb])
                copy("dve", tT[0:64, 6, :], tTb, pt[0:64, 0:128], ptb, partial=True)
                s1 = aux.setdefault((id(tTb), 1), Buf("s1"))
                s2 = aux.setdefault((id(tTb), 2), Buf("s2"))
                S.dma("pool", cqT_d[:, :, t0:t0 + 128].rearrange("c p t -> p c t"), tT[:, 0:4, :], reads=[tTb], pwrites=[cqT_b], sem_buf=tTb)
                S.dma("pool", ckvT_d[:, :, t0:t0 + 128].rearrange("c p t -> p c t"), tT[:, 4:6, :], reads=[tTb], pwrites=[ckvT_b], sem_buf=s1)
                S.dma("pool", kiT_d[:, t0:t0 + 128], tT[0:64, 6, :], reads=[tTb], pwrites=[kiT_b], sem_buf=s2)
        for g in range(NG):
            cg, cgb = xTg.get()
            S.dma("sp", cg[:, 0:4, :], cqT_d[:, :, g * 512:(g + 1) * 512].rearrange("c p t -> p c t"),
                  reads=[cqT_b], writes=[cgb], sem_buf=cgb)
            s3 = aux.setdefault((id(cgb), 3), Buf("s3"))
            S.dma("sp", cg[:, 4:6, :], ckvT_d[:, :, g * 512:(g + 1) * 512].rearrange("c p t -> p c t"),
                  reads=[ckvT_b], pwrites=[cgb], sem_buf=s3)
            for ct in range(4):
                w, wb = load_w_tile(b_w_uq, ct * 512, 512, nk=4)
                proj_fm(cg, cgb, w, wb, 4, 128, 4, lambda m, ct=ct, g=g: qT_d[ct * 4 + m, :, g * 512:(g + 1) * 512], qT_b)
            for ct in range(2):
                w, wb = load_w_tile(b_w_iq, ct * 512, 512, nk=4)
                proj_fm(cg, cgb, w, wb, 4, 64, 8, lambda m, ct=ct, g=g: qiT_d[ct * 8 + m, :, g * 512:(g + 1) * 512], qiT_b)
            for hq in range(4):
                w, wb = wt.get()
                for hh in range(4):
                    S.dma("pool", w[:, 0:2, hh * 128:(hh + 1) * 128], b_w_uk[hq * 4 + hh].rearrange("(k p) d -> p k d", p=128),
                          sem_buf=wb, **(dict(writes=[wb]) if hh == 0 else dict(pwrites=[wb])))
                proj_fm(cg, cgb, w, wb, 2, 128, 4, lambda m, hq=hq, g=g: kT_d[hq * 4 + m, :, g * 512:(g + 1) * 512], kT_b, k0=4)
                w2, w2b = wt.get()
                for hh in range(4):
                    S.dma("pool", w2[:, 0:2, hh * 128:(hh + 1) * 128], b_w_uv[hq * 4 + hh].rearrange("(k p) d -> p k d", p=128),
                          sem_buf=w2b, **(dict(writes=[w2b]) if hh == 0 else dict(pwrites=[w2b])))
                for tb in range(4):
                    r0 = g * 512 + tb * 128
                    proj_tm(cg, cgb, w2, w2b, 2, 512, tb, v_d[r0:r0 + 128, hq * 512:(hq + 1) * 512], v_b, k0=4)
        S.end_phase()

    def dsa_indexer(kiT_b, qiT_b, mT_b):
        S.begin_phase()
        qiR = Ring(S, "qiR", 2, [64, c.IH, 128], BF16)
        kiAll, kiAll_b = S.sbuf("kiAll", [64, SL], BF16)
        relu = Ring(S, "relu", 3, [128, 512], BF16)
        Irow, Irow_b = S.sbuf("Irow", [128, SL], F32)
        Iwork, Iwork_b = S.sbuf("Iwork", [128, SL], F32)
        Mrow, Mrow_b = S.sbuf("Mrow", [128, SL], BF16)
        m8 = Ring(S, "m8", 2, [128, 8], F32)
        mrow = Ring(S, "mrowi", 2, [128, NB, 128], BF16)
        S.dma("sp", kiAll[:], kiT_d, reads=[kiT_b], writes=[kiAll_b], sem_buf=kiAll_b)
        nrounds = c.IDX_TOPK // 8
        for i in range(NB):
            L = (i + 1) * 128
            qi, qib = qiR.get()
            S.dma("sp", qi[:], qiT_d[:, :, i * 128:(i + 1) * 128].rearrange("h d t -> d h t"), reads=[qiT_b], writes=[qib], sem_buf=qib)
            for s0 in range(0, L, 512):
                sn = min(512, L - s0)
                for hh in range(c.IH):
                    ps, psb = mmR.get()
                    S.op("pe", lambda e, ps=ps, qi=qi, hh=hh, s0=s0, sn=sn: e.matmul(ps[:, 0:sn], qi[:, hh, :], kiAll[:, s0:s0 + sn],
                                                                                      start=True, stop=True),
                         reads=[qib, kiAll_b], writes=[psb])
                    r, rb = relu.get()
                    S.op("act", lambda e, r=r, ps=ps, sn=sn: e.activation(r[:, 0:sn], ps[:, 0:sn], AF.Relu), reads=[psb], writes=[rb])
                    if hh == 0:
                        S.op("dve", lambda e, r=r, i=i, hh=hh, s0=s0, sn=sn: e.tensor_scalar(
                            Irow[:, s0:s0 + sn], r[:, 0:sn], wI[:, i, hh:hh + 1], None, ALU.mult),
                            reads=[rb, wI_b, Irow_b], writes=[Irow_b])
                    else:
                        S.op("dve", lambda e, r=r, i=i, hh=hh, s0=s0, sn=sn: e.scalar_tensor_tensor(
                            Irow[:, s0:s0 + sn], r[:, 0:sn], wI[:, i, hh:hh + 1], Irow[:, s0:s0 + sn], ALU.mult, ALU.add),
                            reads=[rb, wI_b, Irow_b], writes=[Irow_b])
            S.op("dve", lambda e, i=i: e.tensor_tensor(Irow[:, i * 128:(i + 1) * 128], Irow[:, i * 128:(i + 1) * 128], chneg[:], ALU.add),
                 reads=[Irow_b, chneg_b], writes=[Irow_b])
            S.op("dve", lambda e, L=L: e.tensor_copy(Iwork[:, 0:L], Irow[:, 0:L]), reads=[Irow_b], writes=[Iwork_b])
            mx, mxb = m8.get()
            for r_ in range(nrounds):
                S.op("dve", lambda e, mx=mx, L=L: e.max(mx[:], Iwork[:, 0:L]), reads=[Iwork_b], writes=[mxb])
                if r_ < nrounds - 1:
                    S.op("dve", lambda e, mx=mx, L=L: e.match_replace(Iwork[:, 0:L], mx[:], Iwork[:, 0:L], NEG),
                         reads=[mxb, Iwork_b], writes=[Iwork_b])
            sm, smb = small.get()
            S.op("dve", lambda e, sm=sm, mx=mx: e.tensor_tensor(sm[:, 1:2], mx[:, 0:1], mx[:, 7:8], ALU.min), reads=[mxb], writes=[smb])
            S.op("dve", lambda e, sm=sm: e.tensor_scalar_max(sm[:, 0:1], sm[:, 1:2], -1.0e29), reads=[smb], pwrites=[smb])
            S.op("dve", lambda e, sm=sm, L=L: e.tensor_scalar(Mrow[:, 0:L], Irow[:, 0:L], sm[:, 0:1], None, ALU.is_ge),
                 reads=[smb, Irow_b], writes=[Mrow_b])
            mt, mtb = mrow.get()
            transpose_cols(Mrow, Mrow_b, L, lambda k, mt=mt: mt[:, k, :], mtb, BF16)
            S.dma("pool", mT_d[i, 0:i + 1].rearrange("j s t -> s j t"), mt[:, 0:i + 1, :], reads=[mtb], pwrites=[mT_b], sem_buf=mtb)
        S.end_phase()

    out_b = Buf("out")
    for q in range(c.NSEQ):
        cur_d, cur_b = x_in[q], Buf("xin")
        run_depth = getattr(c, "RUN_DEPTH", c.DEPTH)
        for li in range(run_depth):
            xT_b, qT_b, kT_b, v_b, o_b = Buf("xT"), Buf("qT"), Buf("kT"), Buf("v"), Buf("o")
            x1_b, x1T_b, x2_b = Buf("x1"), Buf("x1T"), Buf("x2")
            phase_transpose_in(cur_d, cur_b, xT_d, xT_b)
            if li % 2 == 0:
                fox_projections(xT_b, qT_b, kT_b, v_b)
                attention("fox", qT_b, kT_b, v_b, o_b)
                w_out_ap = a_w_out
            else:
                kiT_b, qiT_b, mT_b = Buf("kiT"), Buf("qiT"), Buf("mT")
                dsa_projections(xT_b, qT_b, kT_b, v_b, kiT_b, qiT_b)
                dsa_indexer(kiT_b, qiT_b, mT_b)
                attention("dsa", qT_b, kT_b, v_b, o_b, mT_b)
                w_out_ap = b_w_out
            outproj_ln_router(li, w_out_ap, cur_d, cur_b, o_b, x1_b, x1T_b)
            if li == run_depth - 1:
                moe_and_ln2(li, x1_b, x1T_b, out[q], out_b)
            else:
                moe_and_ln2(li, x1_b, x1T_b, x2_d, x2_b)
                cur_d, cur_b = x2_d, x2_b
    S.begin_phase()
    S.op("sp", lambda e: e.nop(), reads=[out_b])
    S.end_phase()
    st = S.stats
    S.close()
    return nc, st


def make_consts():
    i = np.arange(128)
    ident = np.eye(128, dtype=np.float32)
    tri = (i[:, None] <= i[None, :]).astype(np.float32)
    ones = np.ones((128, 128), np.float32)
    caus = (i[:, None] <= i[None, :]).astype(np.float32)
    chn = np.where((i[None, :] // 64) <= (i[:, None] // 64), 0.0, NEG).astype(np.float32)
    return np.stack([ident, tri, ones, caus, chn]).astype(np.float32)


def bias_layout(rel_bias):
    s = np.arange(128)[:, None]
    t = np.arange(128)[None, :]
    tiles = []
    for off in (0, 1):
        rel = (s - off * 128) - t
        bk = t5_bucket_np(rel.astype(np.int32))
        tiles.append(np.transpose(rel_bias[bk], (2, 0, 1)))
    return np.ascontiguousarray(np.stack(tiles)).astype(np.float32), np.ascontiguousarray(rel_bias[15:16, :])


_CACHE = {}


def run_cfg(cfg, inputs, core_seqs):
    key = (cfg.S, cfg.E, cfg.TOPK, cfg.IDX_TOPK, cfg.NSEQ, getattr(cfg, 'RUN_DEPTH', 2), getattr(cfg, 'LIMIT', 0))
    if key not in _CACHE:
        _CACHE[key] = build_program(cfg)
    nc, st = _CACHE[key]
    bt, b15 = bias_layout(np.asarray(inputs["rel_bias"], np.float32))
    consts = make_consts()
    shared = {}
    for k in ("a_w_in", "a_b_f", "a_w_out", "b_w_in", "b_q_norm", "b_kv_norm", "b_w_uq", "b_w_iq", "b_w_uk",
              "b_w_uv", "b_w_out"):
        shared[k] = np.ascontiguousarray(np.asarray(inputs[k], np.float32)[0])
    for k in ("ln1_g", "ln1_b", "ln2_g", "ln2_b", "router_w", "router_b", "w_gate", "w_up", "w_down",
              "sh_gate", "sh_up", "sh_down"):
        shared[k] = np.ascontiguousarray(np.asarray(inputs[k], np.float32))
    shared["bias_tiles"] = bt
    shared["bias_far"] = b15
    shared["consts"] = consts
    x = np.asarray(inputs["x"], np.float32)
    in_maps = []
    for seqs in core_seqs:
        m = dict(shared)
        m["x"] = np.ascontiguousarray(x[seqs])
        in_maps.append(m)
    res = run_bass_kernel_spmd(nc, in_maps, core_ids=list(range(len(core_seqs))))
    outp = np.zeros_like(x)
    for ci, seqs in enumerate(core_seqs):
        outp[seqs] = res.results[ci]["out"]
    return outp


def kernel(**inputs):
    x = np.asarray(inputs["x"])
    B, SL, D = x.shape
    E = np.asarray(inputs["router_w"]).shape[-1]
    ncore = 4
    nseq = B // ncore
    cfg = Cfg(S=SL, E=E, TOPK=8, IDX_TOPK=256, NSEQ=nseq)
    core_seqs = [list(range(ci * nseq, (ci + 1) * nseq)) for ci in range(ncore)]
    return run_cfg(cfg, inputs, core_seqs)
```

```python
import math
import numpy as np
from contextlib import ExitStack
import concourse.bass as bass
import concourse.mybir as mybir
from concourse.bass_utils import run_bass_kernel_spmd

F32 = mybir.dt.float32
BF16 = mybir.dt.bfloat16
AF = mybir.ActivationFunctionType
ALU = mybir.AluOpType
AX = mybir.AxisListType

ENG_NAMES = ("pe", "act", "dve", "pool", "sp")
SEM_EPOCH = 30000
NEG = -1.0e30


class Buf:
    __slots__ = ("name", "writers", "readers", "dsem", "dcount", "war")

    def __init__(self, name):
        self.name = name
        self.writers = []
        self.readers = []
        self.war = []
        self.dsem = None
        self.dcount = 0


class Op:
    __slots__ = ("eng", "fn", "deps", "is_dma", "dbuf", "signal", "event")


class Sched:
    def __init__(self, nc):
        self.nc = nc
        self.es = ExitStack()
        self.ops = []
        self._sems = []
        self._n = 0
        self.done = 0
        self.bar = 0
        self.last_eng = {}
        self.dma_last = {}
        self.eng_sems = {e: [] for e in ENG_NAMES}
        self.eng_cnt = {e: 0 for e in ENG_NAMES}
        self.waited = {e: {} for e in ENG_NAMES}
        self.pes = None
        self.uid = 0
        self.stats = {e: [0, 0] for e in ENG_NAMES}
        self.free_dsems = []
        self.live_dbufs = []

    def sbuf(self, name, shape, dtype):
        self.uid += 1
        es = self.pes if self.pes is not None else self.es
        t = es.enter_context(self.nc.sbuf_tensor("%s_%d" % (name, self.uid), list(shape), dtype))
        return t, Buf(name)

    def begin_phase(self):
        assert self.pes is None
        self.pes = ExitStack()
        self.phase_no = getattr(self, "phase_no", 0) + 1
        self.skip = self.phase_no > getattr(self, "limit", 10 ** 9)

    def end_phase(self):
        if self.skip:
            self.pes.close()
            self.pes = None
            return
        self.barrier()
        self.emit()
        self.pes.close()
        self.pes = None

    def barrier(self):
        for x in ENG_NAMES:
            deps = [o for y, o in self.last_eng.items() if y != x and o >= self.bar]
            deps += [o for o in self.dma_last.values() if o >= self.bar]
            op = Op()
            op.eng = x
            op.fn = lambda e: e.nop()
            op.is_dma = False
            op.dbuf = None
            op.signal = False
            op.event = None
            op.deps = sorted(set(deps))
            self.ops.append(op)
        self.bar = len(self.ops)
        self.last_eng = {}
        self.dma_last = {}

    def psum(self, name, shape, dtype):
        t = self.es.enter_context(self.nc.psum_tensor(name, list(shape), dtype))
        return t, Buf(name)

    def sem(self, name):
        s = self.es.enter_context(self.nc.semaphore(name))
        self._sems.append(s)
        return s

    def _record(self, eng, fn, reads, writes, pwrites, is_dma, dbuf):
        if getattr(self, "skip", False):
            return -1
        deps = set()
        for b in reads:
            deps.update(b.writers)
        for b in writes:
            deps.update(b.writers)
            deps.update(b.readers)
        for b in pwrites:
            deps.update(b.readers)
            deps.update(b.war)
        op = Op()
        op.eng = eng
        op.fn = fn
        op.is_dma = is_dma
        op.dbuf = dbuf
        op.signal = False
        op.event = None
        oid = len(self.ops)
        keep = []
        if is_dma:
            self.dma_last[id(dbuf)] = oid
        else:
            self.last_eng[eng] = oid
        for d in deps:
            if d < self.bar:
                continue
            p = self.ops[d]
            if (not p.is_dma) and p.eng == eng and eng in ("pe", "sp"):
                continue
            keep.append(d)
        op.deps = sorted(keep)
        self.ops.append(op)
        for b in reads:
            b.readers.append(oid)
        for b in writes:
            b.war = [d for d in list(b.writers) + list(b.readers) if d >= self.bar]
            b.writers = [oid]
            b.readers = []
        for b in pwrites:
            b.writers.append(oid)
        return oid

    def op(self, eng, fn, reads=(), writes=(), pwrites=()):
        return self._record(eng, fn, reads, writes, pwrites, False, None)

    def dma(self, eng, out, in_, reads=(), writes=(), pwrites=(), sem_buf=None, **kw):
        assert sem_buf is not None

        def fn(e):
            return e.dma_start(out=out, in_=in_, **kw)
        return self._record(eng, fn, reads, writes, pwrites, True, sem_buf)

    def emit(self):
        nc = self.nc
        ops = self.ops
        lo = self.done
        for op in ops[lo:]:
            for d in op.deps:
                assert d >= lo, "dep on an op emitted in an earlier batch"
                if not ops[d].is_dma:
                    ops[d].signal = True
        for op in ops[lo:]:
            if op.is_dma:
                b = op.dbuf
                if b.dsem is None:
                    if self.free_dsems:
                        b.dsem, b.dcount = self.free_dsems.pop()
                    else:
                        b.dsem = self.sem("d%d" % len(self._sems))
                        b.dcount = 0
                    self.live_dbufs.append(b)
                b.dcount += 16
                op.event = (b.dsem, b.dcount)
            elif op.signal:
                e = op.eng
                k = self.eng_cnt[e] // SEM_EPOCH
                if k >= len(self.eng_sems[e]):
                    self.eng_sems[e].append(self.sem("e_%s_%d" % (e, k)))
                self.eng_cnt[e] += 1
                op.event = (self.eng_sems[e][k], self.eng_cnt[e] - k * SEM_EPOCH)
        per_eng = {e: [] for e in ENG_NAMES}
        for op in ops[lo:]:
            per_eng[op.eng].append(op)

        def run_engine(ename, eobj):
            waited = self.waited[ename]
            for op in per_eng[ename]:
                need = {}
                for d in op.deps:
                    s, v = ops[d].event
                    key = id(s)
                    if waited.get(key, 0) >= v:
                        continue
                    if key not in need or need[key][1] < v:
                        need[key] = (s, v)
                for key, (s, v) in need.items():
                    eobj.wait_ge(s, v)
                    waited[key] = v
                    self.stats[ename][1] += 1
                ins = op.fn(eobj)
                self.stats[ename][0] += 1
                if op.event is not None:
                    ins.then_inc(op.event[0], 16 if op.is_dma else 1)
                op.fn = None

        with nc.Block() as block:
            @block.sync
            def _(e):
                run_engine("sp", e)

            @block.scalar
            def _(e):
                run_engine("act", e)

            @block.vector
            def _(e):
                run_engine("dve", e)

            @block.gpsimd
            def _(e):
                run_engine("pool", e)

            @block.tensor
            def _(e):
                run_engine("pe", e)
        self.done = len(ops)
        self.n_sems = len(self._sems)
        for b in self.live_dbufs:
            self.free_dsems.append((b.dsem, b.dcount))
            b.dsem = None
        self.live_dbufs = []
        return self.stats

    def close(self):
        self.es.close()


class Ring:
    def __init__(self, S, name, n, shape, dtype, psum=False):
        self.slots = []
        for i in range(n):
            self.slots.append((S.psum if psum else S.sbuf)("%s%d" % (name, i), shape, dtype))
        self.i = 0

    def get(self):
        s = self.slots[self.i % len(self.slots)]
        self.i += 1
        return s


class Cfg:
    def __init__(self, S=4096, E=64, TOPK=8, IDX_TOPK=256, NSEQ=1, DEPTH=2):
        self.S = S
        self.NB = S // 128
        self.D = 2048
        self.H = 16
        self.E = E
        self.TOPK = TOPK
        self.DE = 512
        self.IDX_TOPK = min(IDX_TOPK, S // 4)
        self.NSEQ = NSEQ
        self.DEPTH = DEPTH
        self.ALPHA = (2 * DEPTH) ** 0.25
        self.QL = 512
        self.KVL = 256
        self.IH = 16
        self.ID = 64
        self.BW = 512 + 256 + 64 + 16


def t5_bucket_np(rel):
    half = 16
    max_exact = 8
    n = np.abs(rel)
    large = max_exact + (np.log(np.maximum(n, 1).astype(np.float32) / max_exact)
                         / math.log(128 / max_exact) * (half - max_exact)).astype(np.int32)
    large = np.minimum(large, half - 1)
    return np.where(rel > 0, half, 0) + np.where(n < max_exact, n, large)


def build_program(cfg, debug_outs=()):
    nc = bass.Bass("TRN2", target_bir_lowering=False)
    S = Sched(nc)
    c = cfg
    S.limit = getattr(cfg, "LIMIT", 10 ** 9)
    NB, D, H, E, SL = c.NB, c.D, c.H, c.E, c.S
    DC = D // 128
    NG = SL // 512

    def din(name, shape, dt=F32):
        return nc.dram_tensor(name, list(shape), dt, kind="ExternalInput").ap()

    def dscr(name, shape, dt):
        return nc.dram_tensor(name, list(shape), dt, kind="Internal").ap()

    x_in = din("x", [c.NSEQ, SL, D])
    a_w_in = din("a_w_in", [D, 3 * D + H])
    a_b_f = din("a_b_f", [1, H])
    a_w_out = din("a_w_out", [D, D])
    b_w_in = din("b_w_in", [D, c.BW])
    b_q_norm = din("b_q_norm", [1, c.QL])
    b_kv_norm = din("b_kv_norm", [1, c.KVL])
    b_w_uq = din("b_w_uq", [c.QL, D])
    b_w_iq = din("b_w_iq", [c.QL, c.IH * c.ID])
    b_w_uk = din("b_w_uk", [H, c.KVL, 128])
    b_w_uv = din("b_w_uv", [H, c.KVL, 128])
    b_w_out = din("b_w_out", [D, D])
    bt_in = din("bias_tiles", [2, H, 128, 128])
    b15_in = din("bias_far", [1, H])
    ln1_g = din("ln1_g", [c.DEPTH, D])
    ln1_b = din("ln1_b", [c.DEPTH, D])
    ln2_g = din("ln2_g", [c.DEPTH, D])
    ln2_b = din("ln2_b", [c.DEPTH, D])
    router_w = din("router_w", [c.DEPTH, D, E])
    router_b = din("router_b", [c.DEPTH, E])
    w_gate = din("w_gate", [c.DEPTH, E, D, c.DE])
    w_up = din("w_up", [c.DEPTH, E, D, c.DE])
    w_down = din("w_down", [c.DEPTH, E, c.DE, D])
    sh_gate = din("sh_gate", [c.DEPTH, D, c.DE])
    sh_up = din("sh_up", [c.DEPTH, D, c.DE])
    sh_down = din("sh_down", [c.DEPTH, c.DE, D])
    consts = din("consts", [5, 128, 128])
    out = nc.dram_tensor("out", [c.NSEQ, SL, D], F32, kind="ExternalOutput").ap()
    dbg = {}
    for nm, shp in debug_outs:
        dbg[nm] = nc.dram_tensor("dbg_" + nm, list(shp), F32, kind="ExternalOutput").ap()

    xT_d = dscr("xT_d", [DC, 128, SL], BF16)
    qT_d = dscr("qT_d", [H, 128, SL], BF16)
    kT_d = dscr("kT_d", [H, 128, SL], BF16)
    v_d = dscr("v_d", [SL, D], BF16)
    o_d = dscr("o_d", [SL, D], BF16)
    x1_d = dscr("x1_d", [SL, D], F32)
    x1T_d = dscr("x1T_d", [DC, 128, SL], BF16)
    x2_d = dscr("x2_d", [SL, D], F32)
    cqT_d = dscr("cqT_d", [4, 128, SL], BF16)
    ckvT_d = dscr("ckvT_d", [2, 128, SL], BF16)
    kiT_d = dscr("kiT_d", [64, SL], BF16)
    qiT_d = dscr("qiT_d", [c.IH, 64, SL], BF16)
    mT_d = dscr("mT_d", [NB, NB, 128, 128], BF16)

    identf, identf_b = S.sbuf("identf", [128, 128], F32)
    identb, identb_b = S.sbuf("identb", [128, 128], BF16)
    trif, trif_b = S.sbuf("trif", [128, 128], F32)
    onesf, onesf_b = S.sbuf("onesf", [128, 128], F32)
    causb, causb_b = S.sbuf("causb", [128, 128], BF16)
    chneg, chneg_b = S.sbuf("chneg", [128, 128], F32)
    epsb, epsb_b = S.sbuf("epsb", [128, 4], F32)
    logf, logf_b = S.sbuf("logf", [128, NB, H], F32)
    Fs, Fs_b = S.sbuf("Fs", [128, NB, H], F32)
    Fend, Fend_b = S.sbuf("Fend", [128, NB + 1, H], F32)
    G_all, G_b = S.sbuf("G_all", [128, NB, E + 1], F32)
    wI, wI_b = S.sbuf("wI", [128, NB, c.IH], F32)
    bfb, bfb_b = S.sbuf("bfb", [128, H], F32)
    b15b, b15b_b = S.sbuf("b15b", [128, H], F32)
    lng, lng_b = S.sbuf("lng", [128, D], F32)
    lnb, lnb_b = S.sbuf("lnb", [128, D], F32)
    small = Ring(S, "small", 6, [128, 64], F32)
    stats = Ring(S, "stats", 2, [128, 4, 6], F32)
    obf = Ring(S, "obf", 3, [128, 512], BF16)

    mmR = Ring(S, "pmm", 4, [128, 512], F32, psum=True)
    tpfR = Ring(S, "ptf", 1, [128, 512], F32, psum=True)
    tpbR = Ring(S, "ptb", 1, [128, 1024], BF16, psum=True)
    poR = Ring(S, "ppo", 2, [128, 512], F32, psum=True)

    S.begin_phase()
    S.dma("sp", identf[:], consts[0], writes=[identf_b], sem_buf=identf_b)
    S.dma("pool", identb[:], consts[0], writes=[identb_b], sem_buf=identb_b)
    S.dma("sp", trif[:], consts[1], writes=[trif_b], sem_buf=trif_b)
    S.dma("sp", onesf[:], consts[2], writes=[onesf_b], sem_buf=onesf_b)
    S.dma("pool", causb[:], consts[3], writes=[causb_b], sem_buf=causb_b)
    S.dma("sp", chneg[:], consts[4], writes=[chneg_b], sem_buf=chneg_b)
    S.dma("sp", bfb[:], bass.AP(a_b_f.tensor, a_b_f.offset, [[0, 128], [1, H]]), writes=[bfb_b], sem_buf=bfb_b)
    S.dma("sp", b15b[:], bass.AP(b15_in.tensor, b15_in.offset, [[0, 128], [1, H]]), writes=[b15b_b], sem_buf=b15b_b)
    S.op("dve", lambda e: e.memset(epsb[:, 0:1], 1e-5), writes=[epsb_b])
    S.op("dve", lambda e: e.memset(epsb[:, 1:2], 1e-6), pwrites=[epsb_b])
    S.op("dve", lambda e: e.memset(epsb[:, 2:3], 1.0), pwrites=[epsb_b])
    S.op("dve", lambda e: e.memset(epsb[:, 3:4], 0.0), pwrites=[epsb_b])
    S.end_phase()

    rr = {"i": 0}

    def bcast_rows(ap_row, n):
        return bass.AP(ap_row.tensor, ap_row.offset, [[0, 128], [1, n]])

    def evac_eng():
        return "dve"

    def copy(eng, o, ob, i, ib, partial=False):
        w = dict(pwrites=[ob]) if partial else dict(writes=[ob])
        if eng == "act":
            S.op("act", lambda e: e.copy(o, i), reads=[ib], **w)
        else:
            S.op(eng, lambda e: e.tensor_copy(o, i), reads=[ib], **w)

    def mm_chain(ps, psb, pairs, reads):
        n = len(pairs)
        for k, (l, r) in enumerate(pairs):
            S.op("pe", lambda e, l=l, r=r, k=k: e.matmul(ps, l, r, start=(k == 0), stop=(k == n - 1)),
                 reads=reads, writes=[psb] if (k == 0 or k == n - 1) else ())

    def transpose_cols(src, srcb, ncols, dst_fn, dstb, dt, first_full=True):
        nchunk = ncols // 128
        per = 4 if dt == F32 else 8
        first = first_full
        for k0 in range(0, nchunk, per):
            kn = min(per, nchunk - k0)
            pt, ptb = (tpfR if dt == F32 else tpbR).get()
            idt, idb = (identf, identf_b) if dt == F32 else (identb, identb_b)
            for k in range(kn):
                S.op("pe", lambda e, k=k, k0=k0, pt=pt, idt=idt: e.transpose(
                    pt[:, k * 128:(k + 1) * 128], src[:, (k0 + k) * 128:(k0 + k + 1) * 128], idt[:]),
                    reads=[srcb, idb], writes=[ptb] if (k == 0 or k == kn - 1) else ())
            for k in range(kn):
                copy(evac_eng(), dst_fn(k0 + k), dstb, pt[:, k * 128:(k + 1) * 128], ptb, partial=not first)
                first = False

    def layer_norm_rows(y, yb):
        st, stb = stats.get()
        for k in range(4):
            S.op("dve", lambda e, k=k: e.bn_stats(st[:, k, :], y[:, k * 512:(k + 1) * 512]),
                 reads=[yb], writes=[stb] if k == 0 else (), pwrites=[stb] if k else ())
        sm, smb = small.get()
        S.op("dve", lambda e: e.bn_aggr(sm[:, 0:2], st[:].rearrange("p a b -> p (a b)")), reads=[stb], writes=[smb])
        S.op("act", lambda e: e.activation(sm[:, 2:3], sm[:, 1:2], AF.Sqrt, bias=epsb[:, 0:1], scale=1.0),
             reads=[smb, epsb_b], pwrites=[smb])
        S.op("dve", lambda e: e.reciprocal(sm[:, 3:4], sm[:, 2:3]), reads=[smb], pwrites=[smb])
        S.op("dve", lambda e: e.tensor_scalar(y[:], y[:], sm[:, 0:1], sm[:, 3:4], ALU.subtract, ALU.mult),
             reads=[smb, yb], writes=[yb])
        S.op("pool", lambda e: e.tensor_tensor(y[:], y[:], lng[:], ALU.mult), reads=[yb, lng_b], writes=[yb])
        S.op("pool", lambda e: e.tensor_tensor(y[:], y[:], lnb[:], ALU.add), reads=[yb, lnb_b], writes=[yb])

    def load_ln(gsrc, bsrc, li):
        S.dma("sp", lng[:], bcast_rows(gsrc[li:li + 1, :], D), writes=[lng_b], sem_buf=lng_b)
        S.dma("sp", lnb[:], bcast_rows(bsrc[li:li + 1, :], D), writes=[lnb_b], sem_buf=lnb_b)

    def phase_transpose_in(src_d, src_b, dstT_d, dstT_b):
        S.begin_phase()
        xrow = Ring(S, "xrow", 2, [128, D], F32)
        xTblk = Ring(S, "xTblk", 2, [128, DC, 128], BF16)
        for b in range(NB):
            xr, xrb = xrow.get()
            S.dma("sp", xr[:], src_d[b * 128:(b + 1) * 128, :], reads=[src_b], writes=[xrb], sem_buf=xrb)
            xt, xtb = xTblk.get()
            import os
            if os.environ.get("DBG_P2", "") == "load":
                continue
            transpose_cols(xr, xrb, D, lambda k, xt=xt: xt[:, k, :], xtb, F32)
            v = os.environ.get("DBG_ST", "pool")
            if v != "none":
                S.dma(v, dstT_d[:, :, b * 128:(b + 1) * 128].rearrange("c p t -> p c t"), xt[:],
                      reads=[xtb], pwrites=[dstT_b], sem_buf=xtb)
        S.end_phase()

    def mk_load_w(wt):
        def load_w_tile(wsrc, col0, ncol, nk=DC):
            w, wb = wt.get()
            S.dma("pool", w[:, 0:nk, 0:ncol], wsrc[:, col0:col0 + ncol].rearrange("(c p) n -> p c n", p=128),
                  writes=[wb], sem_buf=wb)
            return w, wb
        return load_w_tile

    def proj_fm(xg, xgb, w, wb, nk, mw, nm, dst_fn, dst_b, k0=0):
        for m in range(nm):
            ps, psb = mmR.get()
            mm_chain(ps[0:mw, :], psb, [(w[:, k, m * mw:(m + 1) * mw], xg[:, k0 + k, :]) for k in range(nk)], [xgb, wb])
            ob_, obb = obf.get()
            copy(evac_eng(), ob_[0:mw, :], obb, ps[0:mw, :], psb)
            S.dma("sp", dst_fn(m), ob_[0:mw, :], reads=[obb], pwrites=[dst_b], sem_buf=obb)

    def proj_tm(xg, xgb, w, wb, nk, ncol, tb, dst_ap, dst_b, k0=0):
        ps, psb = mmR.get()
        mm_chain(ps[:, 0:ncol], psb, [(xg[:, k0 + k, tb * 128:(tb + 1) * 128], w[:, k, 0:ncol]) for k in range(nk)], [xgb, wb])
        ob_, obb = obf.get()
        copy(evac_eng(), ob_[:, 0:ncol], obb, ps[:, 0:ncol], psb)
        S.dma("sp", dst_ap, ob_[:, 0:ncol], reads=[obb], pwrites=[dst_b], sem_buf=obb)

    def fox_projections(xT_b, qT_b, kT_b, v_b):
        S.begin_phase()
        xTg = Ring(S, "xTg", 2, [128, DC, 512], BF16)
        wt = Ring(S, "wt", 3, [128, DC, 512], BF16)
        load_w_tile = mk_load_w(wt)
        for g in range(NG):
            xg, xgb = xTg.get()
            S.dma("sp", xg[:], xT_d[:, :, g * 512:(g + 1) * 512].rearrange("c p t -> p c t"),
                  reads=[xT_b], writes=[xgb], sem_buf=xgb)
            for ct in range(8):
                w, wb = load_w_tile(a_w_in, ct * 512, 512)
                dstT, dstb = (qT_d, qT_b) if ct < 4 else (kT_d, kT_b)
                h0 = (ct % 4) * 4
                proj_fm(xg, xgb, w, wb, DC, 128, 4,
                        lambda m, dstT=dstT, h0=h0, g=g: dstT[h0 + m, :, g * 512:(g + 1) * 512], dstb)
            for ct in range(8, 12):
                w, wb = load_w_tile(a_w_in, ct * 512, 512)
                for tb in range(4):
                    r0 = g * 512 + tb * 128
                    proj_tm(xg, xgb, w, wb, DC, 512, tb, v_d[r0:r0 + 128, (ct - 8) * 512:(ct - 7) * 512], v_b)
            w, wb = load_w_tile(a_w_in, 3 * D, H)
            for tb in range(4):
                blk = g * 4 + tb
                ps, psb = poR.get()
                mm_chain(ps[:, 0:H], psb, [(xg[:, k, tb * 128:(tb + 1) * 128], w[:, k, 0:H]) for k in range(DC)], [xgb, wb])
                sm, smb = small.get()
                S.op("dve", lambda e, sm=sm, ps=ps: e.tensor_tensor(sm[:, 0:16], ps[:, 0:H], bfb[:], ALU.add),
                     reads=[psb, bfb_b], writes=[smb])
                S.op("act", lambda e, sm=sm: e.activation(sm[:, 16:32], sm[:, 0:16], AF.Abs),
                     reads=[smb], pwrites=[smb])
                S.op("act", lambda e, sm=sm: e.activation(sm[:, 32:48], sm[:, 16:32], AF.Exp, scale=-1.0),
                     reads=[smb], pwrites=[smb])
                S.op("act", lambda e, sm=sm: e.activation(sm[:, 32:48], sm[:, 32:48], AF.Ln, bias=epsb[:, 2:3], scale=1.0),
                     reads=[smb, epsb_b], pwrites=[smb])
                S.op("dve", lambda e, sm=sm: e.tensor_scalar_min(sm[:, 48:64], sm[:, 0:16], 0.0), reads=[smb], pwrites=[smb])
                S.op("dve", lambda e, sm=sm, blk=blk: e.tensor_tensor(logf[:, blk, :], sm[:, 48:64], sm[:, 32:48], ALU.subtract),
                     reads=[smb], pwrites=[logf_b])
        S.op("dve", lambda e: e.memset(Fend[:, 0, :], 0.0), reads=[logf_b], writes=[Fend_b])
        for b in range(NB):
            ps, psb = poR.get()
            S.op("pe", lambda e, ps=ps, b=b: e.matmul(ps[:, 0:H], trif[:], logf[:, b, :], start=True, stop=True),
                 reads=[trif_b, logf_b], writes=[psb])
            S.op("pe", lambda e, ps=ps, b=b: e.matmul(ps[:, 32:32 + H], onesf[:], logf[:, b, :], start=True, stop=True),
                 reads=[onesf_b, logf_b], pwrites=[psb])
            S.op("dve", lambda e, ps=ps, b=b: e.tensor_tensor(Fs[:, b, :], ps[:, 0:H], Fend[:, b, :], ALU.add),
                 reads=[psb, Fend_b], pwrites=[Fs_b])
            S.op("dve", lambda e, ps=ps, b=b: e.tensor_tensor(Fend[:, b + 1, :], ps[:, 32:32 + H], Fend[:, b, :], ALU.add),
                 reads=[psb], pwrites=[Fend_b])
        S.end_phase()

    def attention(mode, qT_b, kT_b, v_b, o_b, mT_b=None):
        S.begin_phase()
        scale = 128 ** -0.5
        kTh = Ring(S, "kTh", 2, [128, SL], BF16)
        qTh = Ring(S, "qTh", 2, [128, SL], BF16)
        vh = Ring(S, "vh", 2, [128, NB, 132], BF16)
        pbf = Ring(S, "pbf", 4, [128, 128], BF16)
        biasij = Ring(S, "biasij", 4, [128, H], F32)
        if mode == "dsa":
            pf32 = Ring(S, "pf32", 2, [128, 128], F32)
            mrow = Ring(S, "mrow", 2, [128, NB, 128], BF16)
            btile, btile_b = S.sbuf("btile", [128, 2, H, 128], F32)
            S.dma("sp", btile[:], bt_in.rearrange("o h s t -> s o h t"), writes=[btile_b], sem_buf=btile_b)
        for h in range(H):
            kt, ktb = kTh.get()
            qt, qtb = qTh.get()
            vv, vvb = vh.get()
            S.dma("sp", kt[:], kT_d[h], reads=[kT_b], writes=[ktb], sem_buf=ktb)
            S.dma("sp", qt[:], qT_d[h], reads=[qT_b], writes=[qtb], sem_buf=qtb)
            S.dma("sp", vv[:, :, 0:128], v_d[:, h * 128:(h + 1) * 128].rearrange("(b p) d -> p b d", p=128),
                  reads=[v_b], writes=[vvb], sem_buf=vvb)
            S.op("pool", lambda e, vv=vv: e.memset(vv[:, :, 128:129], 1.0), reads=[vvb], pwrites=[vvb])
            for i in range(NB):
                po, pob = poR.get()
                if mode == "dsa":
                    mr, mrb = mrow.get()
                    S.dma("sp", mr[:, 0:i + 1, :], mT_d[i, 0:i + 1].rearrange("j s t -> s j t"),
                          reads=[mT_b], writes=[mrb], sem_buf=mrb)
                for j in range(i + 1):
                    ps, psb = mmR.get()
                    S.op("pe", lambda e, ps=ps, kt=kt, qt=qt, i=i, j=j: e.matmul(
                        ps[:, 0:128], kt[:, j * 128:(j + 1) * 128], qt[:, i * 128:(i + 1) * 128], start=True, stop=True),
                        reads=[ktb, qtb], writes=[psb])
                    p, pb = pbf.get()
                    if mode == "fox":
                        bi, bib = biasij.get()
                        S.op("dve", lambda e, bi=bi, i=i, j=j: e.tensor_tensor(bi[:], Fend[:, i + 1, :], Fs[:, j, :], ALU.subtract),
                             reads=[Fend_b, Fs_b], writes=[bib])
                        S.op("act", lambda e, p=p, ps=ps, bi=bi, h=h: e.activation(p[:], ps[:, 0:128], AF.Exp,
                                                                                   bias=bi[:, h:h + 1], scale=scale),
                             reads=[psb, bib], writes=[pb])
                        if j == i:
                            S.op("dve", lambda e, p=p: e.tensor_tensor(p[:], p[:], causb[:], ALU.mult),
                                 reads=[pb, causb_b], writes=[pb])
                    else:
                        if j >= i - 1:
                            tf, tfb = pf32.get()
                            S.op("dve", lambda e, tf=tf, ps=ps, i=i, j=j, h=h: e.scalar_tensor_tensor(
                                tf[:], ps[:, 0:128], scale, btile[:, i - j, h, :], ALU.mult, ALU.add),
                                reads=[psb, btile_b], writes=[tfb])
                            S.op("act", lambda e, p=p, tf=tf: e.activation(p[:], tf[:], AF.Exp), reads=[tfb], writes=[pb])
                        else:
                            S.op("act", lambda e, p=p, ps=ps, h=h: e.activation(p[:], ps[:, 0:128], AF.Exp,
                                                                                bias=b15b[:, h:h + 1], scale=scale),
                                 reads=[psb, b15b_b], writes=[pb])
                        S.op("pool", lambda e, p=p, mr=mr, j=j: e.tensor_tensor(p[:], p[:], mr[:, j, :], ALU.mult),
                             reads=[pb, mrb], writes=[pb])
                    S.op("pe", lambda e, po=po, p=p, vv=vv, i=i, j=j: e.matmul(po[:, 0:129], p[:], vv[:, j, 0:129],
                                                                              start=(j == 0), stop=(j == i)),
                         reads=[pb, vvb], writes=[pob] if j == 0 else (), pwrites=[pob] if j > 0 else ())
                sm, smb = small.get()
                S.op("dve", lambda e, sm=sm, po=po: e.reciprocal(sm[:, 0:1], po[:, 128:129]), reads=[pob], writes=[smb])
                ob_, obb = obf.get()
                S.op("dve", lambda e, ob_=ob_, po=po, sm=sm: e.tensor_scalar(ob_[:, 0:128], po[:, 0:128], sm[:, 0:1], None, ALU.mult),
                     reads=[pob, smb], writes=[obb])
                S.dma("sp", o_d[i * 128:(i + 1) * 128, h * 128:(h + 1) * 128], ob_[:, 0:128],
                      reads=[obb], pwrites=[o_b], sem_buf=obb)
        S.end_phase()

    def outproj_ln_router(li, w_out_ap, xres_d, xres_b, o_b, x1_b, x1T_b):
        S.begin_phase()
        wt = Ring(S, "wt", 4, [128, DC, 512], BF16)
        load_w_tile = mk_load_w(wt)
        wres_tiles = [load_w_tile(w_out_ap, n * 512, 512) for n in range(4)]
        obfw = Ring(S, "obfw", 2, [128, D], BF16)
        xTblk = Ring(S, "xTblk", 2, [128, DC, 128], BF16)
        xTf32 = Ring(S, "xTf32", 1, [128, DC, 128], F32)
        xrow = Ring(S, "xrow", 2, [128, D], F32)
        yrow = Ring(S, "yrow", 2, [128, D], F32)
        smallw = Ring(S, "smallw", 2, [128, 2 * E + 16], F32)
        rwt, rwt_b = S.sbuf("rwt", [128, DC, E], F32)
        rbb, rbb_b = S.sbuf("rbb", [128, E], F32)
        load_ln(ln1_g, ln1_b, li)
        S.dma("sp", rwt[:], router_w[li].rearrange("(c p) n -> p c n", p=128), writes=[rwt_b], sem_buf=rwt_b)
        S.dma("sp", rbb[:], bcast_rows(router_b[li:li + 1, :], E), writes=[rbb_b], sem_buf=rbb_b)
        for b in range(NB):
            r0 = b * 128
            orow, orowb = obfw.get()
            S.dma("sp", orow[:], o_d[r0:r0 + 128, :], reads=[o_b], writes=[orowb], sem_buf=orowb)
            oT, oTb = xTblk.get()
            transpose_cols(orow, orowb, D, lambda k, oT=oT: oT[:, k, :], oTb, BF16)
            xr, xrb = xrow.get()
            S.dma("sp", xr[:], xres_d[r0:r0 + 128, :], reads=[xres_b], writes=[xrb], sem_buf=xrb)
            y, yb = yrow.get()
            for n in range(4):
                w, wb = wres_tiles[n]
                ps, psb = mmR.get()
                mm_chain(ps[:], psb, [(oT[:, k, :], w[:, k, :]) for k in range(DC)], [oTb, wb])
                S.op("dve", lambda e, y=y, xr=xr, ps=ps, n=n: e.scalar_tensor_tensor(
                    y[:, n * 512:(n + 1) * 512], xr[:, n * 512:(n + 1) * 512], c.ALPHA, ps[:], ALU.mult, ALU.add),
                    reads=[xrb, psb], writes=[yb] if n == 0 else (), pwrites=[yb] if n else ())
            layer_norm_rows(y, yb)
            S.dma("sp", x1_d[r0:r0 + 128, :], y[:], reads=[yb], pwrites=[x1_b], sem_buf=yb)
            xTf, xTfb = xTf32.get()
            transpose_cols(y, yb, D, lambda k, xTf=xTf: xTf[:, k, :], xTfb, F32)
            xt, xtb = xTblk.get()
            S.op("pool", lambda e, xt=xt, xTf=xTf: e.tensor_copy(xt[:], xTf[:]), reads=[xTfb], writes=[xtb])
            S.dma("pool", x1T_d[:, :, r0:r0 + 128].rearrange("c p t -> p c t"), xt[:], reads=[xtb], pwrites=[x1T_b], sem_buf=xtb)
            ps, psb = poR.get()
            mm_chain(ps[:, 0:E], psb, [(xTf[:, k, :], rwt[:, k, :]) for k in range(DC)], [xTfb, rwt_b])
            sm, smb = smallw.get()
            S.op("act", lambda e, sm=sm, ps=ps: e.activation(sm[:, 0:E], ps[:, 0:E], AF.Sigmoid), reads=[psb], writes=[smb])
            S.op("dve", lambda e, sm=sm: e.tensor_tensor(sm[:, E:2 * E], sm[:, 0:E], rbb[:], ALU.add), reads=[smb, rbb_b], pwrites=[smb])
            S.op("dve", lambda e, sm=sm: e.max(sm[:, 2 * E:2 * E + 8], sm[:, E:2 * E]), reads=[smb], pwrites=[smb])
            assert c.TOPK == 8
            S.op("dve", lambda e, sm=sm: e.tensor_tensor(sm[:, 2 * E + 10:2 * E + 11], sm[:, 2 * E:2 * E + 1], sm[:, 2 * E + 7:2 * E + 8], ALU.min),
                 reads=[smb], pwrites=[smb])
            S.op("dve", lambda e, sm=sm: e.tensor_scalar(sm[:, E:2 * E], sm[:, E:2 * E], sm[:, 2 * E + 10:2 * E + 11], None, ALU.is_ge),
                 reads=[smb], pwrites=[smb])
            S.op("dve", lambda e, sm=sm: e.tensor_tensor(sm[:, 0:E], sm[:, 0:E], sm[:, E:2 * E], ALU.mult), reads=[smb], pwrites=[smb])
            S.op("dve", lambda e, sm=sm: e.reduce_sum(sm[:, 2 * E + 8:2 * E + 9], sm[:, 0:E], axis=AX.X), reads=[smb], pwrites=[smb])
            S.op("dve", lambda e, sm=sm: e.reciprocal(sm[:, 2 * E + 9:2 * E + 10], sm[:, 2 * E + 8:2 * E + 9]), reads=[smb], pwrites=[smb])
            S.op("dve", lambda e, sm=sm: e.tensor_scalar(sm[:, 0:E], sm[:, 0:E], sm[:, 2 * E + 9:2 * E + 10], None, ALU.mult),
                 reads=[smb], pwrites=[smb])
            S.op("dve", lambda e, sm=sm, b=b: e.tensor_scalar(G_all[:, b, 0:E], sm[:, 0:E], 2.5, None, ALU.mult),
                 reads=[smb], pwrites=[G_b])
            S.op("dve", lambda e, b=b: e.memset(G_all[:, b, E:E + 1], 1.0), pwrites=[G_b])
        import os
        if os.environ.get("DBG_G"):
            S.dma("sp", out[0][0:128, 0:NB * (E + 1)], G_all[:].rearrange("p b e -> p (b e)"), reads=[G_b], pwrites=[out_b], sem_buf=G_b)
        S.end_phase()

    def moe_and_ln2(li, x1_b, x1T_b, dst_d, dst_b):
        S.begin_phase()
        xTg = Ring(S, "xTg", 1, [128, DC, 512], BF16)
        wt = Ring(S, "wt", 3, [128, DC, 512], BF16)
        load_w_tile = mk_load_w(wt)
        wdn = Ring(S, "wdn", 1, [128, 4, D], BF16)
        hT = Ring(S, "hT", 2, [128, 4, 512], BF16)
        sg = Ring(S, "sg", 2, [128, 512], F32)
        acc, acc_b = S.sbuf("acc", [128, 4, D], F32)
        xrow = Ring(S, "xrow", 2, [128, D], F32)
        load_ln(ln2_g, ln2_b, li)
        for g in range(NG):
            xg, xgb = xTg.get()
            S.dma("sp", xg[:], x1T_d[:, :, g * 512:(g + 1) * 512].rearrange("c p t -> p c t"),
                  reads=[x1T_b], writes=[xgb], sem_buf=xgb)
            for ei in range(E + 1):
                if ei < E:
                    wgs, wus, wds = w_gate[li, ei], w_up[li, ei], w_down[li, ei]
                else:
                    wgs, wus, wds = sh_gate[li], sh_up[li], sh_down[li]
                wg, wgb = load_w_tile(wgs, 0, 512)
                wu, wub = load_w_tile(wus, 0, 512)
                wd, wdb = wdn.get()
                S.dma("pool", wd[:], wds.rearrange("(c p) n -> p c n", p=128), writes=[wdb], sem_buf=wdb)
                ht, htb = hT.get()
                for m in range(4):
                    psg, psgb = mmR.get()
                    mm_chain(psg[:], psgb, [(wg[:, k, m * 128:(m + 1) * 128], xg[:, k, :]) for k in range(DC)], [xgb, wgb])
                    psu, psub = mmR.get()
                    mm_chain(psu[:], psub, [(wu[:, k, m * 128:(m + 1) * 128], xg[:, k, :]) for k in range(DC)], [xgb, wub])
                    s_, sb_ = sg.get()
                    S.op("act", lambda e, s_=s_, psg=psg: e.activation(s_[:], psg[:], AF.Silu), reads=[psgb], writes=[sb_])
                    S.op("dve", lambda e, ht=ht, s_=s_, psu=psu, m=m: e.tensor_tensor(ht[:, m, :], s_[:], psu[:], ALU.mult),
                         reads=[sb_, psub], writes=[htb] if m == 0 else (), pwrites=[htb] if m else ())
                for tb in range(4):
                    blk = g * 4 + tb
                    for n in range(4):
                        ps, psb = mmR.get()
                        mm_chain(ps[:], psb, [(ht[:, m, tb * 128:(tb + 1) * 128], wd[:, m, n * 512:(n + 1) * 512]) for m in range(4)],
                                 [htb, wdb])
                        a = acc[:, tb, n * 512:(n + 1) * 512]
                        if ei == 0:
                            S.op("dve", lambda e, a=a, ps=ps, blk=blk, ei=ei: e.tensor_scalar(a, ps[:], G_all[:, blk, ei:ei + 1], None, ALU.mult),
                                 reads=[psb, G_b], pwrites=[acc_b])
                        else:
                            S.op("dve", lambda e, a=a, ps=ps, blk=blk, ei=ei: e.scalar_tensor_tensor(
                                a, ps[:], G_all[:, blk, ei:ei + 1], a, ALU.mult, ALU.add),
                                reads=[psb, G_b, acc_b], pwrites=[acc_b])
            for tb in range(4):
                r0 = g * 512 + tb * 128
                xr, xrb = xrow.get()
                S.dma("sp", xr[:], x1_d[r0:r0 + 128, :], reads=[x1_b], writes=[xrb], sem_buf=xrb)
                S.op("dve", lambda e, xr=xr, tb=tb: e.scalar_tensor_tensor(xr[:], xr[:], c.ALPHA, acc[:, tb, :], ALU.mult, ALU.add),
                     reads=[xrb, acc_b], writes=[xrb])
                layer_norm_rows(xr, xrb)
                S.dma("sp", dst_d[r0:r0 + 128, :], xr[:], reads=[xrb], pwrites=[dst_b], sem_buf=xrb)
        S.end_phase()

    def dsa_projections(xT_b, qT_b, kT_b, v_b, kiT_b, qiT_b):
        cqT_b, ckvT_b = Buf("cqT"), Buf("ckvT")
        aux = {}
        S.begin_phase()
        xTg = Ring(S, "xTg", 2, [128, DC, 512], BF16)
        wt = Ring(S, "wt", 3, [128, DC, 512], BF16)
        load_w_tile = mk_load_w(wt)
        of32 = Ring(S, "of32", 4, [128, 512], F32)
        sg = Ring(S, "sg", 2, [128, 512], F32)
        xTblk = Ring(S, "xTblk", 2, [128, 8, 128], BF16)
        qnb, qnb_b = S.sbuf("qnb", [128, c.QL], F32)
        kvnb, kvnb_b = S.sbuf("kvnb", [128, c.KVL], F32)
        S.dma("sp", qnb[:], bcast_rows(b_q_norm, c.QL), writes=[qnb_b], sem_buf=qnb_b)
        S.dma("sp", kvnb[:], bcast_rows(b_kv_norm, c.KVL), writes=[kvnb_b], sem_buf=kvnb_b)
        for g in range(NG):
            xg, xgb = xTg.get()
            S.dma("sp", xg[:], xT_d[:, :, g * 512:(g + 1) * 512].rearrange("c p t -> p c t"),
                  reads=[xT_b], writes=[xgb], sem_buf=xgb)
            w0, w0b = load_w_tile(b_w_in, 0, 512)
            w1, w1b = load_w_tile(b_w_in, 512, c.BW - 512)
            for tb in range(4):
                blk = g * 4 + tb
                t0 = blk * 128
                ps0, ps0b = mmR.get()
                mm_chain(ps0[:], ps0b, [(xg[:, k, tb * 128:(tb + 1) * 128], w0[:, k, :]) for k in range(DC)], [xgb, w0b])
                ps1, ps1b = mmR.get()
                mm_chain(ps1[:, 0:336], ps1b, [(xg[:, k, tb * 128:(tb + 1) * 128], w1[:, k, 0:336]) for k in range(DC)], [xgb, w1b])
                cq, cqb = of32.get()
                ckv, ckvb = of32.get()
                sm, smb = small.get()
                sq, sqb = sg.get()
                S.op("act", lambda e, sq=sq, ps0=ps0, sm=sm: e.activation(sq[:], ps0[:], AF.Square, accum_out=sm[:, 0:1]),
                     reads=[ps0b], writes=[sqb, smb])
                S.op("act", lambda e, sq=sq, ps1=ps1, sm=sm: e.activation(sq[:, 0:256], ps1[:, 0:256], AF.Square, accum_out=sm[:, 1:2]),
                     reads=[ps1b], writes=[sqb], pwrites=[smb])
                S.op("act", lambda e, sm=sm: e.activation(sm[:, 2:3], sm[:, 0:1], AF.Sqrt, bias=epsb[:, 1:2], scale=1.0 / c.QL),
                     reads=[smb, epsb_b], pwrites=[smb])
                S.op("act", lambda e, sm=sm: e.activation(sm[:, 3:4], sm[:, 1:2], AF.Sqrt, bias=epsb[:, 1:2], scale=1.0 / c.KVL),
                     reads=[smb, epsb_b], pwrites=[smb])
                S.op("dve", lambda e, sm=sm: e.reciprocal(sm[:, 4:6], sm[:, 2:4]), reads=[smb], pwrites=[smb])
                S.op("dve", lambda e, cq=cq, ps0=ps0, sm=sm: e.scalar_tensor_tensor(cq[:], ps0[:], sm[:, 4:5], qnb[:], ALU.mult, ALU.mult),
                     reads=[ps0b, smb, qnb_b], writes=[cqb])
                S.op("dve", lambda e, ckv=ckv, ps1=ps1, sm=sm: e.scalar_tensor_tensor(ckv[:, 0:256], ps1[:, 0:256], sm[:, 5:6], kvnb[:], ALU.mult, ALU.mult),
                     reads=[ps1b, smb, kvnb_b], writes=[ckvb])
                S.op("dve", lambda e, ckv=ckv, ps1=ps1: e.tensor_copy(ckv[:, 256:320], ps1[:, 256:320]), reads=[ps1b], pwrites=[ckvb])
                S.op("dve", lambda e, ps1=ps1, blk=blk: e.tensor_scalar(wI[:, blk, :], ps1[:, 320:336], 0.25 * 0.125, None, ALU.mult),
                     reads=[ps1b], pwrites=[wI_b])
                tT, tTb = xTblk.get()
                transpose_cols(cq, cqb, 512, lambda k, tT=tT: tT[:, k, :], tTb, F32)
                transpose_cols(ckv, ckvb, 256, lambda k, tT=tT: tT[:, 4 + k, :], tTb, F32, first_full=False)
                pt, ptb = tpfR.get()
                S.op("pe", lambda e, pt=pt, ckv=ckv: e.transpose(pt[0:64, 0:128], ckv[:, 256:320], identf[:]),
                     reads=[ckvb, identf_b], writes=[ptb])
                copy("dve", tT[0:64, 6, :], tTb, pt[0:64, 0:128], ptb, partial=True)
                s1 = aux.setdefault((id(tTb), 1), Buf("s1"))
                s2 = aux.setdefault((id(tTb), 2), Buf("s2"))
                S.dma("pool", cqT_d[:, :, t0:t0 + 128].rearrange("c p t -> p c t"), tT[:, 0:4, :], reads=[tTb], pwrites=[cqT_b], sem_buf=tTb)
                S.dma("pool", ckvT_d[:, :, t0:t0 + 128].rearrange("c p t -> p c t"), tT[:, 4:6, :], reads=[tTb], pwrites=[ckvT_b], sem_buf=s1)
                S.dma("pool", kiT_d[:, t0:t0 + 128], tT[0:64, 6, :], reads=[tTb], pwrites=[kiT_b], sem_buf=s2)
        for g in range(NG):
            cg, cgb = xTg.get()
            S.dma("sp", cg[:, 0:4, :], cqT_d[:, :, g * 512:(g + 1) * 512].rearrange("c p t -> p c t"),
                  reads=[cqT_b], writes=[cgb], sem_buf=cgb)
            s3 = aux.setdefault((id(cgb), 3), Buf("s3"))
            S.dma("sp", cg[:, 4:6, :], ckvT_d[:, :, g * 512:(g + 1) * 512].rearrange("c p t -> p c t"),
                  reads=[ckvT_b], pwrites=[cgb], sem_buf=s3)
            for ct in range(4):
                w, wb = load_w_tile(b_w_uq, ct * 512, 512, nk=4)
                proj_fm(cg, cgb, w, wb, 4, 128, 4, lambda m, ct=ct, g=g: qT_d[ct * 4 + m, :, g * 512:(g + 1) * 512], qT_b)
            for ct in range(2):
                w, wb = load_w_tile(b_w_iq, ct * 512, 512, nk=4)
                proj_fm(cg, cgb, w, wb, 4, 64, 8, lambda m, ct=ct, g=g: qiT_d[ct * 8 + m, :, g * 512:(g + 1) * 512], qiT_b)
            for hq in range(4):
                w, wb = wt.get()
                for hh in range(4):
                    S.dma("pool", w[:, 0:2, hh * 128:(hh + 1) * 128], b_w_uk[hq * 4 + hh].rearrange("(k p) d -> p k d", p=128),
                          sem_buf=wb, **(dict(writes=[wb]) if hh == 0 else dict(pwrites=[wb])))
                proj_fm(cg, cgb, w, wb, 2, 128, 4, lambda m, hq=hq, g=g: kT_d[hq * 4 + m, :, g * 512:(g + 1) * 512], kT_b, k0=4)
                w2, w2b = wt.get()
                for hh in range(4):
                    S.dma("pool", w2[:, 0:2, hh * 128:(hh + 1) * 128], b_w_uv[hq * 4 + hh].rearrange("(k p) d -> p k d", p=128),
                          sem_buf=w2b, **(dict(writes=[w2b]) if hh == 0 else dict(pwrites=[w2b])))
                for tb in range(4):
                    r0 = g * 512 + tb * 128
                    proj_tm(cg, cgb, w2, w2b, 2, 512, tb, v_d[r0:r0 + 128, hq * 512:(hq + 1) * 512], v_b, k0=4)
        S.end_phase()

    def dsa_indexer(kiT_b, qiT_b, mT_b):
        S.begin_phase()
        qiR = Ring(S, "qiR", 2, [64, c.IH, 128], BF16)
        kiAll, kiAll_b = S.sbuf("kiAll", [64, SL], BF16)
        relu = Ring(S, "relu", 3, [128, 512], BF16)
        Irow, Irow_b = S.sbuf("Irow", [128, SL], F32)
        Iwork, Iwork_b = S.sbuf("Iwork", [128, SL], F32)
        Mrow, Mrow_b = S.sbuf("Mrow", [128, SL], BF16)
        m8 = Ring(S, "m8", 2, [128, 8], F32)
        mrow = Ring(S, "mrowi", 2, [128, NB, 128], BF16)
        S.dma("sp", kiAll[:], kiT_d, reads=[kiT_b], writes=[kiAll_b], sem_buf=kiAll_b)
        nrounds = c.IDX_TOPK // 8
        for i in range(NB):
            L = (i + 1) * 128
            qi, qib = qiR.get()
            S.dma("sp", qi[:], qiT_d[:, :, i * 128:(i + 1) * 128].rearrange("h d t -> d h t"), reads=[qiT_b], writes=[qib], sem_buf=qib)
            for s0 in range(0, L, 512):
                sn = min(512, L - s0)
                for hh in range(c.IH):
                    ps, psb = mmR.get()
                    S.op("pe", lambda e, ps=ps, qi=qi, hh=hh, s0=s0, sn=sn: e.matmul(ps[:, 0:sn], qi[:, hh, :], kiAll[:, s0:s0 + sn],
                                                                                      start=True, stop=True),
                         reads=[qib, kiAll_b], writes=[psb])
                    r, rb = relu.get()
                    S.op("act", lambda e, r=r, ps=ps, sn=sn: e.activation(r[:, 0:sn], ps[:, 0:sn], AF.Relu), reads=[psb], writes=[rb])
                    if hh == 0:
                        S.op("dve", lambda e, r=r, i=i, hh=hh, s0=s0, sn=sn: e.tensor_scalar(
                            Irow[:, s0:s0 + sn], r[:, 0:sn], wI[:, i, hh:hh + 1], None, ALU.mult),
                            reads=[rb, wI_b, Irow_b], writes=[Irow_b])
                    else:
                        S.op("dve", lambda e, r=r, i=i, hh=hh, s0=s0, sn=sn: e.scalar_tensor_tensor(
                            Irow[:, s0:s0 + sn], r[:, 0:sn], wI[:, i, hh:hh + 1], Irow[:, s0:s0 + sn], ALU.mult, ALU.add),
                            reads=[rb, wI_b, Irow_b], writes=[Irow_b])
            S.op("dve", lambda e, i=i: e.tensor_tensor(Irow[:, i * 128:(i + 1) * 128], Irow[:, i * 128:(i + 1) * 128], chneg[:], ALU.add),
                 reads=[Irow_b, chneg_b], writes=[Irow_b])
            S.op("dve", lambda e, L=L: e.tensor_copy(Iwork[:, 0:L], Irow[:, 0:L]), reads=[Irow_b], writes=[Iwork_b])
            mx, mxb = m8.get()
            for r_ in range(nrounds):
                S.op("dve", lambda e, mx=mx, L=L: e.max(mx[:], Iwork[:, 0:L]), reads=[Iwork_b], writes=[mxb])
                if r_ < nrounds - 1:
                    S.op("dve", lambda e, mx=mx, L=L: e.match_replace(Iwork[:, 0:L], mx[:], Iwork[:, 0:L], NEG),
                         reads=[mxb, Iwork_b], writes=[Iwork_b])
            sm, smb = small.get()
            S.op("dve", lambda e, sm=sm, mx=mx: e.tensor_tensor(sm[:, 1:2], mx[:, 0:1], mx[:, 7:8], ALU.min), reads=[mxb], writes=[smb])
            S.op("dve", lambda e, sm=sm: e.tensor_scalar_max(sm[:, 0:1], sm[:, 1:2], -1.0e29), reads=[smb], pwrites=[smb])
            S.op("dve", lambda e, sm=sm, L=L: e.tensor_scalar(Mrow[:, 0:L], Irow[:, 0:L], sm[:, 0:1], None, ALU.is_ge),
                 reads=[smb, Irow_b], writes=[Mrow_b])
            mt, mtb = mrow.get()
            transpose_cols(Mrow, Mrow_b, L, lambda k, mt=mt: mt[:, k, :], mtb, BF16)
            S.dma("pool", mT_d[i, 0:i + 1].rearrange("j s t -> s j t"), mt[:, 0:i + 1, :], reads=[mtb], pwrites=[mT_b], sem_buf=mtb)
        S.end_phase()

    out_b = Buf("out")
    for q in range(c.NSEQ):
        cur_d, cur_b = x_in[q], Buf("xin")
        run_depth = getattr(c, "RUN_DEPTH", c.DEPTH)
        for li in range(run_depth):
            xT_b, qT_b, kT_b, v_b, o_b = Buf("xT"), Buf("qT"), Buf("kT"), Buf("v"), Buf("o")
            x1_b, x1T_b, x2_b = Buf("x1"), Buf("x1T"), Buf("x2")
            phase_transpose_in(cur_d, cur_b, xT_d, xT_b)
            if li % 2 == 0:
                fox_projections(xT_b, qT_b, kT_b, v_b)
                attention("fox", qT_b, kT_b, v_b, o_b)
                w_out_ap = a_w_out
            else:
                kiT_b, qiT_b, mT_b = Buf("kiT"), Buf("qiT"), Buf("mT")
                dsa_projections(xT_b, qT_b, kT_b, v_b, kiT_b, qiT_b)
                dsa_indexer(kiT_b, qiT_b, mT_b)
                attention("dsa", qT_b, kT_b, v_b, o_b, mT_b)
                w_out_ap = b_w_out
            outproj_ln_router(li, w_out_ap, cur_d, cur_b, o_b, x1_b, x1T_b)
            if li == run_depth - 1:
                moe_and_ln2(li, x1_b, x1T_b, out[q], out_b)
            else:
                moe_and_ln2(li, x1_b, x1T_b, x2_d, x2_b)
                cur_d, cur_b = x2_d, x2_b
    S.begin_phase()
    S.op("sp", lambda e: e.nop(), reads=[out_b])
    S.end_phase()
    st = S.stats
    S.close()
    return nc, st


def make_consts():
    i = np.arange(128)
    ident = np.eye(128, dtype=np.float32)
    tri = (i[:, None] <= i[None, :]).astype(np.float32)
    ones = np.ones((128, 128), np.float32)
    caus = (i[:, None] <= i[None, :]).astype(np.float32)
    chn = np.where((i[None, :] // 64) <= (i[:, None] // 64), 0.0, NEG).astype(np.float32)
    return np.stack([ident, tri, ones, caus, chn]).astype(np.float32)


def bias_layout(rel_bias):
    s = np.arange(128)[:, None]
    t = np.arange(128)[None, :]
    tiles = []
    for off in (0, 1):
        rel = (s - off * 128) - t
        bk = t5_bucket_np(rel.astype(np.int32))
        tiles.append(np.transpose(rel_bias[bk], (2, 0, 1)))
    return np.ascontiguousarray(np.stack(tiles)).astype(np.float32), np.ascontiguousarray(rel_bias[15:16, :])


_CACHE = {}


def run_cfg(cfg, inputs, core_seqs):
    key = (cfg.S, cfg.E, cfg.TOPK, cfg.IDX_TOPK, cfg.NSEQ, getattr(cfg, 'RUN_DEPTH', 2), getattr(cfg, 'LIMIT', 0))
    if key not in _CACHE:
        _CACHE[key] = build_program(cfg)
    nc, st = _CACHE[key]
    bt, b15 = bias_layout(np.asarray(inputs["rel_bias"], np.float32))
    consts = make_consts()
    shared = {}
    for k in ("a_w_in", "a_b_f", "a_w_out", "b_w_in", "b_q_norm", "b_kv_norm", "b_w_uq", "b_w_iq", "b_w_uk",
              "b_w_uv", "b_w_out"):
        shared[k] = np.ascontiguousarray(np.asarray(inputs[k], np.float32)[0])
    for k in ("ln1_g", "ln1_b", "ln2_g", "ln2_b", "router_w", "router_b", "w_gate", "w_up", "w_down",
              "sh_gate", "sh_up", "sh_down"):
        shared[k] = np.ascontiguousarray(np.asarray(inputs[k], np.float32))
    shared["bias_tiles"] = bt
    shared["bias_far"] = b15
    shared["consts"] = consts
    x = np.asarray(inputs["x"], np.float32)
    in_maps = []
    for seqs in core_seqs:
        m = dict(shared)
        m["x"] = np.ascontiguousarray(x[seqs])
        in_maps.append(m)
    res = run_bass_kernel_spmd(nc, in_maps, core_ids=list(range(len(core_seqs))))
    outp = np.zeros_like(x)
    for ci, seqs in enumerate(core_seqs):
        outp[seqs] = res.results[ci]["out"]
    return outp


def kernel(**inputs):
    x = np.asarray(inputs["x"])
    B, SL, D = x.shape
    E = np.asarray(inputs["router_w"]).shape[-1]
    ncore = 4
    nseq = B // ncore
    cfg = Cfg(S=SL, E=E, TOPK=8, IDX_TOPK=256, NSEQ=nseq)
    core_seqs = [list(range(ci * nseq, (ci + 1) * nseq)) for ci in range(ncore)]
    return run_cfg(cfg, inputs, core_seqs)
```

```python
import math
import numpy as np
from contextlib import ExitStack
import concourse.bass as bass
import concourse.mybir as mybir
from concourse.bass_utils import run_bass_kernel_spmd

F32 = mybir.dt.float32
BF16 = mybir.dt.bfloat16
AF = mybir.ActivationFunctionType
ALU = mybir.AluOpType
AX = mybir.AxisListType

ENG_NAMES = ("pe", "act", "dve", "pool", "sp")
SEM_EPOCH = 30000
NEG = -1.0e30


class Buf:
    __slots__ = ("name", "writers", "readers", "dsem", "dcount", "war")

    def __init__(self, name):
        self.name = name
        self.writers = []
        self.readers = []
        self.war = []
        self.dsem = None
        self.dcount = 0


class Op:
    __slots__ = ("eng", "fn", "deps", "is_dma", "dbuf", "signal", "event")


class Sched:
    def __init__(self, nc):
        self.nc = nc
        self.es = ExitStack()
        self.ops = []
        self._sems = []
        self._n = 0
        self.done = 0
        self.bar = 0
        self.last_eng = {}
        self.dma_last = {}
        self.eng_sems = {e: [] for e in ENG_NAMES}
        self.eng_cnt = {e: 0 for e in ENG_NAMES}
        self.waited = {e: {} for e in ENG_NAMES}
        self.pes = None
        self.uid = 0
        self.stats = {e: [0, 0] for e in ENG_NAMES}
        self.free_dsems = []
        self.live_dbufs = []

    def sbuf(self, name, shape, dtype):
        self.uid += 1
        es = self.pes if self.pes is not None else self.es
        t = es.enter_context(self.nc.sbuf_tensor("%s_%d" % (name, self.uid), list(shape), dtype))
        return t, Buf(name)

    def begin_phase(self):
        assert self.pes is None
        self.pes = ExitStack()
        self.phase_no = getattr(self, "phase_no", 0) + 1
        self.skip = self.phase_no > getattr(self, "limit", 10 ** 9)

    def end_phase(self):
        if self.skip:
            self.pes.close()
            self.pes = None
            return
        self.barrier()
        self.emit()
        self.pes.close()
        self.pes = None

    def barrier(self):
        for x in ENG_NAMES:
            deps = [o for y, o in self.last_eng.items() if y != x and o >= self.bar]
            deps += [o for o in self.dma_last.values() if o >= self.bar]
            op = Op()
            op.eng = x
            op.fn = lambda e: e.nop()
            op.is_dma = False
            op.dbuf = None
            op.signal = False
            op.event = None
            op.deps = sorted(set(deps))
            self.ops.append(op)
        self.bar = len(self.ops)
        self.last_eng = {}
        self.dma_last = {}

    def psum(self, name, shape, dtype):
        t = self.es.enter_context(self.nc.psum_tensor(name, list(shape), dtype))
        return t, Buf(name)

    def sem(self, name):
        s = self.es.enter_context(self.nc.semaphore(name))
        self._sems.append(s)
        return s

    def _record(self, eng, fn, reads, writes, pwrites, is_dma, dbuf):
        if getattr(self, "skip", False):
            return -1
        deps = set()
        for b in reads:
            deps.update(b.writers)
        for b in writes:
            deps.update(b.writers)
            deps.update(b.readers)
        for b in pwrites:
            deps.update(b.readers)
            deps.update(b.war)
        op = Op()
        op.eng = eng
        op.fn = fn
        op.is_dma = is_dma
        op.dbuf = dbuf
        op.signal = False
        op.event = None
        oid = len(self.ops)
        keep = []
        if is_dma:
            self.dma_last[id(dbuf)] = oid
        else:
            self.last_eng[eng] = oid
        for d in deps:
            if d < self.bar:
                continue
            p = self.ops[d]
            if (not p.is_dma) and p.eng == eng and eng in ("pe", "sp"):
                continue
            keep.append(d)
        op.deps = sorted(keep)
        self.ops.append(op)
        for b in reads:
            b.readers.append(oid)
        for b in writes:
            b.war = [d for d in list(b.writers) + list(b.readers) if d >= self.bar]
            b.writers = [oid]
            b.readers = []
        for b in pwrites:
            b.writers.append(oid)
        return oid

    def op(self, eng, fn, reads=(), writes=(), pwrites=()):
        return self._record(eng, fn, reads, writes, pwrites, False, None)

    def dma(self, eng, out, in_, reads=(), writes=(), pwrites=(), sem_buf=None, **kw):
        assert sem_buf is not None

        def fn(e):
            return e.dma_start(out=out, in_=in_, **kw)
        return self._record(eng, fn, reads, writes, pwrites, True, sem_buf)

    def emit(self):
        nc = self.nc
        ops = self.ops
        lo = self.done
        for op in ops[lo:]:
            for d in op.deps:
                assert d >= lo, "dep on an op emitted in an earlier batch"
                if not ops[d].is_dma:
                    ops[d].signal = True
        for op in ops[lo:]:
            if op.is_dma:
                b = op.dbuf
                if b.dsem is None:
                    if self.free_dsems:
                        b.dsem, b.dcount = self.free_dsems.pop()
                    else:
                        b.dsem = self.sem("d%d" % len(self._sems))
                        b.dcount = 0
                    self.live_dbufs.append(b)
                b.dcount += 16
                op.event = (b.dsem, b.dcount)
            elif op.signal:
                e = op.eng
                k = self.eng_cnt[e] // SEM_EPOCH
                if k >= len(self.eng_sems[e]):
                    self.eng_sems[e].append(self.sem("e_%s_%d" % (e, k)))
                self.eng_cnt[e] += 1
                op.event = (self.eng_sems[e][k], self.eng_cnt[e] - k * SEM_EPOCH)
        per_eng = {e: [] for e in ENG_NAMES}
        for op in ops[lo:]:
            per_eng[op.eng].append(op)

        def run_engine(ename, eobj):
            waited = self.waited[ename]
            for op in per_eng[ename]:
                need = {}
                for d in op.deps:
                    s, v = ops[d].event
                    key = id(s)
                    if waited.get(key, 0) >= v:
                        continue
                    if key not in need or need[key][1] < v:
                        need[key] = (s, v)
                for key, (s, v) in need.items():
                    eobj.wait_ge(s, v)
                    waited[key] = v
                    self.stats[ename][1] += 1
                ins = op.fn(eobj)
                self.stats[ename][0] += 1
                if op.event is not None:
                    ins.then_inc(op.event[0], 16 if op.is_dma else 1)
                op.fn = None

        with nc.Block() as block:
            @block.sync
            def _(e):
                run_engine("sp", e)

            @block.scalar
            def _(e):
                run_engine("act", e)

            @block.vector
            def _(e):
                run_engine("dve", e)

            @block.gpsimd
            def _(e):
                run_engine("pool", e)

            @block.tensor
            def _(e):
                run_engine("pe", e)
        self.done = len(ops)
        self.n_sems = len(self._sems)
        for b in self.live_dbufs:
            self.free_dsems.append((b.dsem, b.dcount))
            b.dsem = None
        self.live_dbufs = []
        return self.stats

    def close(self):
        self.es.close()


class Ring:
    def __init__(self, S, name, n, shape, dtype, psum=False):
        self.slots = []
        for i in range(n):
            self.slots.append((S.psum if psum else S.sbuf)("%s%d" % (name, i), shape, dtype))
        self.i = 0

    def get(self):
        s = self.slots[self.i % len(self.slots)]
        self.i += 1
        return s


class Cfg:
    def __init__(self, S=4096, E=64, TOPK=8, IDX_TOPK=256, NSEQ=1, DEPTH=2):
        self.S = S
        self.NB = S // 128
        self.D = 2048
        self.H = 16
        self.E = E
        self.TOPK = TOPK
        self.DE = 512
        self.IDX_TOPK = min(IDX_TOPK, S // 4)
        self.NSEQ = NSEQ
        self.DEPTH = DEPTH
        self.ALPHA = (2 * DEPTH) ** 0.25
        self.QL = 512
        self.KVL = 256
        self.IH = 16
        self.ID = 64
        self.BW = 512 + 256 + 64 + 16


def t5_bucket_np(rel):
    half = 16
    max_exact = 8
    n = np.abs(rel)
    large = max_exact + (np.log(np.maximum(n, 1).astype(np.float32) / max_exact)
                         / math.log(128 / max_exact) * (half - max_exact)).astype(np.int32)
    large = np.minimum(large, half - 1)
    return np.where(rel > 0, half, 0) + np.where(n < max_exact, n, large)


def build_program(cfg, debug_outs=()):
    nc = bass.Bass("TRN2", target_bir_lowering=False)
    S = Sched(nc)
    c = cfg
    S.limit = getattr(cfg, "LIMIT", 10 ** 9)
    NB, D, H, E, SL = c.NB, c.D, c.H, c.E, c.S
    DC = D // 128
    NG = SL // 512

    def din(name, shape, dt=F32):
        return nc.dram_tensor(name, list(shape), dt, kind="ExternalInput").ap()

    def dscr(name, shape, dt):
        return nc.dram_tensor(name, list(shape), dt, kind="Internal").ap()

    x_in = din("x", [c.NSEQ, SL, D])
    a_w_in = din("a_w_in", [D, 3 * D + H])
    a_b_f = din("a_b_f", [1, H])
    a_w_out = din("a_w_out", [D, D])
    b_w_in = din("b_w_in", [D, c.BW])
    b_q_norm = din("b_q_norm", [1, c.QL])
    b_kv_norm = din("b_kv_norm", [1, c.KVL])
    b_w_uq = din("b_w_uq", [c.QL, D])
    b_w_iq = din("b_w_iq", [c.QL, c.IH * c.ID])
    b_w_uk = din("b_w_uk", [H, c.KVL, 128])
    b_w_uv = din("b_w_uv", [H, c.KVL, 128])
    b_w_out = din("b_w_out", [D, D])
    bt_in = din("bias_tiles", [2, H, 128, 128])
    b15_in = din("bias_far", [1, H])
    ln1_g = din("ln1_g", [c.DEPTH, D])
    ln1_b = din("ln1_b", [c.DEPTH, D])
    ln2_g = din("ln2_g", [c.DEPTH, D])
    ln2_b = din("ln2_b", [c.DEPTH, D])
    router_w = din("router_w", [c.DEPTH, D, E])
    router_b = din("router_b", [c.DEPTH, E])
    w_gate = din("w_gate", [c.DEPTH, E, D, c.DE])
    w_up = din("w_up", [c.DEPTH, E, D, c.DE])
    w_down = din("w_down", [c.DEPTH, E, c.DE, D])
    sh_gate = din("sh_gate", [c.DEPTH, D, c.DE])
    sh_up = din("sh_up", [c.DEPTH, D, c.DE])
    sh_down = din("sh_down", [c.DEPTH, c.DE, D])
    consts = din("consts", [5, 128, 128])
    out = nc.dram_tensor("out", [c.NSEQ, SL, D], F32, kind="ExternalOutput").ap()
    dbg = {}
    for nm, shp in debug_outs:
        dbg[nm] = nc.dram_tensor("dbg_" + nm, list(shp), F32, kind="ExternalOutput").ap()

    xT_d = dscr("xT_d", [DC, 128, SL], BF16)
    qT_d = dscr("qT_d", [H, 128, SL], BF16)
    kT_d = dscr("kT_d", [H, 128, SL], BF16)
    v_d = dscr("v_d", [SL, D], BF16)
    o_d = dscr("o_d", [SL, D], BF16)
    x1_d = dscr("x1_d", [SL, D], F32)
    x1T_d = dscr("x1T_d", [DC, 128, SL], BF16)
    x2_d = dscr("x2_d", [SL, D], F32)
    cqT_d = dscr("cqT_d", [4, 128, SL], BF16)
    ckvT_d = dscr("ckvT_d", [2, 128, SL], BF16)
    kiT_d = dscr("kiT_d", [64, SL], BF16)
    qiT_d = dscr("qiT_d", [c.IH, 64, SL], BF16)
    mT_d = dscr("mT_d", [NB, NB, 128, 128], BF16)

    identf, identf_b = S.sbuf("identf", [128, 128], F32)
    identb, identb_b = S.sbuf("identb", [128, 128], BF16)
    trif, trif_b = S.sbuf("trif", [128, 128], F32)
    onesf, onesf_b = S.sbuf("onesf", [128, 128], F32)
    causb, causb_b = S.sbuf("causb", [128, 128], BF16)
    chneg, chneg_b = S.sbuf("chneg", [128, 128], F32)
    epsb, epsb_b = S.sbuf("epsb", [128, 4], F32)
    logf, logf_b = S.sbuf("logf", [128, NB, H], F32)
    Fs, Fs_b = S.sbuf("Fs", [128, NB, H], F32)
    Fend, Fend_b = S.sbuf("Fend", [128, NB + 1, H], F32)
    G_all, G_b = S.sbuf("G_all", [128, NB, E + 1], F32)
    wI, wI_b = S.sbuf("wI", [128, NB, c.IH], F32)
    bfb, bfb_b = S.sbuf("bfb", [128, H], F32)
    b15b, b15b_b = S.sbuf("b15b", [128, H], F32)
    lng, lng_b = S.sbuf("lng", [128, D], F32)
    lnb, lnb_b = S.sbuf("lnb", [128, D], F32)
    small = Ring(S, "small", 6, [128, 64], F32)
    stats = Ring(S, "stats", 2, [128, 4, 6], F32)
    obf = Ring(S, "obf", 3, [128, 512], BF16)

    mmR = Ring(S, "pmm", 4, [128, 512], F32, psum=True)
    tpfR = Ring(S, "ptf", 1, [128, 512], F32, psum=True)
    tpbR = Ring(S, "ptb", 1, [128, 1024], BF16, psum=True)
    poR = Ring(S, "ppo", 2, [128, 512], F32, psum=True)

    S.begin_phase()
    S.dma("sp", identf[:], consts[0], writes=[identf_b], sem_buf=identf_b)
    S.dma("pool", identb[:], consts[0], writes=[identb_b], sem_buf=identb_b)
    S.dma("sp", trif[:], consts[1], writes=[trif_b], sem_buf=trif_b)
    S.dma("sp", onesf[:], consts[2], writes=[onesf_b], sem_buf=onesf_b)
    S.dma("pool", causb[:], consts[3], writes=[causb_b], sem_buf=causb_b)
    S.dma("sp", chneg[:], consts[4], writes=[chneg_b], sem_buf=chneg_b)
    S.dma("sp", bfb[:], bass.AP(a_b_f.tensor, a_b_f.offset, [[0, 128], [1, H]]), writes=[bfb_b], sem_buf=bfb_b)
    S.dma("sp", b15b[:], bass.AP(b15_in.tensor, b15_in.offset, [[0, 128], [1, H]]), writes=[b15b_b], sem_buf=b15b_b)
    S.op("dve", lambda e: e.memset(epsb[:, 0:1], 1e-5), writes=[epsb_b])
    S.op("dve", lambda e: e.memset(epsb[:, 1:2], 1e-6), pwrites=[epsb_b])
    S.op("dve", lambda e: e.memset(epsb[:, 2:3], 1.0), pwrites=[epsb_b])
    S.op("dve", lambda e: e.memset(epsb[:, 3:4], 0.0), pwrites=[epsb_b])
    S.end_phase()

    rr = {"i": 0}

    def bcast_rows(ap_row, n):
        return bass.AP(ap_row.tensor, ap_row.offset, [[0, 128], [1, n]])

    def evac_eng():
        return "dve"

    def copy(eng, o, ob, i, ib, partial=False):
        w = dict(pwrites=[ob]) if partial else dict(writes=[ob])
        if eng == "act":
            S.op("act", lambda e: e.copy(o, i), reads=[ib], **w)
        else:
            S.op(eng, lambda e: e.tensor_copy(o, i), reads=[ib], **w)

    def mm_chain(ps, psb, pairs, reads):
        n = len(pairs)
        for k, (l, r) in enumerate(pairs):
            S.op("pe", lambda e, l=l, r=r, k=k: e.matmul(ps, l, r, start=(k == 0), stop=(k == n - 1)),
                 reads=reads, writes=[psb] if (k == 0 or k == n - 1) else ())

    def transpose_cols(src, srcb, ncols, dst_fn, dstb, dt, first_full=True):
        nchunk = ncols // 128
        per = 4 if dt == F32 else 8
        first = first_full
        for k0 in range(0, nchunk, per):
            kn = min(per, nchunk - k0)
            pt, ptb = (tpfR if dt == F32 else tpbR).get()
            idt, idb = (identf, identf_b) if dt == F32 else (identb, identb_b)
            for k in range(kn):
                S.op("pe", lambda e, k=k, k0=k0, pt=pt, idt=idt: e.transpose(
                    pt[:, k * 128:(k + 1) * 128], src[:, (k0 + k) * 128:(k0 + k + 1) * 128], idt[:]),
                    reads=[srcb, idb], writes=[ptb] if (k == 0 or k == kn - 1) else ())
            for k in range(kn):
                copy(evac_eng(), dst_fn(k0 + k), dstb, pt[:, k * 128:(k + 1) * 128], ptb, partial=not first)
                first = False

    def layer_norm_rows(y, yb):
        st, stb = stats.get()
        for k in range(4):
            S.op("dve", lambda e, k=k: e.bn_stats(st[:, k, :], y[:, k * 512:(k + 1) * 512]),
                 reads=[yb], writes=[stb] if k == 0 else (), pwrites=[stb] if k else ())
        sm, smb = small.get()
        S.op("dve", lambda e: e.bn_aggr(sm[:, 0:2], st[:].rearrange("p a b -> p (a b)")), reads=[stb], writes=[smb])
        S.op("act", lambda e: e.activation(sm[:, 2:3], sm[:, 1:2], AF.Sqrt, bias=epsb[:, 0:1], scale=1.0),
             reads=[smb, epsb_b], pwrites=[smb])
        S.op("dve", lambda e: e.reciprocal(sm[:, 3:4], sm[:, 2:3]), reads=[smb], pwrites=[smb])
        S.op("dve", lambda e: e.tensor_scalar(y[:], y[:], sm[:, 0:1], sm[:, 3:4], ALU.subtract, ALU.mult),
             reads=[smb, yb], writes=[yb])
        S.op("pool", lambda e: e.tensor_tensor(y[:], y[:], lng[:], ALU.mult), reads=[yb, lng_b], writes=[yb])
        S.op("pool", lambda e: e.tensor_tensor(y[:], y[:], lnb[:], ALU.add), reads=[yb, lnb_b], writes=[yb])

    def load_ln(gsrc, bsrc, li):
        S.dma("sp", lng[:], bcast_rows(gsrc[li:li + 1, :], D), writes=[lng_b], sem_buf=lng_b)
        S.dma("sp", lnb[:], bcast_rows(bsrc[li:li + 1, :], D), writes=[lnb_b], sem_buf=lnb_b)

    def phase_transpose_in(src_d, src_b, dstT_d, dstT_b):
        S.begin_phase()
        xrow = Ring(S, "xrow", 2, [128, D], F32)
        xTblk = Ring(S, "xTblk", 2, [128, DC, 128], BF16)
        for b in range(NB):
            xr, xrb = xrow.get()
            S.dma("sp", xr[:], src_d[b * 128:(b + 1) * 128, :], reads=[src_b], writes=[xrb], sem_buf=xrb)
            xt, xtb = xTblk.get()
            import os
            if os.environ.get("DBG_P2", "") == "load":
                continue
            transpose_cols(xr, xrb, D, lambda k, xt=xt: xt[:, k, :], xtb, F32)
            v = os.environ.get("DBG_ST", "pool")
            if v != "none":
                S.dma(v, dstT_d[:, :, b * 128:(b + 1) * 128].rearrange("c p t -> p c t"), xt[:],
                      reads=[xtb], pwrites=[dstT_b], sem_buf=xtb)
        S.end_phase()

    def mk_load_w(wt):
        def load_w_tile(wsrc, col0, ncol, nk=DC):
            w, wb = wt.get()
            S.dma("pool", w[:, 0:nk, 0:ncol], wsrc[:, col0:col0 + ncol].rearrange("(c p) n -> p c n", p=128),
                  writes=[wb], sem_buf=wb)
            return w, wb
        return load_w_tile

    def proj_fm(xg, xgb, w, wb, nk, mw, nm, dst_fn, dst_b, k0=0):
        for m in range(nm):
            ps, psb = mmR.get()
            mm_chain(ps[0:mw, :], psb, [(w[:, k, m * mw:(m + 1) * mw], xg[:, k0 + k, :]) for k in range(nk)], [xgb, wb])
            ob_, obb = obf.get()
            copy(evac_eng(), ob_[0:mw, :], obb, ps[0:mw, :], psb)
            S.dma("sp", dst_fn(m), ob_[0:mw, :], reads=[obb], pwrites=[dst_b], sem_buf=obb)

    def proj_tm(xg, xgb, w, wb, nk, ncol, tb, dst_ap, dst_b, k0=0):
        ps, psb = mmR.get()
        mm_chain(ps[:, 0:ncol], psb, [(xg[:, k0 + k, tb * 128:(tb + 1) * 128], w[:, k, 0:ncol]) for k in range(nk)], [xgb, wb])
        ob_, obb = obf.get()
        copy(evac_eng(), ob_[:, 0:ncol], obb, ps[:, 0:ncol], psb)
        S.dma("sp", dst_ap, ob_[:, 0:ncol], reads=[obb], pwrites=[dst_b], sem_buf=obb)

    def fox_projections(xT_b, qT_b, kT_b, v_b):
        S.begin_phase()
        xTg = Ring(S, "xTg", 2, [128, DC, 512], BF16)
        wt = Ring(S, "wt", 3, [128, DC, 512], BF16)
        load_w_tile = mk_load_w(wt)
        for g in range(NG):
            xg, xgb = xTg.get()
            S.dma("sp", xg[:], xT_d[:, :, g * 512:(g + 1) * 512].rearrange("c p t -> p c t"),
                  reads=[xT_b], writes=[xgb], sem_buf=xgb)
            for ct in range(8):
                w, wb = load_w_tile(a_w_in, ct * 512, 512)
                dstT, dstb = (qT_d, qT_b) if ct < 4 else (kT_d, kT_b)
                h0 = (ct % 4) * 4
                proj_fm(xg, xgb, w, wb, DC, 128, 4,
                        lambda m, dstT=dstT, h0=h0, g=g: dstT[h0 + m, :, g * 512:(g + 1) * 512], dstb)
            for ct in range(8, 12):
                w, wb = load_w_tile(a_w_in, ct * 512, 512)
                for tb in range(4):
                    r0 = g * 512 + tb * 128
                    proj_tm(xg, xgb, w, wb, DC, 512, tb, v_d[r0:r0 + 128, (ct - 8) * 512:(ct - 7) * 512], v_b)
            w, wb = load_w_tile(a_w_in, 3 * D, H)
            for tb in range(4):
                blk = g * 4 + tb
                ps, psb = poR.get()
                mm_chain(ps[:, 0:H], psb, [(xg[:, k, tb * 128:(tb + 1) * 128], w[:, k, 0:H]) for k in range(DC)], [xgb, wb])
                sm, smb = small.get()
                S.op("dve", lambda e, sm=sm, ps=ps: e.tensor_tensor(sm[:, 0:16], ps[:, 0:H], bfb[:], ALU.add),
                     reads=[psb, bfb_b], writes=[smb])
                S.op("act", lambda e, sm=sm: e.activation(sm[:, 16:32], sm[:, 0:16], AF.Abs),
                     reads=[smb], pwrites=[smb])
                S.op("act", lambda e, sm=sm: e.activation(sm[:, 32:48], sm[:, 16:32], AF.Exp, scale=-1.0),
                     reads=[smb], pwrites=[smb])
                S.op("act", lambda e, sm=sm: e.activation(sm[:, 32:48], sm[:, 32:48], AF.Ln, bias=epsb[:, 2:3], scale=1.0),
                     reads=[smb, epsb_b], pwrites=[smb])
                S.op("dve", lambda e, sm=sm: e.tensor_scalar_min(sm[:, 48:64], sm[:, 0:16], 0.0), reads=[smb], pwrites=[smb])
                S.op("dve", lambda e, sm=sm, blk=blk: e.tensor_tensor(logf[:, blk, :], sm[:, 48:64], sm[:, 32:48], ALU.subtract),
                     reads=[smb], pwrites=[logf_b])
        S.op("dve", lambda e: e.memset(Fend[:, 0, :], 0.0), reads=[logf_b], writes=[Fend_b])
        for b in range(NB):
            ps, psb = poR.get()
            S.op("pe", lambda e, ps=ps, b=b: e.matmul(ps[:, 0:H], trif[:], logf[:, b, :], start=True, stop=True),
                 reads=[trif_b, logf_b], writes=[psb])
            S.op("pe", lambda e, ps=ps, b=b: e.matmul(ps[:, 32:32 + H], onesf[:], logf[:, b, :], start=True, stop=True),
                 reads=[onesf_b, logf_b], pwrites=[psb])
            S.op("dve", lambda e, ps=ps, b=b: e.tensor_tensor(Fs[:, b, :], ps[:, 0:H], Fend[:, b, :], ALU.add),
                 reads=[psb, Fend_b], pwrites=[Fs_b])
            S.op("dve", lambda e, ps=ps, b=b: e.tensor_tensor(Fend[:, b + 1, :], ps[:, 32:32 + H], Fend[:, b, :], ALU.add),
                 reads=[psb], pwrites=[Fend_b])
        S.end_phase()

    def attention(mode, qT_b, kT_b, v_b, o_b, mT_b=None):
        S.begin_phase()
        scale = 128 ** -0.5
        kTh = Ring(S, "kTh", 2, [128, SL], BF16)
        qTh = Ring(S, "qTh", 2, [128, SL], BF16)
        vh = Ring(S, "vh", 2, [128, NB, 132], BF16)
        pbf = Ring(S, "pbf", 6, [128, 128], BF16)
        if mode == "fox":
            npair = NB * (NB + 1) // 2
            ball, ball_b = S.sbuf("ball", [128, npair, H], F32)
            pidx = {}
            for i in range(NB):
                for j in range(i + 1):
                    pi_ = len(pidx)
                    pidx[(i, j)] = pi_
                    S.op("dve", lambda e, i=i, j=j, pi_=pi_: e.tensor_tensor(ball[:, pi_, :], Fend[:, i + 1, :], Fs[:, j, :], ALU.subtract),
                         reads=[Fend_b, Fs_b], writes=[ball_b] if pi_ == 0 else (), pwrites=[ball_b] if pi_ else ())
        if mode == "dsa":
            pf32 = Ring(S, "pf32", 2, [128, 128], F32)
            mrow = Ring(S, "mrow", 2, [128, NB, 128], BF16)
            btile, btile_b = S.sbuf("btile", [128, 2, H, 128], F32)
            S.dma("sp", btile[:], bt_in.rearrange("o h s t -> s o h t"), writes=[btile_b], sem_buf=btile_b)
        for h in range(H):
            kt, ktb = kTh.get()
            qt, qtb = qTh.get()
            vv, vvb = vh.get()
            S.dma("sp", kt[:], kT_d[h], reads=[kT_b], writes=[ktb], sem_buf=ktb)
            S.dma("sp", qt[:], qT_d[h], reads=[qT_b], writes=[qtb], sem_buf=qtb)
            S.dma("sp", vv[:, :, 0:128], v_d[:, h * 128:(h + 1) * 128].rearrange("(b p) d -> p b d", p=128),
                  reads=[v_b], writes=[vvb], sem_buf=vvb)
            S.op("pool", lambda e, vv=vv: e.memset(vv[:, :, 128:129], 1.0), reads=[vvb], pwrites=[vvb])
            for i in range(NB):
                po, pob = poR.get()
                if mode == "dsa":
                    mr, mrb = mrow.get()
                    S.dma("sp", mr[:, 0:i + 1, :], mT_d[i, 0:i + 1].rearrange("j s t -> s j t"),
                          reads=[mT_b], writes=[mrb], sem_buf=mrb)
                for j in range(i + 1):
                    ps, psb = mmR.get()
                    S.op("pe", lambda e, ps=ps, kt=kt, qt=qt, i=i, j=j: e.matmul(
                        ps[:, 0:128], kt[:, j * 128:(j + 1) * 128], qt[:, i * 128:(i + 1) * 128], start=True, stop=True),
                        reads=[ktb, qtb], writes=[psb])
                    p, pb = pbf.get()
                    if mode == "fox":
                        pi_ = pidx[(i, j)]
                        S.op("act", lambda e, p=p, ps=ps, pi_=pi_, h=h: e.activation(p[:], ps[:, 0:128], AF.Exp,
                                                                                     bias=ball[:, pi_, h:h + 1], scale=scale),
                             reads=[psb, ball_b], writes=[pb])
                        if j == i:
                            S.op("dve", lambda e, p=p: e.tensor_tensor(p[:], p[:], causb[:], ALU.mult),
                                 reads=[pb, causb_b], writes=[pb])
                    else:
                        if j >= i - 1:
                            tf, tfb = pf32.get()
                            S.op("dve", lambda e, tf=tf, ps=ps, i=i, j=j, h=h: e.scalar_tensor_tensor(
                                tf[:], ps[:, 0:128], scale, btile[:, i - j, h, :], ALU.mult, ALU.add),
                                reads=[psb, btile_b], writes=[tfb])
                            S.op("act", lambda e, p=p, tf=tf: e.activation(p[:], tf[:], AF.Exp), reads=[tfb], writes=[pb])
                        else:
                            S.op("act", lambda e, p=p, ps=ps, h=h: e.activation(p[:], ps[:, 0:128], AF.Exp,
                                                                                bias=b15b[:, h:h + 1], scale=scale),
                                 reads=[psb, b15b_b], writes=[pb])
                        S.op("dve", lambda e, p=p, mr=mr, j=j: e.tensor_tensor(p[:], p[:], mr[:, j, :], ALU.mult),
                             reads=[pb, mrb], writes=[pb])
                    S.op("pe", lambda e, po=po, p=p, vv=vv, i=i, j=j: e.matmul(po[:, 0:129], p[:], vv[:, j, 0:129],
                                                                              start=(j == 0), stop=(j == i)),
                         reads=[pb, vvb], writes=[pob] if j == 0 else (), pwrites=[pob] if j > 0 else ())
                sm, smb = small.get()
                S.op("dve", lambda e, sm=sm, po=po: e.reciprocal(sm[:, 0:1], po[:, 128:129]), reads=[pob], writes=[smb])
                ob_, obb = obf.get()
                S.op("dve", lambda e, ob_=ob_, po=po, sm=sm: e.tensor_scalar(ob_[:, 0:128], po[:, 0:128], sm[:, 0:1], None, ALU.mult),
                     reads=[pob, smb], writes=[obb])
                S.dma("sp", o_d[i * 128:(i + 1) * 128, h * 128:(h + 1) * 128], ob_[:, 0:128],
                      reads=[obb], pwrites=[o_b], sem_buf=obb)
        S.end_phase()

    def outproj_ln_router(li, w_out_ap, xres_d, xres_b, o_b, x1_b, x1T_b):
        S.begin_phase()
        wt = Ring(S, "wt", 4, [128, DC, 512], BF16)
        load_w_tile = mk_load_w(wt)
        wres_tiles = [load_w_tile(w_out_ap, n * 512, 512) for n in range(4)]
        obfw = Ring(S, "obfw", 2, [128, D], BF16)
        xTblk = Ring(S, "xTblk", 2, [128, DC, 128], BF16)
        xTf32 = Ring(S, "xTf32", 1, [128, DC, 128], F32)
        xrow = Ring(S, "xrow", 2, [128, D], F32)
        yrow = Ring(S, "yrow", 2, [128, D], F32)
        smallw = Ring(S, "smallw", 2, [128, 2 * E + 16], F32)
        rwt, rwt_b = S.sbuf("rwt", [128, DC, E], F32)
        rbb, rbb_b = S.sbuf("rbb", [128, E], F32)
        load_ln(ln1_g, ln1_b, li)
        S.dma("sp", rwt[:], router_w[li].rearrange("(c p) n -> p c n", p=128), writes=[rwt_b], sem_buf=rwt_b)
        S.dma("sp", rbb[:], bcast_rows(router_b[li:li + 1, :], E), writes=[rbb_b], sem_buf=rbb_b)
        for b in range(NB):
            r0 = b * 128
            orow, orowb = obfw.get()
            S.dma("sp", orow[:], o_d[r0:r0 + 128, :], reads=[o_b], writes=[orowb], sem_buf=orowb)
            oT, oTb = xTblk.get()
            transpose_cols(orow, orowb, D, lambda k, oT=oT: oT[:, k, :], oTb, BF16)
            xr, xrb = xrow.get()
            S.dma("sp", xr[:], xres_d[r0:r0 + 128, :], reads=[xres_b], writes=[xrb], sem_buf=xrb)
            y, yb = yrow.get()
            for n in range(4):
                w, wb = wres_tiles[n]
                ps, psb = mmR.get()
                mm_chain(ps[:], psb, [(oT[:, k, :], w[:, k, :]) for k in range(DC)], [oTb, wb])
                S.op("dve", lambda e, y=y, xr=xr, ps=ps, n=n: e.scalar_tensor_tensor(
                    y[:, n * 512:(n + 1) * 512], xr[:, n * 512:(n + 1) * 512], c.ALPHA, ps[:], ALU.mult, ALU.add),
                    reads=[xrb, psb], writes=[yb] if n == 0 else (), pwrites=[yb] if n else ())
            layer_norm_rows(y, yb)
            S.dma("sp", x1_d[r0:r0 + 128, :], y[:], reads=[yb], pwrites=[x1_b], sem_buf=yb)
            xTf, xTfb = xTf32.get()
            transpose_cols(y, yb, D, lambda k, xTf=xTf: xTf[:, k, :], xTfb, F32)
            xt, xtb = xTblk.get()
            S.op("pool", lambda e, xt=xt, xTf=xTf: e.tensor_copy(xt[:], xTf[:]), reads=[xTfb], writes=[xtb])
            S.dma("pool", x1T_d[:, :, r0:r0 + 128].rearrange("c p t -> p c t"), xt[:], reads=[xtb], pwrites=[x1T_b], sem_buf=xtb)
            ps, psb = poR.get()
            mm_chain(ps[:, 0:E], psb, [(xTf[:, k, :], rwt[:, k, :]) for k in range(DC)], [xTfb, rwt_b])
            sm, smb = smallw.get()
            S.op("act", lambda e, sm=sm, ps=ps: e.activation(sm[:, 0:E], ps[:, 0:E], AF.Sigmoid), reads=[psb], writes=[smb])
            S.op("dve", lambda e, sm=sm: e.tensor_tensor(sm[:, E:2 * E], sm[:, 0:E], rbb[:], ALU.add), reads=[smb, rbb_b], pwrites=[smb])
            S.op("dve", lambda e, sm=sm: e.max(sm[:, 2 * E:2 * E + 8], sm[:, E:2 * E]), reads=[smb], pwrites=[smb])
            assert c.TOPK == 8
            S.op("dve", lambda e, sm=sm: e.tensor_tensor(sm[:, 2 * E + 10:2 * E + 11], sm[:, 2 * E:2 * E + 1], sm[:, 2 * E + 7:2 * E + 8], ALU.min),
                 reads=[smb], pwrites=[smb])
            S.op("dve", lambda e, sm=sm: e.tensor_scalar(sm[:, E:2 * E], sm[:, E:2 * E], sm[:, 2 * E + 10:2 * E + 11], None, ALU.is_ge),
                 reads=[smb], pwrites=[smb])
            S.op("dve", lambda e, sm=sm: e.tensor_tensor(sm[:, 0:E], sm[:, 0:E], sm[:, E:2 * E], ALU.mult), reads=[smb], pwrites=[smb])
            S.op("dve", lambda e, sm=sm: e.reduce_sum(sm[:, 2 * E + 8:2 * E + 9], sm[:, 0:E], axis=AX.X), reads=[smb], pwrites=[smb])
            S.op("dve", lambda e, sm=sm: e.reciprocal(sm[:, 2 * E + 9:2 * E + 10], sm[:, 2 * E + 8:2 * E + 9]), reads=[smb], pwrites=[smb])
            S.op("dve", lambda e, sm=sm: e.tensor_scalar(sm[:, 0:E], sm[:, 0:E], sm[:, 2 * E + 9:2 * E + 10], None, ALU.mult),
                 reads=[smb], pwrites=[smb])
            S.op("dve", lambda e, sm=sm, b=b: e.tensor_scalar(G_all[:, b, 0:E], sm[:, 0:E], 2.5, None, ALU.mult),
                 reads=[smb], pwrites=[G_b])
            S.op("dve", lambda e, b=b: e.memset(G_all[:, b, E:E + 1], 1.0), pwrites=[G_b])
        import os
        if os.environ.get("DBG_G"):
            S.dma("sp", out[0][0:128, 0:NB * (E + 1)], G_all[:].rearrange("p b e -> p (b e)"), reads=[G_b], pwrites=[out_b], sem_buf=G_b)
        S.end_phase()

    def moe_and_ln2(li, x1_b, x1T_b, dst_d, dst_b):
        S.begin_phase()
        xTg = Ring(S, "xTg", 1, [128, DC, 512], BF16)
        wt = Ring(S, "wt", 3, [128, DC, 512], BF16)
        load_w_tile = mk_load_w(wt)
        wdn = Ring(S, "wdn", 1, [128, 4, D], BF16)
        hT = Ring(S, "hT", 2, [128, 4, 512], BF16)
        sg = Ring(S, "sg", 2, [128, 512], F32)
        acc, acc_b = S.sbuf("acc", [128, 4, D], F32)
        xrow = Ring(S, "xrow", 2, [128, D], F32)
        load_ln(ln2_g, ln2_b, li)
        for g in range(NG):
            xg, xgb = xTg.get()
            S.dma("sp", xg[:], x1T_d[:, :, g * 512:(g + 1) * 512].rearrange("c p t -> p c t"),
                  reads=[x1T_b], writes=[xgb], sem_buf=xgb)
            for ei in range(E + 1):
                if ei < E:
                    wgs, wus, wds = w_gate[li, ei], w_up[li, ei], w_down[li, ei]
                else:
                    wgs, wus, wds = sh_gate[li], sh_up[li], sh_down[li]
                wg, wgb = load_w_tile(wgs, 0, 512)
                wu, wub = load_w_tile(wus, 0, 512)
                wd, wdb = wdn.get()
                S.dma("pool", wd[:], wds.rearrange("(c p) n -> p c n", p=128), writes=[wdb], sem_buf=wdb)
                ht, htb = hT.get()
                for m in range(4):
                    psg, psgb = mmR.get()
                    mm_chain(psg[:], psgb, [(wg[:, k, m * 128:(m + 1) * 128], xg[:, k, :]) for k in range(DC)], [xgb, wgb])
                    psu, psub = mmR.get()
                    mm_chain(psu[:], psub, [(wu[:, k, m * 128:(m + 1) * 128], xg[:, k, :]) for k in range(DC)], [xgb, wub])
                    s_, sb_ = sg.get()
                    S.op("act", lambda e, s_=s_, psg=psg: e.activation(s_[:], psg[:], AF.Silu), reads=[psgb], writes=[sb_])
                    S.op("dve", lambda e, ht=ht, s_=s_, psu=psu, m=m: e.tensor_tensor(ht[:, m, :], s_[:], psu[:], ALU.mult),
                         reads=[sb_, psub], writes=[htb] if m == 0 else (), pwrites=[htb] if m else ())
                for tb in range(4):
                    blk = g * 4 + tb
                    for n in range(4):
                        ps, psb = mmR.get()
                        mm_chain(ps[:], psb, [(ht[:, m, tb * 128:(tb + 1) * 128], wd[:, m, n * 512:(n + 1) * 512]) for m in range(4)],
                                 [htb, wdb])
                        a = acc[:, tb, n * 512:(n + 1) * 512]
                        if ei == 0:
                            S.op("dve", lambda e, a=a, ps=ps, blk=blk, ei=ei: e.tensor_scalar(a, ps[:], G_all[:, blk, ei:ei + 1], None, ALU.mult),
                                 reads=[psb, G_b], pwrites=[acc_b])
                        else:
                            S.op("dve", lambda e, a=a, ps=ps, blk=blk, ei=ei: e.scalar_tensor_tensor(
                                a, ps[:], G_all[:, blk, ei:ei + 1], a, ALU.mult, ALU.add),
                                reads=[psb, G_b, acc_b], pwrites=[acc_b])
            for tb in range(4):
                r0 = g * 512 + tb * 128
                xr, xrb = xrow.get()
                S.dma("sp", xr[:], x1_d[r0:r0 + 128, :], reads=[x1_b], writes=[xrb], sem_buf=xrb)
                S.op("dve", lambda e, xr=xr, tb=tb: e.scalar_tensor_tensor(xr[:], xr[:], c.ALPHA, acc[:, tb, :], ALU.mult, ALU.add),
                     reads=[xrb, acc_b], writes=[xrb])
                layer_norm_rows(xr, xrb)
                S.dma("sp", dst_d[r0:r0 + 128, :], xr[:], reads=[xrb], pwrites=[dst_b], sem_buf=xrb)
        S.end_phase()

    def dsa_projections(xT_b, qT_b, kT_b, v_b, kiT_b, qiT_b):
        cqT_b, ckvT_b = Buf("cqT"), Buf("ckvT")
        aux = {}
        S.begin_phase()
        xTg = Ring(S, "xTg", 2, [128, DC, 512], BF16)
        wt = Ring(S, "wt", 3, [128, DC, 512], BF16)
        load_w_tile = mk_load_w(wt)
        of32 = Ring(S, "of32", 4, [128, 512], F32)
        sg = Ring(S, "sg", 2, [128, 512], F32)
        xTblk = Ring(S, "xTblk", 2, [128, 8, 128], BF16)
        qnb, qnb_b = S.sbuf("qnb", [128, c.QL], F32)
        kvnb, kvnb_b = S.sbuf("kvnb", [128, c.KVL], F32)
        S.dma("sp", qnb[:], bcast_rows(b_q_norm, c.QL), writes=[qnb_b], sem_buf=qnb_b)
        S.dma("sp", kvnb[:], bcast_rows(b_kv_norm, c.KVL), writes=[kvnb_b], sem_buf=kvnb_b)
        for g in range(NG):
            xg, xgb = xTg.get()
            S.dma("sp", xg[:], xT_d[:, :, g * 512:(g + 1) * 512].rearrange("c p t -> p c t"),
                  reads=[xT_b], writes=[xgb], sem_buf=xgb)
            w0, w0b = load_w_tile(b_w_in, 0, 512)
            w1, w1b = load_w_tile(b_w_in, 512, c.BW - 512)
            for tb in range(4):
                blk = g * 4 + tb
                t0 = blk * 128
                ps0, ps0b = mmR.get()
                mm_chain(ps0[:], ps0b, [(xg[:, k, tb * 128:(tb + 1) * 128], w0[:, k, :]) for k in range(DC)], [xgb, w0b])
                ps1, ps1b = mmR.get()
                mm_chain(ps1[:, 0:336], ps1b, [(xg[:, k, tb * 128:(tb + 1) * 128], w1[:, k, 0:336]) for k in range(DC)], [xgb, w1b])
                cq, cqb = of32.get()
                ckv, ckvb = of32.get()
                sm, smb = small.get()
                sq, sqb = sg.get()
                S.op("act", lambda e, sq=sq, ps0=ps0, sm=sm: e.activation(sq[:], ps0[:], AF.Square, accum_out=sm[:, 0:1]),
                     reads=[ps0b], writes=[sqb, smb])
                S.op("act", lambda e, sq=sq, ps1=ps1, sm=sm: e.activation(sq[:, 0:256], ps1[:, 0:256], AF.Square, accum_out=sm[:, 1:2]),
                     reads=[ps1b], writes=[sqb], pwrites=[smb])
                S.op("act", lambda e, sm=sm: e.activation(sm[:, 2:3], sm[:, 0:1], AF.Sqrt, bias=epsb[:, 1:2], scale=1.0 / c.QL),
                     reads=[smb, epsb_b], pwrites=[smb])
                S.op("act", lambda e, sm=sm: e.activation(sm[:, 3:4], sm[:, 1:2], AF.Sqrt, bias=epsb[:, 1:2], scale=1.0 / c.KVL),
                     reads=[smb, epsb_b], pwrites=[smb])
                S.op("dve", lambda e, sm=sm: e.reciprocal(sm[:, 4:6], sm[:, 2:4]), reads=[smb], pwrites=[smb])
                S.op("dve", lambda e, cq=cq, ps0=ps0, sm=sm: e.scalar_tensor_tensor(cq[:], ps0[:], sm[:, 4:5], qnb[:], ALU.mult, ALU.mult),
                     reads=[ps0b, smb, qnb_b], writes=[cqb])
                S.op("dve", lambda e, ckv=ckv, ps1=ps1, sm=sm: e.scalar_tensor_tensor(ckv[:, 0:256], ps1[:, 0:256], sm[:, 5:6], kvnb[:], ALU.mult, ALU.mult),
                     reads=[ps1b, smb, kvnb_b], writes=[ckvb])
                S.op("dve", lambda e, ckv=ckv, ps1=ps1: e.tensor_copy(ckv[:, 256:320], ps1[:, 256:320]), reads=[ps1b], pwrites=[ckvb])
                S.op("dve", lambda e, ps1=ps1, blk=blk: e.tensor_scalar(wI[:, blk, :], ps1[:, 320:336], 0.25 * 0.125, None, ALU.mult),
                     reads=[ps1b], pwrites=[wI_b])
                tT, tTb = xTblk.get()
                transpose_cols(cq, cqb, 512, lambda k, tT=tT: tT[:, k, :], tTb, F32)
                transpose_cols(ckv, ckvb, 256, lambda k, tT=tT: tT[:, 4 + k, :], tTb, F32, first_full=False)
                pt, ptb = tpfR.get()
                S.op("pe", lambda e, pt=pt, ckv=ckv: e.transpose(pt[0:64, 0:128], ckv[:, 256:320], identf[:]),
                     reads=[ckvb, identf_b], writes=[ptb])
                copy("dve", tT[0:64, 6, :], tTb, pt[0:64, 0:128], ptb, partial=True)
                s1 = aux.setdefault((id(tTb), 1), Buf("s1"))
                s2 = aux.setdefault((id(tTb), 2), Buf("s2"))
                S.dma("pool", cqT_d[:, :, t0:t0 + 128].rearrange("c p t -> p c t"), tT[:, 0:4, :], reads=[tTb], pwrites=[cqT_b], sem_buf=tTb)
                S.dma("pool", ckvT_d[:, :, t0:t0 + 128].rearrange("c p t -> p c t"), tT[:, 4:6, :], reads=[tTb], pwrites=[ckvT_b], sem_buf=s1)
                S.dma("pool", kiT_d[:, t0:t0 + 128], tT[0:64, 6, :], reads=[tTb], pwrites=[kiT_b], sem_buf=s2)
        for g in range(NG):
            cg, cgb = xTg.get()
            S.dma("sp", cg[:, 0:4, :], cqT_d[:, :, g * 512:(g + 1) * 512].rearrange("c p t -> p c t"),
                  reads=[cqT_b], writes=[cgb], sem_buf=cgb)
            s3 = aux.setdefault((id(cgb), 3), Buf("s3"))
            S.dma("sp", cg[:, 4:6, :], ckvT_d[:, :, g * 512:(g + 1) * 512].rearrange("c p t -> p c t"),
                  reads=[ckvT_b], pwrites=[cgb], sem_buf=s3)
            for ct in range(4):
                w, wb = load_w_tile(b_w_uq, ct * 512, 512, nk=4)
                proj_fm(cg, cgb, w, wb, 4, 128, 4, lambda m, ct=ct, g=g: qT_d[ct * 4 + m, :, g * 512:(g + 1) * 512], qT_b)
            for ct in range(2):
                w, wb = load_w_tile(b_w_iq, ct * 512, 512, nk=4)
                proj_fm(cg, cgb, w, wb, 4, 64, 8, lambda m, ct=ct, g=g: qiT_d[ct * 8 + m, :, g * 512:(g + 1) * 512], qiT_b)
            for hq in range(4):
                w, wb = wt.get()
                for hh in range(4):
                    S.dma("pool", w[:, 0:2, hh * 128:(hh + 1) * 128], b_w_uk[hq * 4 + hh].rearrange("(k p) d -> p k d", p=128),
                          sem_buf=wb, **(dict(writes=[wb]) if hh == 0 else dict(pwrites=[wb])))
                proj_fm(cg, cgb, w, wb, 2, 128, 4, lambda m, hq=hq, g=g: kT_d[hq * 4 + m, :, g * 512:(g + 1) * 512], kT_b, k0=4)
                w2, w2b = wt.get()
                for hh in range(4):
                    S.dma("pool", w2[:, 0:2, hh * 128:(hh + 1) * 128], b_w_uv[hq * 4 + hh].rearrange("(k p) d -> p k d", p=128),
                          sem_buf=w2b, **(dict(writes=[w2b]) if hh == 0 else dict(pwrites=[w2b])))
                for tb in range(4):
                    r0 = g * 512 + tb * 128
                    proj_tm(cg, cgb, w2, w2b, 2, 512, tb, v_d[r0:r0 + 128, hq * 512:(hq + 1) * 512], v_b, k0=4)
        S.end_phase()

    def dsa_indexer(kiT_b, qiT_b, mT_b):
        S.begin_phase()
        qiR = Ring(S, "qiR", 2, [64, c.IH, 128], BF16)
        kiAll, kiAll_b = S.sbuf("kiAll", [64, SL], BF16)
        relu = Ring(S, "relu", 3, [128, 512], BF16)
        Irow, Irow_b = S.sbuf("Irow", [128, SL], F32)
        Iwork, Iwork_b = S.sbuf("Iwork", [128, SL], F32)
        Mrow, Mrow_b = S.sbuf("Mrow", [128, SL], BF16)
        m8 = Ring(S, "m8", 2, [128, 8], F32)
        mrow = Ring(S, "mrowi", 2, [128, NB, 128], BF16)
        S.dma("sp", kiAll[:], kiT_d, reads=[kiT_b], writes=[kiAll_b], sem_buf=kiAll_b)
        nrounds = c.IDX_TOPK // 8
        for i in range(NB):
            L = (i + 1) * 128
            qi, qib = qiR.get()
            S.dma("sp", qi[:], qiT_d[:, :, i * 128:(i + 1) * 128].rearrange("h d t -> d h t"), reads=[qiT_b], writes=[qib], sem_buf=qib)
            for s0 in range(0, L, 512):
                sn = min(512, L - s0)
                for hh in range(c.IH):
                    ps, psb = mmR.get()
                    S.op("pe", lambda e, ps=ps, qi=qi, hh=hh, s0=s0, sn=sn: e.matmul(ps[:, 0:sn], qi[:, hh, :], kiAll[:, s0:s0 + sn],
                                                                                      start=True, stop=True),
                         reads=[qib, kiAll_b], writes=[psb])
                    r, rb = relu.get()
                    S.op("act", lambda e, r=r, ps=ps, sn=sn: e.activation(r[:, 0:sn], ps[:, 0:sn], AF.Relu), reads=[psb], writes=[rb])
                    if hh == 0:
                        S.op("dve", lambda e, r=r, i=i, hh=hh, s0=s0, sn=sn: e.tensor_scalar(
                            Irow[:, s0:s0 + sn], r[:, 0:sn], wI[:, i, hh:hh + 1], None, ALU.mult),
                            reads=[rb, wI_b, Irow_b], writes=[Irow_b])
                    else:
                        S.op("dve", lambda e, r=r, i=i, hh=hh, s0=s0, sn=sn: e.scalar_tensor_tensor(
                            Irow[:, s0:s0 + sn], r[:, 0:sn], wI[:, i, hh:hh + 1], Irow[:, s0:s0 + sn], ALU.mult, ALU.add),
                            reads=[rb, wI_b, Irow_b], writes=[Irow_b])
            S.op("dve", lambda e, i=i: e.tensor_tensor(Irow[:, i * 128:(i + 1) * 128], Irow[:, i * 128:(i + 1) * 128], chneg[:], ALU.add),
                 reads=[Irow_b, chneg_b], writes=[Irow_b])
            S.op("dve", lambda e, L=L: e.tensor_copy(Iwork[:, 0:L], Irow[:, 0:L]), reads=[Irow_b], writes=[Iwork_b])
            mx, mxb = m8.get()
            for r_ in range(nrounds):
                S.op("dve", lambda e, mx=mx, L=L: e.max(mx[:], Iwork[:, 0:L]), reads=[Iwork_b], writes=[mxb])
                if r_ < nrounds - 1:
                    S.op("dve", lambda e, mx=mx, L=L: e.match_replace(Iwork[:, 0:L], mx[:], Iwork[:, 0:L], NEG),
                         reads=[mxb, Iwork_b], writes=[Iwork_b])
            sm, smb = small.get()
            S.op("dve", lambda e, sm=sm, mx=mx: e.tensor_tensor(sm[:, 1:2], mx[:, 0:1], mx[:, 7:8], ALU.min), reads=[mxb], writes=[smb])
            S.op("dve", lambda e, sm=sm: e.tensor_scalar_max(sm[:, 0:1], sm[:, 1:2], -1.0e29), reads=[smb], pwrites=[smb])
            S.op("dve", lambda e, sm=sm, L=L: e.tensor_scalar(Mrow[:, 0:L], Irow[:, 0:L], sm[:, 0:1], None, ALU.is_ge),
                 reads=[smb, Irow_b], writes=[Mrow_b])
            mt, mtb = mrow.get()
            transpose_cols(Mrow, Mrow_b, L, lambda k, mt=mt: mt[:, k, :], mtb, BF16)
            S.dma("pool", mT_d[i, 0:i + 1].rearrange("j s t -> s j t"), mt[:, 0:i + 1, :], reads=[mtb], pwrites=[mT_b], sem_buf=mtb)
        S.end_phase()

    out_b = Buf("out")
    for q in range(c.NSEQ):
        cur_d, cur_b = x_in[q], Buf("xin")
        run_depth = getattr(c, "RUN_DEPTH", c.DEPTH)
        for li in range(run_depth):
            xT_b, qT_b, kT_b, v_b, o_b = Buf("xT"), Buf("qT"), Buf("kT"), Buf("v"), Buf("o")
            x1_b, x1T_b, x2_b = Buf("x1"), Buf("x1T"), Buf("x2")
            phase_transpose_in(cur_d, cur_b, xT_d, xT_b)
            if li % 2 == 0:
                fox_projections(xT_b, qT_b, kT_b, v_b)
                attention("fox", qT_b, kT_b, v_b, o_b)
                w_out_ap = a_w_out
            else:
                kiT_b, qiT_b, mT_b = Buf("kiT"), Buf("qiT"), Buf("mT")
                dsa_projections(xT_b, qT_b, kT_b, v_b, kiT_b, qiT_b)
                dsa_indexer(kiT_b, qiT_b, mT_b)
                attention("dsa", qT_b, kT_b, v_b, o_b, mT_b)
                w_out_ap = b_w_out
            outproj_ln_router(li, w_out_ap, cur_d, cur_b, o_b, x1_b, x1T_b)
            if li == run_depth - 1:
                moe_and_ln2(li, x1_b, x1T_b, out[q], out_b)
            else:
                moe_and_ln2(li, x1_b, x1T_b, x2_d, x2_b)
                cur_d, cur_b = x2_d, x2_b
    S.begin_phase()
    S.op("sp", lambda e: e.nop(), reads=[out_b])
    S.end_phase()
    st = S.stats
    S.close()
    return nc, st


def make_consts():
    i = np.arange(128)
    ident = np.eye(128, dtype=np.float32)
    tri = (i[:, None] <= i[None, :]).astype(np.float32)
    ones = np.ones((128, 128), np.float32)
    caus = (i[:, None] <= i[None, :]).astype(np.float32)
    chn = np.where((i[None, :] // 64) <= (i[:, None] // 64), 0.0, NEG).astype(np.float32)
    return np.stack([ident, tri, ones, caus, chn]).astype(np.float32)


def bias_layout(rel_bias):
    s = np.arange(128)[:, None]
    t = np.arange(128)[None, :]
    tiles = []
    for off in (0, 1):
        rel = (s - off * 128) - t
        bk = t5_bucket_np(rel.astype(np.int32))
        tiles.append(np.transpose(rel_bias[bk], (2, 0, 1)))
    return np.ascontiguousarray(np.stack(tiles)).astype(np.float32), np.ascontiguousarray(rel_bias[15:16, :])


_CACHE = {}


def run_cfg(cfg, inputs, core_seqs):
    key = (cfg.S, cfg.E, cfg.TOPK, cfg.IDX_TOPK, cfg.NSEQ, getattr(cfg, 'RUN_DEPTH', 2), getattr(cfg, 'LIMIT', 0))
    if key not in _CACHE:
        _CACHE[key] = build_program(cfg)
    nc, st = _CACHE[key]
    bt, b15 = bias_layout(np.asarray(inputs["rel_bias"], np.float32))
    consts = make_consts()
    shared = {}
    for k in ("a_w_in", "a_b_f", "a_w_out", "b_w_in", "b_q_norm", "b_kv_norm", "b_w_uq", "b_w_iq", "b_w_uk",
              "b_w_uv", "b_w_out"):
        shared[k] = np.ascontiguousarray(np.asarray(inputs[k], np.float32)[0])
    for k in ("ln1_g", "ln1_b", "ln2_g", "ln2_b", "router_w", "router_b", "w_gate", "w_up", "w_down",
              "sh_gate", "sh_up", "sh_down"):
        shared[k] = np.ascontiguousarray(np.asarray(inputs[k], np.float32))
    shared["bias_tiles"] = bt
    shared["bias_far"] = b15
    shared["consts"] = consts
    x = np.asarray(inputs["x"], np.float32)
    in_maps = []
    for seqs in core_seqs:
        m = dict(shared)
        m["x"] = np.ascontiguousarray(x[seqs])
        in_maps.append(m)
    res = run_bass_kernel_spmd(nc, in_maps, core_ids=list(range(len(core_seqs))))
    outp = np.zeros_like(x)
    for ci, seqs in enumerate(core_seqs):
        outp[seqs] = res.results[ci]["out"]
    return outp


def kernel(**inputs):
    x = np.asarray(inputs["x"])
    B, SL, D = x.shape
    E = np.asarray(inputs["router_w"]).shape[-1]
    ncore = 4
    nseq = B // ncore
    cfg = Cfg(S=SL, E=E, TOPK=8, IDX_TOPK=256, NSEQ=nseq)
    core_seqs = [list(range(ci * nseq, (ci + 1) * nseq)) for ci in range(ncore)]
    return run_cfg(cfg, inputs, core_seqs)
```
